# Optimizing a Trainium2 kernel written in Bass

```python
import jax
import jax.numpy as jnp
from jax import lax
import numpy as np

D_MODEL = 1024
BATCH = 8
SEQ = 2048
DEPTH = 2

GRID_W = 64
CTX_LEN = 256

DN_HEADS = 4
DN_DK = 64
DN_DV = 64
DN_CONV = 3
DN_CHUNK = 64
GQA_HEADS = 8
GQA_KV_HEADS = 2
GQA_DIM = 64
MLA_HEADS = 4
MLA_Q_RANK = 192
MLA_KV_RANK = 128
MLA_NOPE = 64
MLA_ROPE = 32
MLA_V = 64
N_EXPERTS = 32
TOP_K = 4
D_EXPERT = 1024
SWIGLU_LIMIT = 7.0
SWIGLU_ALPHA = 1.702

Q_BLOCK = 128
ROPE_THETA = 10000.0
EPS = 1e-6
ALPHA = (2 * DEPTH) ** 0.25
BETA_INIT = (8 * DEPTH) ** -0.25

DN_QK_W = DN_HEADS * DN_DK
DN_V_W = DN_HEADS * DN_DV
IN_SIZES = (
    2 * DN_QK_W + DN_V_W,
    DN_V_W,
    4 * DN_HEADS,
    GQA_HEADS * GQA_DIM,
    GQA_KV_HEADS * GQA_DIM,
    GQA_KV_HEADS * GQA_DIM,
    MLA_Q_RANK,
    MLA_KV_RANK,
    MLA_ROPE,
)
IN_WIDTH = sum(IN_SIZES)
MIX_W = DN_V_W + GQA_HEADS * GQA_DIM + MLA_HEADS * MLA_V

kernel_name = "hybrid_parallel_heads_dit_moe"


def layer_norm(x, g=None, b=None):
    xf = x.astype(jnp.float32)
    mu = jnp.mean(xf, axis=-1, keepdims=True)
    var = jnp.mean(jnp.square(xf - mu), axis=-1, keepdims=True)
    y = (xf - mu) * lax.rsqrt(var + EPS)
    if g is not None:
        y = y * g.astype(jnp.float32) + b.astype(jnp.float32)
    return y.astype(x.dtype)


def rms_norm(x, w):
    xf = x.astype(jnp.float32)
    y = xf * lax.rsqrt(jnp.mean(jnp.square(xf), axis=-1, keepdims=True) + EPS)
    return (y * w.astype(jnp.float32)).astype(x.dtype)


def l2_normalize(x):
    return x * lax.rsqrt(jnp.sum(jnp.square(x), axis=-1, keepdims=True) + EPS)


def rope_1d(x, pos):
    half = x.shape[-1] // 2
    inv_freq = ROPE_THETA ** (-jnp.arange(half, dtype=jnp.float32) / half)
    ang = pos.astype(jnp.float32)[:, None] * inv_freq[None, :]
    cos, sin = jnp.cos(ang), jnp.sin(ang)
    x1 = x[..., :half].astype(jnp.float32)
    x2 = x[..., half:].astype(jnp.float32)
    return jnp.concatenate([x1 * cos - x2 * sin, x2 * cos + x1 * sin], axis=-1).astype(x.dtype)


def rope_2d(x, row, col):
    h = x.shape[-1] // 2
    return jnp.concatenate([rope_1d(x[..., :h], row), rope_1d(x[..., h:], col)], axis=-1)


def merge_heads(t):
    b, h, length, d = t.shape
    return jnp.transpose(t, (0, 2, 1, 3)).reshape(b, length, h * d)


def split_columns(z):
    offsets = np.cumsum(IN_SIZES)[:-1].tolist()
    return jnp.split(z, offsets, axis=-1)


def block_attention(q, k, v, scale):
    b, hkv, grp, lq, dk = q.shape
    nb = lq // Q_BLOCK
    qb = jnp.moveaxis(q.reshape(b, hkv, grp, nb, Q_BLOCK, dk), 3, 0)

    def one_block(qblk):
        s = jnp.einsum('bhgqd,bhkd->bhgqk', qblk, k).astype(jnp.float32) * scale
        p = jax.nn.softmax(s, axis=-1).astype(v.dtype)
        return jnp.einsum('bhgqk,bhkd->bhgqd', p, v)

    o = lax.map(one_block, qb)
    return jnp.moveaxis(o, 0, 3).reshape(b, hkv, grp, lq, v.shape[-1])


def short_conv(x, w):
    ch = x.shape[-1]
    y = lax.conv_general_dilated(
        x, w[:, None, :].astype(x.dtype), window_strides=(1,),
        padding=[((DN_CONV - 1) // 2, DN_CONV // 2)],
        dimension_numbers=('NWC', 'WIO', 'NWC'), feature_group_count=ch)
    return jax.nn.silu(y)


def chunk_gated_delta(q, k, v, g, beta, s0):
    b, h, length, dk = q.shape
    dv = v.shape[-1]
    cs = DN_CHUNK
    n = length // cs
    q = q.reshape(b, h, n, cs, dk)
    k = k.reshape(b, h, n, cs, dk)
    v = v.reshape(b, h, n, cs, dv)
    g = g.reshape(b, h, n, cs)
    beta = beta.reshape(b, h, n, cs)
    gc = jnp.cumsum(g, axis=-1)
    tri = jnp.tril(jnp.ones((cs, cs), dtype=bool))
    strict = jnp.tril(jnp.ones((cs, cs), dtype=bool), -1)
    diff = gc[..., :, None] - gc[..., None, :]
    decay = jnp.where(tri, jnp.exp(jnp.where(tri, diff, 0.0)), 0.0)
    kb = k * beta[..., None]
    vb = v * beta[..., None]
    a_kk = jnp.where(strict, jnp.einsum('bhncd,bhnsd->bhncs', kb, k) * decay, 0.0)
    eye = jnp.eye(cs, dtype=q.dtype)
    t_inv = lax.linalg.triangular_solve(
        eye + a_kk, jnp.broadcast_to(eye, a_kk.shape),
        left_side=True, lower=True, unit_diagonal=True)
    u = jnp.einsum('bhncs,bhnse->bhnce', t_inv, vb)
    w = jnp.einsum('bhncs,bhnsd->bhncd', t_inv, kb * jnp.exp(gc)[..., None])
    qg = q * jnp.exp(gc)[..., None]
    a_qk = jnp.einsum('bhncd,bhnsd->bhncs', q, k) * decay
    g_last = gc[..., -1]
    k_tail = k * jnp.exp(g_last[..., None] - gc)[..., None]

    def step(s, xs):
        u_n, w_n, qg_n, aqk_n, kt_n, gl_n = xs
        v_new = u_n - jnp.einsum('bhcd,bhde->bhce', w_n, s)
        o_n = jnp.einsum('bhcd,bhde->bhce', qg_n, s) + jnp.einsum('bhcs,bhse->bhce', aqk_n, v_new)
        s = s * jnp.exp(gl_n)[..., None, None] + jnp.einsum('bhcd,bhce->bhde', kt_n, v_new)
        return s, o_n

    xs = tuple(jnp.moveaxis(t, 2, 0) for t in (u, w, qg, a_qk, k_tail, g_last))
    s_fin, o = lax.scan(step, s0, xs)
    o = jnp.moveaxis(o, 0, 2).reshape(b, h, length, dv)
    return o, s_fin


def dn_prepare(qkv, ab, conv_w, a_log, dt_bias):
    b, length, _ = qkv.shape
    qkv = short_conv(qkv, conv_w).astype(jnp.float32)
    q = l2_normalize(qkv[..., :DN_QK_W].reshape(b, length, DN_HEADS, DN_DK)) * DN_DK ** -0.5
    k = l2_normalize(qkv[..., DN_QK_W:2 * DN_QK_W].reshape(b, length, DN_HEADS, DN_DK))
    v = qkv[..., 2 * DN_QK_W:].reshape(b, length, DN_HEADS, DN_DV)
    ab = ab.astype(jnp.float32).reshape(b, length, 2, 2, DN_HEADS)
    g = -jnp.exp(a_log.astype(jnp.float32)) * jax.nn.softplus(ab[:, :, 0] + dt_bias.astype(jnp.float32))
    beta = jax.nn.sigmoid(ab[:, :, 1])
    bhl = lambda t: jnp.transpose(t, (0, 2, 1, 3))
    return bhl(q), bhl(k), bhl(v), jnp.transpose(g, (2, 0, 3, 1)), jnp.transpose(beta, (2, 0, 3, 1))


def dn_output(o, gate, norm_w):
    b, h, length, dv = o.shape
    o = jnp.transpose(o, (0, 2, 1, 3))
    y = rms_norm(o, norm_w) * jax.nn.silu(gate.astype(jnp.float32)).reshape(b, length, h, dv)
    return y.reshape(b, length, h * dv).astype(gate.dtype)


def flip_seq(t):
    return jnp.flip(t, axis=2)


def deltanet_mixer(qkv_x, gate_x, ab_x, qkv_c, gate_c, ab_c, conv_w, a_log, dt_bias, norm_w, ctx_out):
    qx, kx, vx, g_x, b_x = dn_prepare(qkv_x, ab_x, conv_w, a_log, dt_bias)
    qc, kc, vc, g_c, b_c = dn_prepare(qkv_c, ab_c, conv_w, a_log, dt_bias)
    s0 = jnp.zeros((qx.shape[0], DN_HEADS, DN_DK, DN_DV), jnp.float32)
    oc_f, sc_f = chunk_gated_delta(qc, kc, vc, g_c[0], b_c[0], s0)
    oc_b, sc_b = chunk_gated_delta(flip_seq(qc), flip_seq(kc), flip_seq(vc),
                                   flip_seq(g_c[1]), flip_seq(b_c[1]), s0)
    ox_f, _ = chunk_gated_delta(qx, kx, vx, g_x[0], b_x[0], sc_f)
    ox_b, _ = chunk_gated_delta(flip_seq(qx), flip_seq(kx), flip_seq(vx),
                                flip_seq(g_x[1]), flip_seq(b_x[1]), sc_b)
    ox = dn_output(ox_f + flip_seq(ox_b), gate_x, norm_w)
    oc = dn_output(oc_f + flip_seq(oc_b), gate_c, norm_w) if ctx_out else None
    return ox, oc


def gqa_mixer(q_x, k_x, v_x, q_c, k_c, v_c, q_norm, k_norm, row, col, ctx_out):
    grp = GQA_HEADS // GQA_KV_HEADS
    scale = GQA_DIM ** -0.5

    def heads(t, n):
        b, length, _ = t.shape
        return jnp.transpose(t.reshape(b, length, n, GQA_DIM), (0, 2, 1, 3))

    kx = rope_2d(rms_norm(heads(k_x, GQA_KV_HEADS), k_norm), row, col)
    kc = rms_norm(heads(k_c, GQA_KV_HEADS), k_norm)
    vx = heads(v_x, GQA_KV_HEADS)
    vc = heads(v_c, GQA_KV_HEADS)
    k_all = jnp.concatenate([kc, kx], axis=2)
    v_all = jnp.concatenate([vc, vx], axis=2)
    qx = rope_2d(rms_norm(heads(q_x, GQA_HEADS), q_norm), row, col)
    b, _, length, _ = qx.shape
    ox = block_attention(qx.reshape(b, GQA_KV_HEADS, grp, length, GQA_DIM), k_all, v_all, scale)
    ox = merge_heads(ox.reshape(b, GQA_HEADS, length, GQA_DIM))
    oc = None
    if ctx_out:
        qc = rms_norm(heads(q_c, GQA_HEADS), q_norm)
        lc = qc.shape[2]
        oc = block_attention(qc.reshape(b, GQA_KV_HEADS, grp, lc, GQA_DIM), kc, vc, scale)
        oc = merge_heads(oc.reshape(b, GQA_HEADS, lc, GQA_DIM))
    return ox, oc


def mla_mixer(cq_x, ckv_x, kr_x, cq_c, ckv_c, kr_c, q_norm, kv_norm, w_uq, w_ukv, row, col, ctx_out):
    scale = (MLA_NOPE + MLA_ROPE) ** -0.5

    def queries(cq, rotate):
        b, length, _ = cq.shape
        q = (rms_norm(cq, q_norm) @ w_uq).reshape(b, length, MLA_HEADS, MLA_NOPE + MLA_ROPE)
        q = jnp.transpose(q, (0, 2, 1, 3))
        q_nope, q_rope = q[..., :MLA_NOPE], q[..., MLA_NOPE:]
        if rotate:
            q_rope = rope_2d(q_rope, row, col)
        return jnp.concatenate([q_nope, q_rope], axis=-1)[:, :, None]

    def keys_values(ckv, kr, rotate):
        b, length, _ = ckv.shape
        kv = (rms_norm(ckv, kv_norm) @ w_ukv).reshape(b, length, MLA_HEADS, MLA_NOPE + MLA_V)
        kv = jnp.transpose(kv, (0, 2, 1, 3))
        k_nope, v = kv[..., :MLA_NOPE], kv[..., MLA_NOPE:]
        kr = kr[:, None]
        if rotate:
            kr = rope_2d(kr, row, col)
        k = jnp.concatenate([k_nope, jnp.broadcast_to(kr, (b, MLA_HEADS, length, MLA_ROPE))], axis=-1)
        return k, v

    kx, vx = keys_values(ckv_x, kr_x, True)
    kc, vc = keys_values(ckv_c, kr_c, False)
    k_all = jnp.concatenate([kc, kx], axis=2)
    v_all = jnp.concatenate([vc, vx], axis=2)
    qx = queries(cq_x, True)
    b, _, _, length, _ = qx.shape
    ox = merge_heads(block_attention(qx, k_all, v_all, scale).reshape(b, MLA_HEADS, length, MLA_V))
    oc = None
    if ctx_out:
        qc = queries(cq_c, False)
        lc = qc.shape[3]
        oc = merge_heads(block_attention(qc, kc, vc, scale).reshape(b, MLA_HEADS, lc, MLA_V))
    return ox, oc


def moe_ffn(h, router_w, router_b, w_gate, b_gate, w_up, b_up, w_down, b_down):
    logits = (h @ router_w + router_b).astype(jnp.float32)
    top_val, top_idx = lax.top_k(logits, TOP_K)
    top_w = jax.nn.softmax(top_val, axis=-1)
    gates = jnp.sum(jax.nn.one_hot(top_idx, N_EXPERTS, dtype=jnp.float32) * top_w[..., None], axis=1)
    y = jnp.zeros(h.shape, jnp.float32)
    for e in range(N_EXPERTS):
        gl = jnp.minimum(h @ w_gate[e] + b_gate[e], SWIGLU_LIMIT)
        up = jnp.clip(h @ w_up[e] + b_up[e], -SWIGLU_LIMIT, SWIGLU_LIMIT)
        act = gl * jax.nn.sigmoid(SWIGLU_ALPHA * gl) * (up + 1.0)
        y = y + gates[:, e:e + 1] * (act @ w_down[e] + b_down[e]).astype(jnp.float32)
    return y.astype(h.dtype)


def modulation(cvec, w, b):
    return jnp.split(jax.nn.silu(cvec) @ w + b, 6, axis=-1)


def modulate(h, shift, scale):
    return layer_norm(h) * (1.0 + scale) + shift


def setup_inputs(seed: int = 0) -> dict:
    key = jax.random.key(seed)
    keys = jax.random.split(key, 30)
    d = D_MODEL

    def nrm(i, shape, scale):
        return scale * jax.random.normal(keys[i], shape, jnp.float32)

    def gain(i, shape):
        return 1.0 + nrm(i, shape, 0.02)

    dt = jnp.exp(jax.random.uniform(keys[9], (DEPTH, 2, DN_HEADS), jnp.float32,
                                    float(np.log(1e-3)), float(np.log(1e-1))))
    return {
        'x': nrm(0, (BATCH, SEQ, d), 1.0),
        'c': nrm(1, (BATCH, d), 1.0),
        'ctx': nrm(2, (BATCH, CTX_LEN, d), 1.0),
        'c_ctx': nrm(3, (d,), 1.0),
        'w_mod': nrm(4, (DEPTH, d, 6 * d), 0.5 * d ** -0.5),
        'b_mod': nrm(5, (DEPTH, 6 * d), 0.02),
        'w_in': nrm(6, (DEPTH, d, IN_WIDTH), d ** -0.5),
        'dn_conv': nrm(7, (DEPTH, DN_CONV, 2 * DN_QK_W + DN_V_W), DN_CONV ** -0.5),
        'dn_a_log': jnp.log(jax.random.uniform(keys[8], (DEPTH, 2, DN_HEADS), jnp.float32, 1.0, 16.0)),
        'dn_dt_bias': dt + jnp.log(-jnp.expm1(-dt)),
        'dn_norm': gain(10, (DEPTH, DN_DV)),
        'gqa_q_norm': gain(11, (DEPTH, GQA_DIM)),
        'gqa_k_norm': gain(12, (DEPTH, GQA_DIM)),
        'mla_q_norm': gain(13, (DEPTH, MLA_Q_RANK)),
        'mla_kv_norm': gain(14, (DEPTH, MLA_KV_RANK)),
        'mla_w_uq': nrm(15, (DEPTH, MLA_Q_RANK, MLA_HEADS * (MLA_NOPE + MLA_ROPE)), MLA_Q_RANK ** -0.5),
        'mla_w_ukv': nrm(16, (DEPTH, MLA_KV_RANK, MLA_HEADS * (MLA_NOPE + MLA_V)), MLA_KV_RANK ** -0.5),
        'w_out': nrm(17, (DEPTH, MIX_W, d), BETA_INIT * MIX_W ** -0.5),
        'ln1_g': gain(18, (DEPTH, d)),
        'ln1_b': nrm(19, (DEPTH, d), 0.02),
        'router_w': nrm(20, (DEPTH, d, N_EXPERTS), d ** -0.5),
        'router_b': nrm(21, (DEPTH, N_EXPERTS), 0.01),
        'exp_w_gate': nrm(22, (DEPTH, N_EXPERTS, d, D_EXPERT), d ** -0.5),
        'exp_b_gate': nrm(23, (DEPTH, N_EXPERTS, D_EXPERT), 0.02),
        'exp_w_up': nrm(24, (DEPTH, N_EXPERTS, d, D_EXPERT), d ** -0.5),
        'exp_b_up': nrm(25, (DEPTH, N_EXPERTS, D_EXPERT), 0.02),
        'exp_w_down': nrm(26, (DEPTH, N_EXPERTS, D_EXPERT, d), BETA_INIT * D_EXPERT ** -0.5),
        'exp_b_down': nrm(27, (DEPTH, N_EXPERTS, d), 0.02),
        'ln2_g': gain(28, (DEPTH, d)),
        'ln2_b': nrm(29, (DEPTH, d), 0.02),
    }


def reference(x, c, ctx, c_ctx, w_mod, b_mod, w_in, dn_conv, dn_a_log, dn_dt_bias, dn_norm,
              gqa_q_norm, gqa_k_norm, mla_q_norm, mla_kv_norm, mla_w_uq, mla_w_ukv, w_out,
              ln1_g, ln1_b, router_w, router_b, exp_w_gate, exp_b_gate, exp_w_up, exp_b_up,
              exp_w_down, exp_b_down, ln2_g, ln2_b):
    b, length, d = x.shape
    ROWS = length // GRID_W
    row = jnp.repeat(jnp.arange(ROWS, dtype=jnp.int32), GRID_W)
    col = jnp.tile(jnp.arange(GRID_W, dtype=jnp.int32), ROWS)
    for l in range(DEPTH):
        ctx_out = l < DEPTH - 1
        sh1, sc1, gt1, sh2, sc2, gt2 = [m[:, None] for m in modulation(c, w_mod[l], b_mod[l])]
        sh1c, sc1c, gt1c, sh2c, sc2c, gt2c = modulation(c_ctx, w_mod[l], b_mod[l])

        zx = split_columns(modulate(x, sh1, sc1) @ w_in[l])
        zc = split_columns(modulate(ctx, sh1c, sc1c) @ w_in[l])
        dn_x, dn_c = deltanet_mixer(zx[0], zx[1], zx[2], zc[0], zc[1], zc[2],
                                    dn_conv[l], dn_a_log[l], dn_dt_bias[l], dn_norm[l], ctx_out)
        gqa_x, gqa_c = gqa_mixer(zx[3], zx[4], zx[5], zc[3], zc[4], zc[5],
                                 gqa_q_norm[l], gqa_k_norm[l], row, col, ctx_out)
        mla_x, mla_c = mla_mixer(zx[6], zx[7], zx[8], zc[6], zc[7], zc[8],
                                 mla_q_norm[l], mla_kv_norm[l], mla_w_uq[l], mla_w_ukv[l],
                                 row, col, ctx_out)
        ox = jnp.concatenate([dn_x, gqa_x, mla_x], axis=-1) @ w_out[l]
        x = layer_norm(ALPHA * x + gt1 * ox, ln1_g[l], ln1_b[l])
        if ctx_out:
            oc = jnp.concatenate([dn_c, gqa_c, mla_c], axis=-1) @ w_out[l]
            ctx = layer_norm(ALPHA * ctx + gt1c * oc, ln1_g[l], ln1_b[l])

        hx = modulate(x, sh2, sc2).reshape(b * length, d)
        if ctx_out:
            lc = ctx.shape[1]
            hc = modulate(ctx, sh2c, sc2c).reshape(b * lc, d)
            y = moe_ffn(jnp.concatenate([hx, hc], axis=0), router_w[l], router_b[l],
                        exp_w_gate[l], exp_b_gate[l], exp_w_up[l], exp_b_up[l],
                        exp_w_down[l], exp_b_down[l])
            ctx = layer_norm(ALPHA * ctx + gt2c * y[b * length:].reshape(b, lc, d), ln2_g[l], ln2_b[l])
            y = y[:b * length]
        else:
            y = moe_ffn(hx, router_w[l], router_b[l], exp_w_gate[l], exp_b_gate[l],
                        exp_w_up[l], exp_b_up[l], exp_w_down[l], exp_b_down[l])
        x = layer_norm(ALPHA * x + gt2 * y.reshape(b, length, d), ln2_g[l], ln2_b[l])
    return x
```

```python
import contextlib
import numpy as np
import concourse.bass as bass
import concourse.mybir as mybir
from concourse.bass_utils import run_bass_kernel_spmd

F32 = mybir.dt.float32
BF16 = mybir.dt.bfloat16
AF = mybir.ActivationFunctionType
ALU = mybir.AluOpType
AX = mybir.AxisListType

NDMA_SEMS = 6
D = 1024
T = 2304
NT = 18
NCTX_T = 2
DEPTH = 2
NE = 32
EPS = 1e-6
ALPHA = (2 * DEPTH) ** 0.25
NEG = -30000.0
ARENA_W = 53100
PHASES = {"dn", "gqa", "mla"}
CUT = 99


class Prog:
    def __init__(self):
        self.nc = bass.Bass("TRN2", target_bir_lowering=False)
        self.ops = []
        self.lastw = {}
        self.readers = {}
        self.es = contextlib.ExitStack()
        self.n_uid = 0
        self._bar_at = -1

    def sb(self, name, shape, dt):
        self.n_uid += 1
        return self.es.enter_context(self.nc.sbuf_tensor(f"{name}_{self.n_uid}", list(shape), dt))

    def ps(self, name, shape, dt=F32):
        self.n_uid += 1
        return self.es.enter_context(self.nc.psum_tensor(f"{name}_{self.n_uid}", list(shape), dt))

    def record(self):
        self._rec = []

    def stop_record(self):
        L = self._rec
        self._rec = None
        return L

    def replay_interleaved(self, lists, skew):
        def is_ps(k):
            return isinstance(k, tuple) and k[0] == "ps"
        written = set()
        for L in lists:
            for o in L:
                written.update(o[3])
                written.update(k for k in o[2] if is_ps(k))
        first, last = [], []
        for L in lists:
            f, la = {}, {}
            for i, o in enumerate(L):
                for k in list(o[2]) + list(o[3]):
                    if k in written:
                        f.setdefault(k, i)
                        la[k] = i
            first.append(f)
            last.append(la)
        need = max(1, skew)
        lastuse = {}
        for t in range(len(lists)):
            for k, fi in first[t].items():
                if k in lastuse:
                    tp_, lp = lastuse[k]
                    d = t - tp_
                    req = -(-(lp - fi) // d)
                    need = max(need, req)
            for k, la in last[t].items():
                lastuse[k] = (t, la)
        items = []
        for t, L in enumerate(lists):
            for i, o in enumerate(L):
                items.append((i + t * need, t, i, o))
        items.sort(key=lambda x: (x[0], x[1], x[2]))
        for _, _, _, o in items:
            self.op(o[0], o[1], r=o[2], w=o[3], dma=o[4])

    def op(self, eng, fn, r=(), w=(), dma=False):
        if getattr(self, "_rec", None) is not None:
            self._rec.append((eng, fn, list(r), list(w), dma))
            return
        r = list(r)
        w = list(w)
        for k in r:
            if isinstance(k, tuple) and k[0] == "ps" and k not in w:
                w.append(k)
        i = len(self.ops)
        deps = set()
        for k in r:
            if k in self.lastw:
                deps.add(self.lastw[k])
        for k in w:
            if k in self.lastw:
                deps.add(self.lastw[k])
            deps.update(self.readers.get(k, ()))
        for k in r:
            self.readers.setdefault(k, []).append(i)
        for k in w:
            self.lastw[k] = i
            self.readers[k] = []
        self.ops.append(dict(eng=eng, fn=fn, deps=deps, dma=dma, sig=dma))
        return i

    def barrier(self):
        last = {}
        for i, o in enumerate(self.ops):
            if o["fn"] is not None and not o["dma"]:
                last[o["eng"]] = i
        dmas = [i for i, o in enumerate(self.ops) if o["dma"] and i > self._bar_at]
        deps = set(last.values()) | set(dmas)
        self._bar_at = len(self.ops)
        for e in ("pe", "act", "dve", "pool", "sp"):
            self.ops.append(dict(eng=e, fn=None, deps=set(deps), dma=False, sig=False))
        self.lastw = {}
        self.readers = {}

    def mm(self, out, lhsT, rhs, r, w, start=True, stop=True):
        self.op("pe", lambda e: e.matmul(out, lhsT=lhsT, rhs=rhs, start=start, stop=stop), r=r, w=w)

    def tp(self, out, in_, ident, r, w):
        self.op("pe", lambda e: e.transpose(out=out, in_=in_, identity=ident), r=list(r) + ["cst"], w=w)

    def act(self, out, in_, func, r, w, bias=None, scale=None, accum=None):
        kw = {}
        if bias is not None:
            kw["bias"] = bias
        if scale is not None:
            kw["scale"] = scale
        if accum is not None:
            kw["accum_out"] = accum
        self.op("act", lambda e: e.activation(out=out, in_=in_, func=func, **kw), r=r, w=w)

    def ts(self, eng, out, in0, s1, s2, op0, op1, r, w):
        if op1 is None:
            self.op(eng, lambda e: e.tensor_scalar(out=out, in0=in0, scalar1=s1, scalar2=None, op0=op0), r=r, w=w)
        else:
            self.op(eng, lambda e: e.tensor_scalar(out=out, in0=in0, scalar1=s1, scalar2=s2, op0=op0, op1=op1), r=r, w=w)

    def tt(self, eng, out, in0, in1, op, r, w):
        self.op(eng, lambda e: e.tensor_tensor(out=out, in0=in0, in1=in1, op=op), r=r, w=w)

    def stt(self, eng, out, in0, sc, in1, op0, op1, r, w):
        self.op(eng, lambda e: e.scalar_tensor_tensor(out=out, in0=in0, scalar=sc, in1=in1, op0=op0, op1=op1), r=r, w=w)

    def cp(self, eng, out, in_, r, w):
        if eng == "act":
            self.op("act", lambda e: e.activation(out=out, in_=in_, func=AF.Identity), r=r, w=w)
        else:
            self.op(eng, lambda e: e.tensor_copy(out=out, in_=in_), r=r, w=w)

    def dma(self, q, out, in_, r, w):
        self.op(q, lambda e: e.dma_start(out=out, in_=in_), r=r, w=w, dma=True)

    def finalize(self):
        nc = self.nc
        ops = self.ops
        engs = ("pe", "act", "dve", "pool", "sp")
        for i, o in enumerate(ops):
            for j in o["deps"]:
                p = ops[j]
                if p["fn"] is None:
                    continue
                if (not p["dma"]) and p["eng"] == "pe" and o["eng"] == "pe" and not o["dma"] and o["fn"] is not None:
                    continue
                p["sig"] = True
        sems = {e: self.es.enter_context(nc.semaphore(f"s_{e}")) for e in engs}
        dsems = {e: [self.es.enter_context(nc.semaphore(f"d_{e}{k}")) for k in range(NDMA_SEMS)]
                 for e in ("sp", "act", "pool")}
        cnt = {e: 0 for e in engs}
        dcnt = {e: 0 for e in dsems}
        for o in ops:
            if o["fn"] is None:
                continue
            e = o["eng"]
            if o["dma"]:
                n = dcnt[e]
                dcnt[e] += 1
                o["sem"] = dsems[e][n % NDMA_SEMS]
                o["val"] = 16 * (n // NDMA_SEMS + 1)
                o["semkey"] = ("d", e, n % NDMA_SEMS)
            elif o["sig"]:
                cnt[e] += 1
                o["sem"] = sems[e]
                o["val"] = cnt[e]
                o["semkey"] = ("c", e)
        per = {e: [] for e in engs}
        for i, o in enumerate(ops):
            per[o["eng"]].append(i)
        final_dma = {}
        for o in ops:
            if o["fn"] is not None and o["dma"]:
                final_dma[o["semkey"]] = (o["sem"], o["val"])

        def emit(ename, eng):
            seen = {}

            def wait(sem, key, val):
                if seen.get(key, 0) >= val:
                    return
                seen[key] = val
                eng.wait_ge(sem, val)

            for i in per[ename]:
                o = ops[i]
                for j in sorted(o["deps"]):
                    p = ops[j]
                    if p["fn"] is None:
                        continue
                    if (not p["dma"]) and p["eng"] == "pe" and ename == "pe" and not o["dma"] and o["fn"] is not None:
                        continue
                    wait(p["sem"], p["semkey"], p["val"])
                if o["fn"] is None:
                    continue
                if o["dma"] and o["val"] > 16:
                    wait(o["sem"], o["semkey"], o["val"] - 16)
                ins = o["fn"](eng)
                if o["dma"]:
                    ins.then_inc(o["sem"], 16)
                elif o["sig"]:
                    ins.then_inc(o["sem"], 1)
            if ename == "sp":
                for key, (sem, val) in final_dma.items():
                    wait(sem, key, val)

        with nc.Block() as block:
            @block.tensor
            def _(e):
                emit("pe", e)

            @block.scalar
            def _(e):
                emit("act", e)

            @block.vector
            def _(e):
                emit("dve", e)

            @block.gpsimd
            def _(e):
                emit("pool", e)

            @block.sync
            def _(e):
                emit("sp", e)
        self.es.close()
        return nc


class Arena:
    def __init__(self, P):
        self.t = P.sb("arena", [128, ARENA_W], F32)
        self.top = 0

    def alloc(self, free_shape, dt=F32):
        n = int(np.prod(free_shape))
        nw = n if dt == F32 else (n + 1) // 2
        assert self.top + nw <= ARENA_W, f"arena overflow {self.top}+{nw}"
        ap = self.t[:, self.top:self.top + nw]
        self.top += nw
        if dt != F32:
            ap = ap.bitcast(dt)[:, 0:n]
        if len(free_shape) == 2:
            ap = ap.rearrange("p (a b) -> p a b", a=free_shape[0])
        elif len(free_shape) == 3:
            ap = ap.rearrange("p (a b c) -> p a b c", a=free_shape[0], b=free_shape[1])
        elif len(free_shape) == 4:
            ap = ap.rearrange("p (a b c d) -> p a b c d", a=free_shape[0], b=free_shape[1], c=free_shape[2])
        return ap


def build(dbg=None):
    P = Prog()
    nc = P.nc

    def din(name, shape):
        return nc.dram_tensor(name, list(shape), F32, kind="ExternalInput").ap()

    xin = din("xin", [T, D])
    cT_d = din("cT", [128, 8, 2])
    w_mod = din("w_mod", [DEPTH, D, 6 * D])
    b_modT = din("b_modT", [DEPTH, 128, 48])
    b_mod = din("b_mod", [DEPTH, 6 * D])
    w_in = din("w_in", [DEPTH, D, 2160])
    convT = din("convT", [DEPTH, 128, 6, 3])
    a_log = din("a_log", [DEPTH, 8])
    dt_bias = din("dt_bias", [DEPTH, 8])
    dn_norm = din("dn_norm", [DEPTH, 64])
    gq_norm = din("gq_norm", [DEPTH, 64])
    gk_norm = din("gk_norm", [DEPTH, 64])
    mq_norm = din("mq_norm", [DEPTH, 192])
    mkv_norm = din("mkv_norm", [DEPTH, 128])
    w_uq = din("w_uq", [DEPTH, 192, 512])
    w_ukv = din("w_ukv", [DEPTH, 128, 512])
    w_out = din("w_out", [DEPTH, D, D])
    ln1_g = din("ln1_g", [DEPTH, D])
    ln1_b = din("ln1_b", [DEPTH, D])
    ln2_g = din("ln2_g", [DEPTH, D])
    ln2_b = din("ln2_b", [DEPTH, D])
    router_w = din("router_w", [DEPTH, D, NE])
    router_b = din("router_b", [DEPTH, NE])
    wg_d = din("exp_w_gate", [DEPTH, NE, D, D])
    wu_d = din("exp_w_up", [DEPTH, NE, D, D])
    wd_d = din("exp_w_down", [DEPTH, NE, D, D])
    bgT = din("bgT", [DEPTH, 128, NE, 8])
    buT = din("buT", [DEPTH, 128, NE, 8])
    b_down = din("b_down", [DEPTH, NE, D])
    cst_d = din("cst", [128, 13, 128])
    ropeg = din("ropeg", [128, 16, 2, 64])
    ropem = din("ropem", [128, 16, 2, 32])
    out_d = nc.dram_tensor("out", [2048, D], F32, kind="ExternalOutput").ap()
    dbg_d = None
    if dbg is not None:
        dbg_d = nc.dram_tensor("dbg", [T, D], F32, kind="ExternalOutput").ap()

    xres = nc.dram_tensor("xres", [T, D], F32, kind="Internal").ap()
    gts = nc.dram_tensor("gts", [DEPTH * 4, D], F32, kind="Internal").ap()

    A = Arena(P)
    pst = [P.ps(f"ps{i}", [128, 512]) for i in range(8)]

    def PS(i):
        return pst[i][:, :]

    cst = A.alloc([13, 128])
    I_ = cst[:, 0, :]
    ONES = cst[:, 5, :]
    BLK = cst[:, 6, :]
    modT = A.alloc([DEPTH, 48, 2])
    epsc = A.alloc([2])
    P.dma("sp", cst, cst_d, r=[], w=["cst"])
    P.op("pool", lambda e: e.memset(epsc[:, 0:1], EPS), w=["epsc"])
    P.op("pool", lambda e: e.memset(epsc[:, 1:2], 1.0), w=["epsc"])
    dummy = A.alloc([8])
    hT = A.alloc([8, T], BF16)
    gates = A.alloc([NT, NE])
    gsc = A.alloc([NT, NE])
    base_top = A.top

    def phase_mod():
        A.top = base_top
        sT = A.alloc([8, 2])
        srep = A.alloc([2, 8, 128])
        wm = [A.alloc([8, 512]) for _ in range(2)]
        bmb = A.alloc([2, 1024])
        bmT = A.alloc([DEPTH, 48])
        grow = A.alloc([4, 1024])
        P.dma("sp", sT, cT_d, r=[], w=["sT"])
        P.dma("sp", bmT, b_modT.rearrange("l p k -> p l k"), r=[], w=["bmT"])
        P.act(sT, sT, AF.Silu, r=["sT"], w=["sT"])
        for j in range(2):
            for dc in range(8):
                P.ts("dve", srep[:, j, dc, :], ONES, sT[:, dc, j:j + 1], None, ALU.mult, None,
                     r=["sT", "cst"], w=[("srep", j, dc)])
        npc = 0
        for l in range(DEPTH):
            for gi, g in enumerate((2, 5)):
                P.dma("sp", bmb[:, gi, :], b_mod[l, g * 1024:(g + 1) * 1024].partition_broadcast(128), r=[], w=[("bmb", gi)])
            for p in range(12):
                g, half = p // 2, p % 2
                buf = wm[npc % 2]
                bk = ("wm", npc % 2)
                npc += 1
                P.dma("sp", buf, w_mod[l, :, p * 512:(p + 1) * 512].rearrange("(c p) n -> p c n", p=128), r=[], w=[bk])
                if g in (0, 1, 3, 4):
                    for fcl in range(4):
                        k = p * 4 + fcl
                        pm = PS(fcl % 2)[:, 0:2]
                        for dc in range(8):
                            P.mm(pm, buf[:, dc, fcl * 128:(fcl + 1) * 128], sT[:, dc, :], r=[bk, "sT"], w=[("ps", fcl % 2)],
                                 start=(dc == 0), stop=(dc == 7))
                        P.ts("dve", modT[:, l, k, :], pm, bmT[:, l, k:k + 1], 1.0 if g in (1, 4) else 0.0, ALU.add, ALU.add,
                             r=[("ps", fcl % 2), "bmT"], w=["modT"])
                else:
                    gi = 0 if g == 2 else 1
                    for j in range(2):
                        pg = PS(2 + j)
                        for dc in range(8):
                            P.mm(pg, srep[:, j, dc, :], buf[:, dc, :], r=[bk, ("srep", j, dc)], w=[("ps", 2 + j)],
                                 start=(dc == 0), stop=(dc == 7))
                        P.tt("dve", grow[0:1, gi * 2 + j, half * 512:(half + 1) * 512], pg[0:1, :],
                             bmb[0:1, gi, half * 512:(half + 1) * 512], ALU.add,
                             r=[("ps", 2 + j), ("bmb", gi)], w=[("grow", gi * 2 + j, half)])
            for q in range(4):
                P.dma("sp", gts[l * 4 + q:l * 4 + q + 1, :], grow[0:1, q, :], r=[("grow", q, 0), ("grow", q, 1)], w=[("gts", l * 4 + q)])
        P.barrier()

    class LNM:
        def __init__(self, n=2):
            self.n = n
            self.xt = [A.alloc([1024]) for _ in range(n)]
            self.st = [A.alloc([2, 6]) for _ in range(n)]
            self.mv = [A.alloc([4]) for _ in range(n)]
            self.i = 0

    def ln_stats(L, k, x_ap, xkey):
        st, mv = L.st[k], L.mv[k]
        P.op("dve", lambda e: e.bn_stats(out=st[:, 0, :], in_=x_ap[:, 0:512]), r=[xkey], w=[("st", k)])
        P.op("dve", lambda e: e.bn_stats(out=st[:, 1, :], in_=x_ap[:, 512:1024]), r=[xkey], w=[("st", k)])
        P.op("dve", lambda e: e.bn_aggr(out=mv[:, 0:2], in_=st.rearrange("p a b -> p (a b)")), r=[("st", k)], w=[("mv", k)])
        P.act(mv[:, 2:3], mv[:, 1:2], AF.Sqrt, r=[("mv", k), "epsc"], w=[("mv", k)], bias=epsc[:, 0:1])
        P.op("dve", lambda e: e.reciprocal(out=mv[:, 3:4], in_=mv[:, 2:3]), r=[("mv", k)], w=[("mv", k)])
        return mv

    def lnmod_tile(L, k, x_ap, xkey, l, ti, which, hf=None, hfkey=None, psb=(4, 5)):
        mv = ln_stats(L, k, x_ap, xkey)
        xh = L.xt[k]
        P.ts("dve", xh, x_ap, mv[:, 0:1], mv[:, 3:4], ALU.subtract, ALU.mult, r=[xkey, ("mv", k)], w=[("xt", k)])
        j = 1 if ti < NCTX_T else 0
        for half in range(2):
            pb = PS(psb[half])
            for q in range(4):
                dc = half * 4 + q
                P.tp(pb[:, q * 128:(q + 1) * 128], xh[:, dc * 128:(dc + 1) * 128], I_, r=[("xt", k)], w=[("ps", psb[half])])
            for q in range(4):
                dc = half * 4 + q
                ksh = (3 * which + 0) * 8 + dc
                ksc = (3 * which + 1) * 8 + dc
                dst = hT[:, dc, ti * 128:(ti + 1) * 128] if hf is None else hf[:, dc, :]
                dkey = ("hT", ti) if hf is None else hfkey
                P.act(dst, pb[:, q * 128:(q + 1) * 128], AF.Identity, r=[("ps", psb[half]), "modT"], w=[dkey],
                      scale=modT[:, l, ksc, j:j + 1], bias=modT[:, l, ksh, j:j + 1])
        if hf is not None:
            P.cp("pool", hT[:, :, ti * 128:(ti + 1) * 128], hf, r=[hfkey], w=[("hT", ti)])

    def phase_lnmod_input():
        A.top = base_top
        NB = 3
        L = LNM(NB)
        xb = [A.alloc([1024]) for _ in range(NB)]
        lists = []
        for ti in range(NT):
            k = ti % NB
            P.record()
            P.dma("sp", xb[k], xin[ti * 128:(ti + 1) * 128, :], r=[], w=[("xb", k)])
            P.dma("act", xres[ti * 128:(ti + 1) * 128, :], xb[k], r=[("xb", k)], w=[("xres", ti)])
            lnmod_tile(L, k, xb[k], ("xb", k), 0, ti, 0, psb=((4, 5), (6, 7), (2, 3))[k])
            lists.append(P.stop_record())
        P.replay_interleaved(lists, max(1, len(lists[0]) // NB))
        P.barrier()

    def phase_dn(l, mixT):
        A.top = mix_top
        w_dn = A.alloc([8, 3, 128], BF16)
        w_ga = A.alloc([8, 272], BF16)
        cw = A.alloc([6, 3])
        zr = [A.alloc([2308])] * 2
        ycs = A.alloc([3, T])
        kTM = A.alloc([NT, 128])
        vTM = A.alloc([NT, 128])
        o_acc = A.alloc([NT, 256])
        ab = A.alloc([NT, 16])
        g_ = A.alloc([NT, 8])
        lnb = A.alloc([NT, 8])
        beta = A.alloc([NT, 8])
        egs = A.alloc([NT, 24])
        gc = A.alloc([NT, 8])
        ngc = A.alloc([NT, 8])
        gb = A.alloc([NT, 8])
        bge = A.alloc([NT, 8])
        tmp8 = A.alloc([NT, 8])
        al_bc = A.alloc([8])
        dtb_bc = A.alloc([8])
        nw_bc = A.alloc([256])
        S_ = A.alloc([2, 64])
        sq = [A.alloc([512]) for _ in range(2)]
        units_start = A.top
        Dm = A.alloc([4, 256])
        X12 = A.alloc([4, 256])
        Nm = A.alloc([4, 128])
        NmT = A.alloc([4, 128])
        R_ = A.alloc([4, 128])
        AQ = [A.alloc([4, 128]) for _ in range(2)]
        u_ = [A.alloc([4, 64]) for _ in range(2)]
        wT = [A.alloc([2, 128]) for _ in range(2)]
        kt = [A.alloc([4, 64]) for _ in range(2)]
        vb = A.alloc([4, 64])
        kbg = A.alloc([4, 64])
        vn = A.alloc([4, 64])
        o1 = A.alloc([4, 64])
        egl2 = A.alloc([NT, 2])

        P.dma("sp", cw, convT[l], r=[], w=["cw"])
        P.dma("sp", al_bc, a_log[l].partition_broadcast(128), r=[], w=["al"])
        P.dma("sp", dtb_bc, dt_bias[l].partition_broadcast(128), r=[], w=["dtb"])
        for h in range(4):
            P.dma("sp", nw_bc[:, h * 64:(h + 1) * 64], dn_norm[l].partition_broadcast(128), r=[], w=["nw"])
        P.dma("pool", w_ga, w_in[l, :, 768:1040].rearrange("(c p) n -> p c n", p=128), r=[], w=["w_ga"])
        P.act(al_bc, al_bc, AF.Exp, r=["al"], w=["al"])
        P.ts("dve", al_bc, al_bc, -1.0, None, ALU.mult, None, r=["al"], w=["al"])

        for ti in range(NT):
            pb = PS(ti % 8)
            for dc in range(8):
                P.mm(pb[:, 0:16], hT[:, dc, ti * 128:(ti + 1) * 128], w_ga[:, dc, 256:272], r=[("hT", ti), "w_ga"], w=[("ps", ti % 8)],
                     start=(dc == 0), stop=(dc == 7))
            P.cp("dve", ab[:, ti, :], pb[:, 0:16], r=[("ps", ti % 8)], w=[("ab", ti)])
        P.op("pool", lambda e: e.memset(dummy[:, 3:4], 0.0), r=[("ab", ti) for ti in range(NT)], w=["ab"])
        for ti in range(NT):
            P.tt("dve", tmp8[:, ti, :], ab[:, ti, 0:8], dtb_bc, ALU.add, r=["ab", "dtb"], w=["tmp8"])
        P.act(tmp8, tmp8, AF.Exp, r=["tmp8"], w=["tmp8"])
        P.act(tmp8, tmp8, AF.Ln, r=["tmp8", "epsc"], w=["tmp8"], bias=epsc[:, 1:2])
        for ti in range(NT):
            P.tt("dve", g_[:, ti, :], tmp8[:, ti, :], al_bc, ALU.mult, r=["tmp8", "al"], w=["g"])
        P.act(beta, ab[:, :, 8:16], AF.Sigmoid, r=["ab"], w=["beta"])
        P.act(lnb, beta, AF.Ln, r=["beta"], w=["lnb"])
        for ti in range(NT):
            pb = PS(ti % 8)
            pbk = ("ps", ti % 8)
            P.mm(pb[:, 0:4], cst[:, 1, :], g_[:, ti, 0:4], r=["g", "cst"], w=[pbk])
            P.mm(pb[:, 4:8], cst[:, 2, :], g_[:, ti, 4:8], r=["g", "cst"], w=[pbk])
            P.mm(pb[:, 8:12], cst[:, 3, :], g_[:, ti, 0:4], r=["g", "cst"], w=[pbk])
            P.mm(pb[:, 12:16], cst[:, 4, :], g_[:, ti, 4:8], r=["g", "cst"], w=[pbk])
            P.mm(pb[:, 16:24], ONES, g_[:, ti, :], r=["g", "cst"], w=[pbk])
            P.cp("dve", gc[:, ti, :], pb[:, 0:8], r=[pbk], w=[("gc", ti)])
            P.act(egs[:, ti, :], pb[:, 0:24], AF.Exp, r=[pbk], w=[("egs", ti)])
        P.op("pool", lambda e: e.memset(dummy[:, 4:5], 0.0), r=[("gc", ti) for ti in range(NT)], w=["gc"])
        P.op("pool", lambda e: e.memset(dummy[:, 5:6], 0.0), r=[("egs", ti) for ti in range(NT)], w=["egs"])
        P.ts("dve", ngc, gc, -1.0, None, ALU.mult, None, r=["gc"], w=["ngc"])
        P.tt("dve", gb, gc, lnb, ALU.add, r=["gc", "lnb"], w=["gb"])
        P.tt("dve", bge, beta, egs[:, :, 0:8], ALU.mult, r=["beta", "egs"], w=["bge"])

        groups = [(0, 512), (512, 512), (1024, 512), (1536, 512), (2048, 256)]

        def pad_idx(t0):
            return 1 + t0 if t0 < 256 else 3 + t0

        for hp in range(2):
            for j in range(3):
                c0 = j * 256 + hp * 128
                P.dma("pool", w_dn[:, :, j, :], w_in[l, :, c0:c0 + 128].rearrange("(c p) n -> p c n", p=128), r=[], w=[("w_dn", j)])
            for j in range(3):
                z = zr[j % 2]
                zk = ("zr", 0)
                P.op("pool", lambda e, z=z: e.memset(z, 0.0), w=[zk])
                for gi, (t0, n) in enumerate(groups):
                    pb = PS(gi % 2)
                    for dc in range(8):
                        P.mm(pb[:, 0:n], w_dn[:, dc, j, :], hT[:, dc, t0:t0 + n], r=[("w_dn", j)] + [("hT", t0 // 128 + q) for q in range(n // 128)],
                             w=[("ps", gi % 2)], start=(dc == 0), stop=(dc == 7))
                    if t0 == 0:
                        P.cp("act", z[:, 1:257], pb[:, 0:256], r=[("ps", gi % 2)], w=[zk])
                        P.cp("act", z[:, 259:515], pb[:, 256:512], r=[("ps", gi % 2)], w=[zk])
                    else:
                        P.cp("act", z[:, 3 + t0:3 + t0 + n], pb[:, 0:n], r=[("ps", gi % 2)], w=[zk])
                fch = j * 2 + hp
                yc = ycs[:, j, :]
                yk = ("ycs", j)
                for (o0, p0, n) in ((0, 1, 256), (256, 259, 2048)):
                    P.ts("dve", yc[:, o0:o0 + n], z[:, p0 - 1:p0 - 1 + n], cw[:, fch, 0:1], None, ALU.mult, None, r=[zk, "cw"], w=[yk])
                    P.stt("dve", yc[:, o0:o0 + n], z[:, p0:p0 + n], cw[:, fch, 1:2], yc[:, o0:o0 + n], ALU.mult, ALU.add, r=[zk, "cw", yk], w=[yk])
                    P.stt("dve", yc[:, o0:o0 + n], z[:, p0 + 1:p0 + 1 + n], cw[:, fch, 2:3], yc[:, o0:o0 + n], ALU.mult, ALU.add, r=[zk, "cw", yk], w=[yk])
                P.act(yc, yc, AF.Silu, r=[yk], w=[yk])
                if j < 2:
                    for gi, (t0, n) in enumerate(groups):
                        s_ = sq[gi % 2]
                        sk = ("sq", gi % 2)
                        P.act(s_[:, 0:n], yc[:, t0:t0 + n], AF.Square, r=[yk], w=[sk])
                        pb = PS(2 + gi % 2)
                        P.mm(pb[:, 0:n], BLK, s_[:, 0:n], r=[sk, "cst"], w=[("ps", 2 + gi % 2)])
                        P.act(s_[:, 0:n], pb[:, 0:n], AF.Sqrt, r=[("ps", 2 + gi % 2), "epsc"], w=[sk], bias=epsc[:, 0:1])
                        P.op("dve", lambda e, s_=s_, n=n: e.reciprocal(out=s_[:, 0:n], in_=s_[:, 0:n]), r=[sk], w=[sk])
                        if j == 0:
                            P.stt("dve", yc[:, t0:t0 + n], yc[:, t0:t0 + n], 0.125, s_[:, 0:n], ALU.mult, ALU.mult, r=[yk, sk], w=[yk])
                        else:
                            P.tt("dve", yc[:, t0:t0 + n], yc[:, t0:t0 + n], s_[:, 0:n], ALU.mult, r=[yk, sk], w=[yk])
            for ti in range(NT):
                pb = PS(4 + ti % 2)
                P.tp(pb[:, 0:128], ycs[:, 1, ti * 128:(ti + 1) * 128], I_, r=[("ycs", 1)], w=[("ps", 4 + ti % 2)])
                P.tp(pb[:, 128:256], ycs[:, 2, ti * 128:(ti + 1) * 128], I_, r=[("ycs", 2)], w=[("ps", 4 + ti % 2)])
                P.cp("act", kTM[:, ti, :], pb[:, 0:128], r=[("ps", 4 + ti % 2)], w=["kTM"])
                P.cp("dve", vTM[:, ti, :], pb[:, 128:256], r=[("ps", 4 + ti % 2)], w=["vTM"])

            c0s = [dr * 4 + 2 * hp for dr in range(2)]
            for dr in range(2):
                for hh in range(2):
                    P.cp("pool", egl2[hh * 64:(hh + 1) * 64, :, dr], egs[hh * 64:(hh + 1) * 64, :, 16 + c0s[dr] + hh], r=["egs"], w=["egl2"])
            P.op("dve", lambda e: e.memset(S_, 0.0), w=[("S", 0), ("S", 1)])
            order = [list(range(NT)), [1, 0] + list(range(NT - 1, 1, -1))]
            E0, E1, K0, K1 = ("ps", 0), ("ps", 1), ("ps", 2), ("ps", 3)

            def local_a(step):
                st = step % 2
                for dr in range(2):
                    ti = order[dr][step]
                    c0 = c0s[dr]
                    i2 = I_.unsqueeze(1).to_broadcast([128, 2, 128])
                    P.tt("dve", Dm[:, dr * 2:dr * 2 + 2, 0:128], i2, gb[:, ti, c0:c0 + 2].unsqueeze(2).to_broadcast([128, 2, 128]), ALU.mult,
                         r=["gb", "cst"], w=["Dm"])
                    P.tt("dve", Dm[:, dr * 2:dr * 2 + 2, 128:256], i2, gc[:, ti, c0:c0 + 2].unsqueeze(2).to_broadcast([128, 2, 128]), ALU.mult,
                         r=["gc", "cst"], w=["Dm"])
                    v2 = vTM[:, ti, :].rearrange("p (h d) -> p h d", h=2)
                    k2 = kTM[:, ti, :].rearrange("p (h d) -> p h d", h=2)
                    P.tt("pool", vb[:, dr * 2:dr * 2 + 2, :], v2, beta[:, ti, c0:c0 + 2].unsqueeze(2).to_broadcast([128, 2, 64]), ALU.mult,
                         r=["vTM", "beta"], w=["vb"])
                    P.tt("pool", kbg[:, dr * 2:dr * 2 + 2, :], k2, bge[:, ti, c0:c0 + 2].unsqueeze(2).to_broadcast([128, 2, 64]), ALU.mult,
                         r=["kTM", "bge"], w=["kbg"])
                    P.tt("pool", kt[st][:, dr * 2:dr * 2 + 2, :], k2, egs[:, ti, 8 + c0:8 + c0 + 2].unsqueeze(2).to_broadcast([128, 2, 64]), ALU.mult,
                         r=["kTM", "egs"], w=[("kt", st)])
                for ui in range(4):
                    dr, hh = ui // 2, ui % 2
                    ti = order[dr][step]
                    r0 = hh * 64
                    tsl = slice(ti * 128, (ti + 1) * 128)
                    pe_ = PS(ui // 2)[:, (ui % 2) * 256:(ui % 2) * 256 + 256]
                    pk = ("ps", ui // 2)
                    P.mm(pe_, ONES, Dm[:, ui, :], r=["Dm", "cst"], w=[pk], start=True, stop=False)
                    P.mm(pe_, I_, cst[:, 7 + 2 * dr:9 + 2 * dr, :].rearrange("p a b -> p (a b)"), r=["cst"], w=[pk], start=False, stop=False)
                    P.mm(pe_, Dm[:, ui, 128:256], cst[:, 11:13, :].rearrange("p a b -> p (a b)"), r=["Dm", "cst"], w=[pk], start=False, stop=True)
                    pq_ = PS(2 + hh)[:, dr * 256:dr * 256 + 256]
                    pk2 = ("ps", 2 + hh)
                    P.mm(pq_[:, 0:128], ycs[r0:r0 + 64, 1, tsl], ycs[r0:r0 + 64, 1, tsl], r=[("ycs", 1)], w=[pk2])
                    P.mm(pq_[:, 128:256], ycs[r0:r0 + 64, 1, tsl], ycs[r0:r0 + 64, 0, tsl], r=[("ycs", 1), ("ycs", 0)], w=[pk2])
                for b_ in range(2):
                    P.act(X12[:, 2 * b_:2 * b_ + 2, :].rearrange("p a b -> p (a b)"), PS(b_), AF.Exp, r=[("ps", b_)], w=["X12"])
                for b_ in range(2):
                    pk3 = PS(2 + b_).rearrange("p (u c) -> p u c", u=2)
                    P.stt("dve", Nm[:, b_::2, :], X12[:, b_::2, 0:128], -1.0, pk3[:, :, 0:128], ALU.mult, ALU.mult,
                          r=["X12", ("ps", 2 + b_)], w=["Nm"])
                    P.tt("dve", AQ[st][:, b_::2, :], X12[:, b_::2, 128:256], pk3[:, :, 128:256], ALU.mult,
                         r=["X12", ("ps", 2 + b_)], w=[("AQ", st)])
                for ui in range(4):
                    P.tp(PS(3)[:, ui * 128:(ui + 1) * 128], Nm[:, ui, :], I_, r=["Nm"], w=[K1])
                P.cp("act", NmT.rearrange("p a b -> p (a b)"), PS(3), r=[K1], w=["NmT"])
                P.tt("pool", R_, Nm, I_.unsqueeze(1).to_broadcast([128, 4, 128]), ALU.add, r=["Nm", "cst"], w=["R"])

            def lvl_bufs(lvl):
                if lvl % 2 == 1:
                    return (Nm, NmT, "Nm", "NmT", X12[:, :, 0:128], X12[:, :, 128:256], "X12", "X12")
                return (X12[:, :, 0:128], X12[:, :, 128:256], "X12", "X12", Nm, NmT, "Nm", "NmT")

            def local_sq(lvl):
                Pp, PpT, kp, kpT, Pn, PnT, kn, knT = lvl_bufs(lvl)
                for ui in range(4):
                    P.mm(PS(0)[:, ui * 128:(ui + 1) * 128], PpT[:, ui, :], Pp[:, ui, :], r=[kp, kpT], w=[E0])
                for ui in range(4):
                    P.mm(PS(1)[:, ui * 128:(ui + 1) * 128], Pp[:, ui, :], PpT[:, ui, :], r=[kp, kpT], w=[E1])
                P.cp("dve", PnT, PS(1).rearrange("p (u c) -> p u c", u=4), r=[E1], w=[knT])
                if lvl < 6:
                    P.cp("act", Pn, PS(0).rearrange("p (u c) -> p u c", u=4), r=[E0], w=[kn])

            def local_ru(lvl):
                Pp, PpT, kp, kpT, Pn, PnT, kn, knT = lvl_bufs(lvl)
                for ui in range(4):
                    P.mm(PS(2)[:, ui * 128:(ui + 1) * 128], PnT[:, ui, :], R_[:, ui, :], r=[knT, "R"], w=[K0])
                P.tt("dve", R_, R_, PS(2).rearrange("p (u c) -> p u c", u=4), ALU.add, r=["R", K0], w=["R"])

            def local_b(step):
                st = step % 2
                for ui in range(4):
                    dr, hh = ui // 2, ui % 2
                    r0 = hh * 64
                    P.mm(PS(0)[:, ui * 64:(ui + 1) * 64], R_[:, ui, :], vb[:, ui, :], r=["R", "vb"], w=[E0])
                    P.mm(PS(1)[r0:r0 + 64, dr * 128:(dr + 1) * 128], kbg[:, ui, :], R_[:, ui, :], r=["R", "kbg"], w=[E1])
                P.cp("act", u_[st].rearrange("p a b -> p (a b)"), PS(0)[:, 0:256], r=[E0], w=[("u", st)])
                P.cp("dve", wT[st].rearrange("p a b -> p (a b)"), PS(1)[:, 0:256], r=[E1], w=[("wT", st)])

            def scan_a(step):
                st = step % 2
                for ui in range(4):
                    dr, hh = ui // 2, ui % 2
                    ti = order[dr][step]
                    r0 = hh * 64
                    tsl = slice(ti * 128, (ti + 1) * 128)
                    Sv = S_[r0:r0 + 64, dr, :]
                    pb = PS(4 + hh)
                    P.mm(pb[:, dr * 64:(dr + 1) * 64], ycs[r0:r0 + 64, 0, tsl], Sv, r=[("ycs", 0), ("S", dr)], w=[("ps", 4 + hh)])
                    P.mm(pb[:, 128 + dr * 64:128 + (dr + 1) * 64], wT[st][r0:r0 + 64, dr, :], Sv, r=[("wT", st), ("S", dr)], w=[("ps", 4 + hh)])
                for hh in range(2):
                    P.tt("dve", vn[:, hh::2, :], u_[st][:, hh::2, :], PS(4 + hh)[:, 128:256].rearrange("p (u c) -> p u c", u=2), ALU.subtract,
                         r=[("u", st), ("ps", 4 + hh)], w=["vn"])
                for ui in range(4):
                    dr, hh = ui // 2, ui % 2
                    ti = order[dr][step]
                    P.ts("dve", o1[:, ui, :], PS(4 + hh)[:, dr * 64:(dr + 1) * 64], egs[:, ti, c0s[dr] + hh:c0s[dr] + hh + 1], None, ALU.mult, None,
                         r=[("ps", 4 + hh), "egs"], w=["o1"])

            def scan_b(step):
                st = step % 2
                for ui in range(4):
                    dr, hh = ui // 2, ui % 2
                    r0 = hh * 64
                    P.mm(PS(6)[:, ui * 64:(ui + 1) * 64], AQ[st][:, ui, :], vn[:, ui, :], r=[("AQ", st), "vn"], w=[("ps", 6)])
                    P.mm(PS(7)[r0:r0 + 64, dr * 64:(dr + 1) * 64], kt[st][:, ui, :], vn[:, ui, :], r=[("kt", st), "vn"], w=[("ps", 7)])
                P.tt("dve", o1.rearrange("p a b -> p (a b)"), o1.rearrange("p a b -> p (a b)"), PS(6)[:, 0:256], ALU.add, r=["o1", ("ps", 6)], w=["o1"])
                for dr in range(2):
                    ti = order[dr][step]
                    oa = o_acc[:, ti, hp * 128:(hp + 1) * 128]
                    ok = ("o_acc", ti, hp)
                    src = o1[:, dr * 2:dr * 2 + 2, :].rearrange("p a b -> p (a b)")
                    if ok not in P.lastw:
                        P.cp("pool", oa, src, r=["o1"], w=[ok])
                    else:
                        P.tt("pool", oa, oa, src, ALU.add, r=["o1", ok], w=[ok])
                    P.stt("dve", S_[:, dr, :], S_[:, dr, :], egl2[:, ti, dr:dr + 1], PS(7)[:, dr * 64:(dr + 1) * 64], ALU.mult, ALU.add,
                          r=[("S", dr), "egl2", ("ps", 7)], w=[("S", dr)])

            local_a(0)
            local_sq(1)
            for lvl in range(2, 7):
                local_sq(lvl)
                local_ru(lvl - 1)
            local_ru(6)
            local_b(0)
            for step in range(NT):
                nxt = step + 1 < NT
                if nxt:
                    local_a(step + 1)
                    local_sq(1)
                scan_a(step)
                if nxt:
                    local_sq(2)
                    local_ru(1)
                    local_sq(3)
                    local_ru(2)
                scan_b(step)
                if nxt:
                    local_sq(4)
                    local_ru(3)
                    local_sq(5)
                    local_ru(4)
                    local_sq(6)
                    local_ru(5)
                    local_ru(6)
                    local_b(step + 1)

        P.barrier()
        A.top = units_start
        ot = [A.alloc([256]) for _ in range(2)]
        osq = [A.alloc([256]) for _ in range(2)]
        oss = [A.alloc([8]) for _ in range(2)]
        sg = [A.alloc([256]) for _ in range(2)]
        for ti in range(NT):
            k = ti % 2
            pb = PS(k)
            for dc in range(8):
                P.mm(pb[:, 0:256], hT[:, dc, ti * 128:(ti + 1) * 128], w_ga[:, dc, 0:256], r=[("hT", ti), "w_ga"], w=[("ps", k)],
                     start=(dc == 0), stop=(dc == 7))
            P.act(sg[k], pb[:, 0:256], AF.Silu, r=[("ps", k)], w=[("sg", k)])
            oa = o_acc[:, ti, :]
            okeys = [("o_acc", ti, h) for h in range(2)]
            P.tt("dve", osq[k], oa, oa, ALU.mult, r=okeys, w=[("osq", k)])
            P.op("dve", lambda e, k=k: e.tensor_reduce(out=oss[k][:, 0:4], in_=osq[k].rearrange("p (h d) -> p h d", h=4), axis=AX.X, op=ALU.add),
                 r=[("osq", k)], w=[("oss", k)])
            P.act(oss[k][:, 4:8], oss[k][:, 0:4], AF.Sqrt, r=[("oss", k), "epsc"], w=[("oss", k)], bias=epsc[:, 0:1], scale=1.0 / 64)
            P.op("dve", lambda e, k=k: e.reciprocal(out=oss[k][:, 0:4], in_=oss[k][:, 4:8]), r=[("oss", k)], w=[("oss", k)])
            P.tt("dve", ot[k].rearrange("p (h d) -> p h d", h=4), oa.rearrange("p (h d) -> p h d", h=4),
                 oss[k][:, 0:4].unsqueeze(2).to_broadcast([128, 4, 64]), ALU.mult, r=okeys + [("oss", k)], w=[("ot", k)])
            P.tt("pool", ot[k], ot[k], nw_bc, ALU.mult, r=[("ot", k), "nw"], w=[("ot", k)])
            P.tt("pool", ot[k], ot[k], sg[k], ALU.mult, r=[("ot", k), ("sg", k)], w=[("ot", k)])
            pt = PS(2 + k)
            P.tp(pt[:, 0:128], ot[k][:, 0:128], I_, r=[("ot", k)], w=[("ps", 2 + k)])
            P.tp(pt[:, 128:256], ot[k][:, 128:256], I_, r=[("ot", k)], w=[("ps", 2 + k)])
            P.cp("act", mixT[:, 0, ti * 128:(ti + 1) * 128], pt[:, 0:128], r=[("ps", 2 + k)], w=[("mixT", 0, ti)])
            P.cp("act", mixT[:, 1, ti * 128:(ti + 1) * 128], pt[:, 128:256], r=[("ps", 2 + k)], w=[("mixT", 1, ti)])
        P.barrier()

    def rms_rope(eng_t, src_ps, dst, nh, hd, wbc, tabs, ti, tk, rope, keys_r, key_w, tmp, tmpk, ss, ssk):
        v3 = lambda a: a.rearrange("p (h d) -> p h d", h=nh)
        tmpv = tmp[:, 0:nh * hd]
        P.act(tmpv, src_ps, AF.Square, r=keys_r, w=[tmpk])
        P.op("dve", lambda e: e.tensor_reduce(out=ss[:, 0:nh], in_=v3(tmpv), axis=AX.X, op=ALU.add), r=[tmpk], w=[ssk])
        P.act(ss[:, nh:2 * nh], ss[:, 0:nh], AF.Sqrt, r=[ssk, "epsc"], w=[ssk], bias=epsc[:, 0:1], scale=1.0 / hd)
        P.op("dve", lambda e: e.reciprocal(out=ss[:, 0:nh], in_=ss[:, nh:2 * nh]), r=[ssk], w=[ssk])
        P.tt("dve", v3(dst), v3(src_ps), ss[:, 0:nh].unsqueeze(2).to_broadcast([128, nh, hd]), ALU.mult, r=keys_r + [ssk], w=[key_w])
        P.tt("pool", dst, dst, wbc, ALU.mult, r=[key_w, "normw"], w=[key_w])
        if rope:
            rope_apply(dst, key_w, nh, hd, hd, 0, tabs, ti, tmp, tmpk)

    def rope_apply(dst, key_w, nh, stride_h, rd, off, tabs, ti, tmp, tmpk):
        q = rd // 4
        xi = ti - NCTX_T
        d4 = dst.rearrange("p (h d) -> p h d", h=nh)[:, :, off:off + rd]
        t4 = tmp[:, 0:nh * rd].rearrange("p (h d) -> p h d", h=nh)
        cos = tabs[:, xi, 0, :].unsqueeze(1).to_broadcast([128, nh, rd])
        sin = tabs[:, xi, 1, :].unsqueeze(1).to_broadcast([128, nh, rd])
        for blk in range(2):
            for hf_ in range(2):
                a0 = blk * 2 * q + hf_ * q
                b0 = blk * 2 * q + (1 - hf_) * q
                P.tt("pool", t4[:, :, a0:a0 + q], d4[:, :, b0:b0 + q], sin[:, :, a0:a0 + q], ALU.mult, r=[key_w, "rope"], w=[tmpk])
        P.tt("dve", d4, d4, cos, ALU.mult, r=[key_w, "rope"], w=[key_w])
        P.tt("dve", d4, d4, t4, ALU.add, r=[key_w, tmpk], w=[key_w])

    def attention(QT, KT_of, V_of, nheads, scale, mixT, chunk_of, l, kq_rows):
        PT = [A.alloc([512], BF16) for _ in range(3)]
        rc = [A.alloc([512]) for _ in range(2)]
        qgroups = [(256 + 512 * i, 512, range(NT)) for i in range(4)]
        if l < DEPTH - 1:
            qgroups = [(0, 256, range(NCTX_T))] + qgroups
        its = []
        gid = 0
        for h in range(nheads):
            for (q0, qn, ktiles) in qgroups:
                kts = list(ktiles)
                for ki, kt in enumerate(kts):
                    its.append(dict(h=h, q0=q0, qn=qn, kt=kt, first=(ki == 0), last=(ki == len(kts) - 1), gid=gid))
                gid += 1

        def emitS(i):
            it = its[i]
            qap, qrows, qkeyf = QT(it["h"])
            kap, kkey = KT_of(it["h"])
            qn, q0, kt = it["qn"], it["q0"], it["kt"]
            P.mm(PS(i % 3)[:, 0:qn], kap[qrows, kt * 128:(kt + 1) * 128], qap[qrows, q0:q0 + qn], r=[kkey, qkeyf], w=[("ps", i % 3)])

        def emitE(i):
            qn = its[i]["qn"]
            P.act(PT[i % 3][:, 0:qn], PS(i % 3)[:, 0:qn], AF.Exp, r=[("ps", i % 3)], w=[("PT", i % 3)], scale=scale)

        def emitPV(i):
            it = its[i]
            qn, q0, g = it["qn"], it["q0"], it["gid"]
            po = PS(6 + g % 2)
            pok = ("ps", 6 + g % 2)
            vap, vkey = V_of(it["h"], it["kt"])
            P.mm(po[:, 0:qn], vap, PT[i % 3][:, 0:qn], r=[vkey, ("PT", i % 3)], w=[pok], start=it["first"], stop=it["last"])
            if it["last"]:
                mc, mr0 = chunk_of(it["h"])
                rcb = rc[g % 2]
                rck = ("rc", g % 2)
                P.op("dve", lambda e: e.reciprocal(out=rcb[0:64, 0:qn], in_=po[64:128, 0:qn]), r=[pok], w=[rck])
                P.tt("dve", mixT[mr0:mr0 + 64, mc, q0:q0 + qn], po[0:64, 0:qn], rcb[0:64, 0:qn], ALU.mult, r=[pok, rck],
                     w=[("mixT", mc, q0 // 128 + i_) for i_ in range(qn // 128)])

        n = len(its)
        emitS(0)
        emitS(1)
        for i in range(n):
            emitE(i)
            if i + 2 < n:
                emitS(i + 2)
            emitPV(i)

    def phase_gqa(l, mixT):
        A.top = mix_top
        w_a = A.alloc([8, 768], BF16)
        QTg = A.alloc([4, T], BF16)
        KTg = A.alloc([2, T], BF16)
        Vg = A.alloc([NT, 2, 128], BF16)
        tabs = A.alloc([16, 2, 64])
        qw = A.alloc([512])
        kw = A.alloc([128])
        qtm = [A.alloc([512]) for _ in range(3)]
        ktm = [A.alloc([128]) for _ in range(3)]
        tmp = [A.alloc([512]) for _ in range(3)]
        ss = [A.alloc([16]) for _ in range(3)]
        P.dma("pool", w_a, w_in[l, :, 1040:1808].rearrange("(c p) n -> p c n", p=128), r=[], w=["w_a"])
        P.dma("sp", tabs, ropeg, r=[], w=["rope"])
        for h in range(8):
            P.dma("sp", qw[:, h * 64:(h + 1) * 64], gq_norm[l].partition_broadcast(128), r=[], w=["normw"])
        for h in range(2):
            P.dma("sp", kw[:, h * 64:(h + 1) * 64], gk_norm[l].partition_broadcast(128), r=[], w=["normw"])
        P.op("pool", lambda e: e.memset(Vg[:, :, :, 64:128], 1.0), w=["Vg1"])
        lists = []
        for ti in range(NT):
            k = ti % 3
            kb = ti % 2
            P.record()
            rope = ti >= NCTX_T
            pq, pk_ = PS(kb), PS(2 + kb)
            hk = [("hT", ti), "w_a"]
            for dc in range(8):
                P.mm(pq, hT[:, dc, ti * 128:(ti + 1) * 128], w_a[:, dc, 0:512], r=hk, w=[("ps", kb)], start=(dc == 0), stop=(dc == 7))
            for dc in range(8):
                P.mm(pk_[:, 0:256], hT[:, dc, ti * 128:(ti + 1) * 128], w_a[:, dc, 512:768], r=hk, w=[("ps", 2 + kb)], start=(dc == 0), stop=(dc == 7))
            need_q = rope or (l < DEPTH - 1)
            if need_q:
                rms_rope("dve", pq, qtm[k], 8, 64, qw, tabs, ti, None, rope, [("ps", kb)], ("qtm", k), tmp[k], ("tmp", k), ss[k], ("ss", k))
                pt = PS(4 + kb)
                for c in range(4):
                    P.tp(pt[:, c * 128:(c + 1) * 128], qtm[k][:, c * 128:(c + 1) * 128], I_, r=[("qtm", k)], w=[("ps", 4 + kb)])
                P.cp("act", QTg[:, :, ti * 128:(ti + 1) * 128], pt.rearrange("p (c n) -> p c n", c=4), r=[("ps", 4 + kb)], w=[("QTg", ti)])
            rms_rope("dve", pk_[:, 0:128], ktm[k], 2, 64, kw, tabs, ti, None, rope, [("ps", 2 + kb)], ("ktm", k), tmp[k], ("tmp", k), ss[k], ("ss", k))
            P.cp("dve", Vg[:, ti, :, 0:64], pk_[:, 128:256].rearrange("p (g d) -> p g d", g=2), r=[("ps", 2 + kb)], w=[("Vg", ti)])
            pt2 = PS(6 + kb)
            P.tp(pt2[:, 0:128], ktm[k], I_, r=[("ktm", k)], w=[("ps", 6 + kb)])
            for g in range(2):
                P.cp("act", KTg[0:64, g, ti * 128:(ti + 1) * 128], pt2[g * 64:(g + 1) * 64, 0:128], r=[("ps", 6 + kb)], w=[("KTg", ti)])
                P.cp("act", KTg[64:128, g, ti * 128:(ti + 1) * 128], pt2[g * 64:(g + 1) * 64, 0:128], r=[("ps", 6 + kb)], w=[("KTg", ti)])
            lists.append(P.stop_record())
        P.replay_interleaved(lists, max(1, len(lists[-1]) // 3))
        allq = [("QTg", ti) for ti in range(NT) if (ti >= NCTX_T or l < DEPTH - 1)]
        allk = [("KTg", ti) for ti in range(NT)]
        P.op("pool", lambda e: e.memset(dummy[:, 0:1], 0.0), r=allq, w=["QTgA"])
        P.op("pool", lambda e: e.memset(dummy[:, 1:2], 0.0), r=allk, w=["KTgA"])
        P.op("pool", lambda e: e.memset(dummy[:, 2:3], 0.0), r=[("Vg", ti) for ti in range(NT)] + ["Vg1"], w=["VgA"])

        def QT(h):
            r0 = (h % 2) * 64
            return QTg[:, h // 2, :], slice(r0, r0 + 64), "QTgA"

        def KT_of(h):
            return KTg[:, h // 4, :], "KTgA"

        def V_of(h, kt):
            return Vg[:, kt, h // 4, :], "VgA"

        def chunk_of(h):
            f0 = 256 + 64 * h
            return f0 // 128, f0 % 128

        attention(QT, KT_of, V_of, 8, 64 ** -0.5, mixT, chunk_of, l, None)
        P.barrier()

    def phase_mla(l, mixT):
        A.top = mix_top
        w_a = A.alloc([8, 352], BF16)
        QTm = A.alloc([4, T], BF16)
        KTm = A.alloc([4, T], BF16)
        Vm = A.alloc([NT, 4, 128], BF16)
        tabs = A.alloc([16, 2, 32])
        wuq = A.alloc([2, 512])
        wukv = A.alloc([512])
        qw = A.alloc([192])
        kvw = A.alloc([128])
        cq = [A.alloc([256]) for _ in range(3)]
        ckv = [A.alloc([128]) for _ in range(3)]
        kr = [A.alloc([32]) for _ in range(3)]
        cqT = [A.alloc([2, 128]) for _ in range(3)]
        ckvT = [A.alloc([128]) for _ in range(3)]
        qtm = [A.alloc([512]) for _ in range(3)]
        kcat = [A.alloc([512]) for _ in range(3)]
        tmp = [A.alloc([512]) for _ in range(3)]
        ss = [A.alloc([16]) for _ in range(3)]
        P.dma("pool", w_a, w_in[l, :, 1808:2160].rearrange("(c p) n -> p c n", p=128), r=[], w=["w_a"])
        P.dma("sp", tabs, ropem, r=[], w=["rope"])
        P.dma("sp", wukv, w_ukv[l], r=[], w=["wukv"])
        P.dma("sp", qw, mq_norm[l].partition_broadcast(128), r=[], w=["normw"])
        P.dma("sp", kvw, mkv_norm[l].partition_broadcast(128), r=[], w=["normw"])
        P.op("pool", lambda e: e.memset(Vm[:, :, :, 64:128], 1.0), w=["Vm1"])
        for k in range(3):
            P.op("pool", lambda e, k=k: e.memset(kcat[k], 0.0), w=[("kcat", k)])
            P.op("pool", lambda e, k=k: e.memset(cq[k], 0.0), w=[("cq", k)])
        P.op("pool", lambda e: e.memset(wuq[:, 1, :], 0.0), w=["wuq1"])
        P.dma("sp", wuq[:, 0, :], w_uq[l, 0:128, :], r=[], w=["wuq"])
        P.dma("sp", wuq[0:64, 1, :], w_uq[l, 128:192, :], r=[], w=["wuq1"])
        lists = []
        for ti in range(NT):
            k = ti % 3
            kb = ti % 2
            P.record()
            rope = ti >= NCTX_T
            pz = PS(kb)
            for dc in range(8):
                P.mm(pz[:, 0:352], hT[:, dc, ti * 128:(ti + 1) * 128], w_a[:, dc, :], r=[("hT", ti), "w_a"], w=[("ps", kb)],
                     start=(dc == 0), stop=(dc == 7))
            need_q = rope or (l < DEPTH - 1)
            rms_rope("dve", pz[:, 192:320], ckv[k], 1, 128, kvw, None, ti, None, False, [("ps", kb)], ("ckv", k), tmp[k], ("tmp", k), ss[k], ("ss", k))
            P.cp("dve", kr[k], pz[:, 320:352], r=[("ps", kb)], w=[("kr", k)])
            if rope:
                rope_apply(kr[k], ("kr", k), 1, 32, 32, 0, tabs, ti, tmp[k], ("tmp", k))
            if need_q:
                rms_rope("dve", pz[:, 0:192], cq[k][:, 0:192], 1, 192, qw, None, ti, None, False, [("ps", kb)], ("cq", k), tmp[k], ("tmp", k), ss[k], ("ss", k))
            pt = PS(2 + kb)
            ptk = ("ps", 2 + kb)
            P.tp(pt[:, 0:128], ckv[k], I_, r=[("ckv", k)], w=[ptk])
            if need_q:
                P.tp(pt[:, 128:256], cq[k][:, 0:128], I_, r=[("cq", k)], w=[ptk])
                P.tp(pt[:, 256:384], cq[k][:, 128:256], I_, r=[("cq", k)], w=[ptk])
                P.cp("act", cqT[k].rearrange("p a b -> p (a b)"), pt[:, 128:384], r=[ptk], w=[("cqT", k)])
            P.cp("dve", ckvT[k], pt[:, 0:128], r=[ptk], w=[("ckvT", k)])
            pu = PS(4 + kb)
            puk = ("ps", 4 + kb)
            P.mm(pu, ckvT[k], wukv, r=[("ckvT", k), "wukv"], w=[puk])
            pu3 = pu.rearrange("p (h d) -> p h d", h=4)
            kc3 = kcat[k].rearrange("p (h d) -> p h d", h=4)
            P.cp("dve", kc3[:, :, 0:64], pu3[:, :, 0:64], r=[puk], w=[("kcat", k)])
            P.cp("act", Vm[:, ti, :, 0:64], pu3[:, :, 64:128], r=[puk], w=[("Vm", ti)])
            P.cp("pool", kc3[:, :, 64:96], kr[k].unsqueeze(1).to_broadcast([128, 4, 32]), r=[("kr", k)], w=[("kcat", k)])
            pt2 = PS(6 + kb)
            pt2k = ("ps", 6 + kb)
            for h in range(4):
                P.tp(pt2[:, h * 128:(h + 1) * 128], kcat[k][:, h * 128:(h + 1) * 128], I_, r=[("kcat", k)], w=[pt2k])
            P.cp("act", KTm[:, :, ti * 128:(ti + 1) * 128], pt2.rearrange("p (c n) -> p c n", c=4), r=[pt2k], w=[("KTm", ti)])
            if need_q:
                pq = PS(4 + kb)
                P.mm(pq, cqT[k][:, 0, :], wuq[:, 0, :], r=[("cqT", k), "wuq"], w=[puk], start=True, stop=False)
                P.mm(pq, cqT[k][:, 1, :], wuq[:, 1, :], r=[("cqT", k), "wuq", "wuq1"], w=[puk], start=False, stop=True)
                P.cp("dve", qtm[k], pq, r=[puk], w=[("qtm", k)])
                if rope:
                    rope_apply(qtm[k], ("qtm", k), 4, 128, 32, 64, tabs, ti, tmp[k], ("tmp", k))
                pt3 = PS(6 + kb)
                for h in range(4):
                    P.tp(pt3[:, h * 128:(h + 1) * 128], qtm[k][:, h * 128:(h + 1) * 128], I_, r=[("qtm", k)], w=[pt2k])
                P.cp("act", QTm[:, :, ti * 128:(ti + 1) * 128], pt3.rearrange("p (c n) -> p c n", c=4), r=[pt2k], w=[("QTm", ti)])
            lists.append(P.stop_record())
        P.replay_interleaved(lists, max(1, len(lists[-1]) // 3))
        if CUT <= 5:
            P.barrier()
            return
        allq = [("QTm", ti) for ti in range(NT) if (ti >= NCTX_T or l < DEPTH - 1)]
        P.op("pool", lambda e: e.memset(dummy[:, 0:1], 0.0), r=allq, w=["QTmA"])
        P.op("pool", lambda e: e.memset(dummy[:, 1:2], 0.0), r=[("KTm", ti) for ti in range(NT)], w=["KTmA"])
        P.op("pool", lambda e: e.memset(dummy[:, 2:3], 0.0), r=[("Vm", ti) for ti in range(NT)] + ["Vm1"], w=["VmA"])

        def QT(h):
            return QTm[:, h, :], slice(0, 128), "QTmA"

        def KT_of(h):
            return KTm[:, h, :], "KTmA"

        def V_of(h, kt):
            return Vm[:, kt, h, :], "VmA"

        def chunk_of(h):
            f0 = 768 + 64 * h
            return f0 // 128, f0 % 128

        attention(QT, KT_of, V_of, 4, 96 ** -0.5, mixT, chunk_of, l, None)
        P.barrier()

    def bc_load(dst, src_row, key):
        P.dma("sp", dst, src_row.partition_broadcast(128), r=[], w=[key])

    def phase_out(l, mixT, yacc, gates, gsc):
        A.top = moe_top
        wo = A.alloc([8, 1024], BF16)
        gt = A.alloc([2, 1024])
        lg = A.alloc([1024])
        lb = A.alloc([1024])
        rw = A.alloc([8, NE])
        rb = A.alloc([NE])
        NB = 3
        L = LNM(NB)
        xr = [A.alloc([1024]) for _ in range(NB)]
        xn = [A.alloc([1024]) for _ in range(NB)]
        hf = [A.alloc([8, 128]) for _ in range(NB)]
        lg_ = [A.alloc([NE]) for _ in range(NB)]
        m8 = [A.alloc([8]) for _ in range(NB)]
        ex = [A.alloc([NE]) for _ in range(NB)]
        msk = [A.alloc([NE]) for _ in range(NB)]
        sm = [A.alloc([4]) for _ in range(NB)]
        P.dma("pool", wo, w_out[l].rearrange("(c p) n -> p c n", p=128), r=[], w=["wo"])
        bc_load(gt[:, 0, :], gts[l * 4 + 0], "gt")
        bc_load(gt[:, 1, :], gts[l * 4 + 1], "gt")
        bc_load(lg, ln1_g[l], "lg")
        bc_load(lb, ln1_b[l], "lb")
        bc_load(rb, router_b[l], "rb")
        P.dma("sp", rw, router_w[l].rearrange("(c p) n -> p c n", p=128), r=[], w=["rw"])
        tiles = [ti for ti in range(NT) if not (l == DEPTH - 1 and ti < NCTX_T)]

        def stage_a(ti):
            k = ti % NB
            j = 1 if ti < NCTX_T else 0
            P.dma("act", xr[k], xres[ti * 128:(ti + 1) * 128, :], r=[("xres", ti)], w=[("xr", k)])
            for half in range(2):
                pb = PS(half)
                for fc in range(8):
                    P.mm(pb, mixT[:, fc, ti * 128:(ti + 1) * 128], wo[:, fc, half * 512:(half + 1) * 512], r=[("mixT", fc, ti), "wo"],
                         w=[("ps", half)], start=(fc == 0), stop=(fc == 7))
                P.tt("dve", xn[k][:, half * 512:(half + 1) * 512], pb, gt[:, j, half * 512:(half + 1) * 512], ALU.mult, r=[("ps", half), "gt"],
                     w=[("xn", k)])
            P.stt("dve", xn[k], xr[k], ALPHA, xn[k], ALU.mult, ALU.add, r=[("xr", k), ("xn", k)], w=[("xn", k)])
            mv = ln_stats(L, k, xn[k], ("xn", k))
            P.ts("dve", xn[k], xn[k], mv[:, 0:1], mv[:, 3:4], ALU.subtract, ALU.mult, r=[("xn", k), ("mv", k)], w=[("xn", k)])
            P.tt("pool", xn[k], xn[k], lg, ALU.mult, r=[("xn", k), "lg"], w=[("xn", k)])
            P.tt("pool", xn[k], xn[k], lb, ALU.add, r=[("xn", k), "lb"], w=[("xn", k)])
            P.dma("act", xres[ti * 128:(ti + 1) * 128, :], xn[k], r=[("xn", k)], w=[("xres", ti)])
            if dbg == ("x1", l):
                P.dma("sp", dbg_d[ti * 128:(ti + 1) * 128, :], xn[k], r=[("xn", k)], w=[])

        def stage_b(ti):
            k = ti % NB
            lnmod_tile(L, k, xn[k], ("xn", k), l, ti, 1, hf=hf[k], hfkey=("hf", k), psb=(2, 3))
            pr = PS(4 + k)
            prk = ("ps", 4 + k)
            for dc in range(8):
                P.mm(pr[:, 0:NE], hf[k][:, dc, :], rw[:, dc, :], r=[("hf", k), "rw"], w=[prk], start=(dc == 0), stop=(dc == 7))
            P.tt("dve", lg_[k], pr[:, 0:NE], rb, ALU.add, r=[prk, "rb"], w=[("lg_", k)])
            P.op("dve", lambda e, k=k: e.max(out=m8[k], in_=lg_[k]), r=[("lg_", k)], w=[("m8", k)])
            P.ts("dve", sm[k][:, 0:1], m8[k][:, 0:1], -1.0, None, ALU.mult, None, r=[("m8", k)], w=[("sm", k)])
            P.act(ex[k], lg_[k], AF.Exp, r=[("lg_", k), ("sm", k)], w=[("ex", k)], bias=sm[k][:, 0:1])
            P.ts("dve", msk[k], lg_[k], m8[k][:, 3:4], None, ALU.is_ge, None, r=[("lg_", k), ("m8", k)], w=[("msk", k)])
            P.tt("dve", ex[k], ex[k], msk[k], ALU.mult, r=[("ex", k), ("msk", k)], w=[("ex", k)])
            P.op("dve", lambda e, k=k: e.tensor_reduce(out=sm[k][:, 1:2], in_=ex[k], axis=AX.X, op=ALU.add), r=[("ex", k)], w=[("sm", k)])
            P.op("dve", lambda e, k=k: e.reciprocal(out=sm[k][:, 2:3], in_=sm[k][:, 1:2]), r=[("sm", k)], w=[("sm", k)])
            P.ts("dve", gates[:, ti, :], ex[k], sm[k][:, 2:3], None, ALU.mult, None, r=[("ex", k), ("sm", k)], w=[("gates", ti)])
            P.ts("dve", gsc[:, ti, :], ex[k], sm[k][:, 2:3], 1.0 / 1.702, ALU.mult, ALU.mult, r=[("ex", k), ("sm", k)], w=[("gsc", ti)])

        lists = []
        for ti in tiles:
            P.record()
            stage_a(ti)
            stage_b(ti)
            lists.append(P.stop_record())
        P.replay_interleaved(lists, max(1, len(lists[0]) // NB))
        P.barrier()

    def phase_moe(l, yacc, gsc):
        A.top = moe_work_top
        wg = A.alloc([8, 8, 128], BF16)
        wu = A.alloc([8, 8, 128], BF16)
        wd = A.alloc([2, 8, 512], BF16)
        actT = [A.alloc([8, 768], BF16) for _ in range(2)]
        bg = A.alloc([NE, 8])
        bu1 = A.alloc([NE, 8])
        glt = [A.alloc([512], BF16) for _ in range(2)]
        sgt = [A.alloc([512], BF16) for _ in range(2)]
        upt = [A.alloc([512], BF16) for _ in range(2)]
        P.dma("sp", bg, bgT[l], r=[], w=["bg"])
        P.dma("sp", bu1, buT[l], r=[], w=["bu1"])
        P.ts("dve", bu1, bu1, 1.0, None, ALU.add, None, r=["bu1"], w=["bu1"])
        bd = A.alloc([1024])
        gT = [A.alloc([128]) for _ in range(2)]
        P.dma("sp", bd[0:NE, :], b_down[l], r=[], w=["bd"])
        for ti in range(NT):
            if l == DEPTH - 1 and ti < NCTX_T:
                continue
            k = ti % 2
            pg = PS(6 + k)
            pgk = ("ps", 6 + k)
            P.tp(pg[0:NE, 0:128], gates[:, ti, :], I_, r=[("gates", ti)], w=[pgk])
            P.cp("act", gT[k][0:NE, :], pg[0:NE, 0:128], r=[pgk], w=[("gT", k)])
            for half in range(2):
                py = PS(4 + half)
                P.mm(py, gT[k][0:NE, :], bd[0:NE, half * 512:(half + 1) * 512], r=[("gT", k), "bd"], w=[("ps", 4 + half)])
                P.cp("act", yacc[:, ti, half * 512:(half + 1) * 512], py, r=[("ps", 4 + half)], w=[("yacc", ti, half)])
        if l < DEPTH - 1:
            parts = [(0, 6), (6, 6), (12, 6)]
        else:
            parts = [(2, 6), (8, 6), (14, 4)]
        cnt = [0]

        def load_gu(e):
            for fc in range(8):
                P.dma("pool", wg[:, fc, :, :], wg_d[l, e, :, fc * 128:(fc + 1) * 128].rearrange("(c p) f -> p c f", p=128), r=[], w=[("wg", fc)])
                P.dma("pool", wu[:, fc, :, :], wu_d[l, e, :, fc * 128:(fc + 1) * 128].rearrange("(c p) f -> p c f", p=128), r=[], w=[("wu", fc)])

        def load_d(e):
            for dh in range(2):
                P.dma("pool", wd[:, dh, :, :], wd_d[l, e, :, dh * 512:(dh + 1) * 512].rearrange("(c p) d -> p c d", p=128), r=[], w=[("wd", dh)])

        def stage_A(e, pi, slot):
            tile0, ntl = parts[pi]
            h0, hn = tile0 * 128, ntl * 128
            ngrp = (hn + 511) // 512
            for fc in range(8):
                for gi in range(ngrp):
                    t0 = h0 + gi * 512
                    n = min(512, h0 + hn - t0)
                    hk = [("hT", t0 // 128 + q) for q in range(n // 128)]
                    k = cnt[0] % 2
                    cnt[0] += 1
                    pg_, pu_ = PS(k), PS(2 + k)
                    for dc in range(8):
                        P.mm(pg_[:, 0:n], wg[:, fc, dc, :], hT[:, dc, t0:t0 + n], r=hk + [("wg", fc)], w=[("ps", k)], start=(dc == 0), stop=(dc == 7))
                    for dc in range(8):
                        P.mm(pu_[:, 0:n], wu[:, fc, dc, :], hT[:, dc, t0:t0 + n], r=hk + [("wu", fc)], w=[("ps", 2 + k)], start=(dc == 0), stop=(dc == 7))
                    P.ts("dve", glt[k][:, 0:n], pg_[:, 0:n], bg[:, e, fc:fc + 1], 7.0, ALU.add, ALU.min, r=[("ps", k), "bg"], w=[("glt", k)])
                    P.act(sgt[k][:, 0:n], glt[k][:, 0:n], AF.Silu, r=[("glt", k)], w=[("sgt", k)], scale=1.702)
                    P.ts("dve", upt[k][:, 0:n], pu_[:, 0:n], bu1[:, e, fc:fc + 1], 8.0, ALU.add, ALU.min, r=[("ps", 2 + k), "bu1"], w=[("upt", k)])
                    P.stt("dve", actT[slot][:, fc, t0 - h0:t0 - h0 + n], upt[k][:, 0:n], -6.0, sgt[k][:, 0:n], ALU.max, ALU.mult,
                          r=[("upt", k), ("sgt", k)], w=[("actT", slot, gi)])

        def stage_B(e, pi, slot):
            tile0, ntl = parts[pi]
            for dh in range(2):
                for tt_ in range(ntl):
                    ti = tile0 + tt_
                    k = cnt[0] % 2
                    cnt[0] += 1
                    py = PS(4 + k)
                    for fc in range(8):
                        P.mm(py, actT[slot][:, fc, tt_ * 128:(tt_ + 1) * 128], wd[:, dh, fc, :], r=[("actT", slot, tt_ // 4), ("wd", dh)], w=[("ps", 4 + k)],
                             start=(fc == 0), stop=(fc == 7))
                    ya = yacc[:, ti, dh * 512:(dh + 1) * 512]
                    P.stt("dve", ya, py, gsc[:, ti, e:e + 1], ya, ALU.mult, ALU.add, r=[("ps", 4 + k), ("gsc", ti), ("yacc", ti, dh)],
                          w=[("yacc", ti, dh)])

        seq = [(e, pi) for e in range(NE) for pi in range(len(parts))]
        load_gu(0)
        load_d(0)
        stage_A(0, 0, 0)
        for i, (e, pi) in enumerate(seq):
            if i + 1 < len(seq):
                e2, p2 = seq[i + 1]
                if p2 == 0:
                    load_gu(e2)
                stage_A(e2, p2, (i + 1) % 2)
            stage_B(e, pi, i % 2)
            if pi == len(parts) - 1 and e + 1 < NE:
                load_d(e + 1)
        P.barrier()

    def phase_final(l, yacc):
        A.top = moe_work_top
        gt = A.alloc([2, 1024])
        lg = A.alloc([1024])
        lb = A.alloc([1024])
        NB = 3
        L = LNM(NB)
        xr = [A.alloc([1024]) for _ in range(NB)]
        lists = []
        bc_load(gt[:, 0, :], gts[l * 4 + 2], "gt")
        bc_load(gt[:, 1, :], gts[l * 4 + 3], "gt")
        bc_load(lg, ln2_g[l], "lg")
        bc_load(lb, ln2_b[l], "lb")
        for ti in range(NT):
            if l == DEPTH - 1 and ti < NCTX_T:
                continue
            k = ti % NB
            j = 1 if ti < NCTX_T else 0
            P.record()
            ya = yacc[:, ti, :]
            yk = [("yacc", ti, 0), ("yacc", ti, 1)]
            P.dma("act", xr[k], xres[ti * 128:(ti + 1) * 128, :], r=[("xres", ti)], w=[("xr", k)])
            P.tt("dve", ya, ya, gt[:, j, :], ALU.mult, r=yk + ["gt"], w=yk)
            P.stt("dve", ya, xr[k], ALPHA, ya, ALU.mult, ALU.add, r=[("xr", k)] + yk, w=yk)
            mv = ln_stats(L, k, ya, yk[0])
            P.ts("dve", ya, ya, mv[:, 0:1], mv[:, 3:4], ALU.subtract, ALU.mult, r=yk + [("mv", k)], w=yk)
            P.tt("pool", ya, ya, lg, ALU.mult, r=yk + ["lg"], w=yk)
            P.tt("pool", ya, ya, lb, ALU.add, r=yk + ["lb"], w=yk)
            if l == DEPTH - 1:
                P.dma("sp", out_d[(ti - NCTX_T) * 128:(ti - NCTX_T + 1) * 128, :], ya, r=yk, w=[])
            else:
                P.dma("sp", xres[ti * 128:(ti + 1) * 128, :], ya, r=yk, w=[("xres", ti)])
                lnmod_tile(L, k, ya, yk[0], l + 1, ti, 0, psb=((4, 5), (6, 7), (2, 3))[k])
            lists.append(P.stop_record())
        P.replay_interleaved(lists, max(1, len(lists[0]) // NB))
        P.barrier()

    mixT = A.alloc([8, T], BF16)
    mix_top = A.top
    A.top = base_top
    yacc = A.alloc([NT, 1024])
    moe_work_top = A.top
    moe_top = mix_top

    phase_mod()
    phase_lnmod_input()
    if dbg == ("h1", 0):
        dump_hT(P, A, hT, dbg_d, PS, I_, base_top)
    for l in range(DEPTH):
        if dbg is not None and dbg[0] in ("h1",) and dbg[1] == l:
            break
        if dbg is not None and len(PHASES) < 3:
            P.op("pool", lambda e: e.memset(mixT, 0.0), w=[("mixT", c, ti) for c in range(8) for ti in range(NT)])
            P.barrier()
        if "dn" in PHASES:
            phase_dn(l, mixT)
        if "gqa" in PHASES:
            phase_gqa(l, mixT)
        if "mla" in PHASES:
            phase_mla(l, mixT)
        if dbg == ("mix", l):
            dump_FM(P, A, mixT, "mixT", dbg_d, PS, I_, moe_top)
            break
        phase_out(l, mixT, yacc, gates, gsc)
        if dbg == ("x1", l):
            break
        phase_moe(l, yacc, gsc)
        phase_final(l, yacc)
        if dbg == ("h1", l + 1):
            dump_hT(P, A, hT, dbg_d, PS, I_, moe_work_top)
            break
    return P.finalize()


def dump_FM(P, A, src, name, dbg_d, PS, I_, top):
    A.top = top
    tf = [A.alloc([128]) for _ in range(2)]
    to = [A.alloc([1024]) for _ in range(2)]
    n = 0
    for ti in range(NT):
        k = ti % 2
        for half in range(2):
            pb = PS(half)
            for q in range(4):
                c = half * 4 + q
                kk = n % 2
                n += 1
                P.cp("dve", tf[kk], src[:, c, ti * 128:(ti + 1) * 128], r=[(name, c, ti), (name[:2], ti)], w=[("tf", kk)])
                P.tp(pb[:, q * 128:(q + 1) * 128], tf[kk], I_, r=[("tf", kk)], w=[("ps", half)])
            P.cp("act", to[k][:, half * 512:(half + 1) * 512], pb, r=[("ps", half)], w=[("to", k)])
        P.dma("sp", dbg_d[ti * 128:(ti + 1) * 128, :], to[k], r=[("to", k)], w=[])
    P.barrier()


def dump_hT(P, A, hT, dbg_d, PS, I_, top):
    dump_FM(P, A, hT, "hT", dbg_d, PS, I_, top)


def _consts():
    j = np.arange(128)[:, None]
    c = np.arange(128)[None, :]
    cst = np.zeros((128, 13, 128), np.float32)
    cst[:, 0] = (j == c)
    cst[:, 1] = (j <= c)
    cst[:, 2] = (j >= c)
    cst[:, 3] = (j > c)
    cst[:, 4] = (j < c)
    cst[:, 5] = 1.0
    cst[:, 6] = ((j // 64) == (c // 64))
    s = j
    cst[:, 7] = np.where(c > s, 0.0, NEG)
    cst[:, 8] = np.where(c >= s, 0.0, NEG)
    cst[:, 9] = np.where(c < s, 0.0, NEG)
    cst[:, 10] = np.where(c <= s, 0.0, NEG)
    cst[:, 11] = -1.0
    cst[:, 12] = -1.0
    return cst


def _rope_tab(rd):
    t = np.arange(2048)
    row = (t // 64).astype(np.float32)
    col = (t % 64).astype(np.float32)
    q = rd // 4
    inv = (10000.0 ** (-np.arange(q, dtype=np.float32) / q)).astype(np.float32)
    ar = row[:, None] * inv[None, :]
    ac = col[:, None] * inv[None, :]
    cos = np.concatenate([np.cos(ar), np.cos(ar), np.cos(ac), np.cos(ac)], 1)
    sin = np.concatenate([-np.sin(ar), np.sin(ar), -np.sin(ac), np.sin(ac)], 1)
    tab = np.stack([cos, sin], 1).astype(np.float32)
    return np.ascontiguousarray(tab.reshape(16, 128, 2, rd).transpose(1, 0, 2, 3))


def _prep_shared(inp):
    f = lambda a: np.ascontiguousarray(np.asarray(a, dtype=np.float32))
    sh = {}
    sh["w_mod"] = f(inp["w_mod"])
    sh["b_mod"] = f(inp["b_mod"])
    sh["b_modT"] = f(np.asarray(inp["b_mod"]).reshape(DEPTH, 48, 128).transpose(0, 2, 1))
    sh["w_in"] = f(inp["w_in"])
    sh["convT"] = f(np.asarray(inp["dn_conv"]).reshape(DEPTH, 3, 6, 128).transpose(0, 3, 2, 1))
    sh["a_log"] = f(np.asarray(inp["dn_a_log"]).reshape(DEPTH, 8))
    sh["dt_bias"] = f(np.asarray(inp["dn_dt_bias"]).reshape(DEPTH, 8))
    sh["dn_norm"] = f(inp["dn_norm"])
    sh["gq_norm"] = f(inp["gqa_q_norm"])
    sh["gk_norm"] = f(inp["gqa_k_norm"])
    sh["mq_norm"] = f(inp["mla_q_norm"])
    sh["mkv_norm"] = f(inp["mla_kv_norm"])
    wuq = np.zeros((DEPTH, 192, 4, 128), np.float32)
    wuq[:, :, :, 0:96] = np.asarray(inp["mla_w_uq"]).reshape(DEPTH, 192, 4, 96)
    sh["w_uq"] = wuq.reshape(DEPTH, 192, 512)
    sh["w_ukv"] = f(inp["mla_w_ukv"])
    sh["w_out"] = f(inp["w_out"])
    for k in ("ln1_g", "ln1_b", "ln2_g", "ln2_b", "router_w", "router_b", "exp_w_gate", "exp_w_up", "exp_w_down"):
        sh[k] = f(inp[k])
    sh["bgT"] = f(np.asarray(inp["exp_b_gate"]).reshape(DEPTH, NE, 8, 128).transpose(0, 3, 1, 2))
    sh["buT"] = f(np.asarray(inp["exp_b_up"]).reshape(DEPTH, NE, 8, 128).transpose(0, 3, 1, 2))
    sh["b_down"] = f(inp["exp_b_down"])
    sh["cst"] = _consts()
    sh["ropeg"] = _rope_tab(64)
    sh["ropem"] = _rope_tab(32)
    return sh


def _prep_core(inp, b):
    x = np.asarray(inp["x"], dtype=np.float32)
    ctx = np.asarray(inp["ctx"], dtype=np.float32)
    c = np.asarray(inp["c"], dtype=np.float32)
    cc = np.asarray(inp["c_ctx"], dtype=np.float32)
    xin = np.ascontiguousarray(np.concatenate([ctx[b], x[b]], 0))
    cT = np.ascontiguousarray(np.stack([c[b].reshape(8, 128).T, cc.reshape(8, 128).T], -1))
    return {"xin": xin, "cT": cT}


_NC_CACHE = {}


def kernel(**inputs):
    if "nc" not in _NC_CACHE:
        _NC_CACHE["nc"] = build()
    nc = _NC_CACHE["nc"]
    sh = _prep_shared(inputs)
    in_maps = []
    for b in range(8):
        m = dict(sh)
        m.update(_prep_core(inputs, b))
        in_maps.append(m)
    res = run_bass_kernel_spmd(nc, in_maps, core_ids=list(range(8)))
    out = np.stack([np.asarray(r["out"], dtype=np.float32) for r in res.results], 0)
    return out
```

```python
import contextlib
import numpy as np
import concourse.bass as bass
import concourse.mybir as mybir
from concourse.bass_utils import run_bass_kernel_spmd

F32 = mybir.dt.float32
BF16 = mybir.dt.bfloat16
AF = mybir.ActivationFunctionType
ALU = mybir.AluOpType
AX = mybir.AxisListType

NDMA_SEMS = 6
D = 1024
T = 2304
NT = 18
NCTX_T = 2
DEPTH = 2
NE = 32
EPS = 1e-6
ALPHA = (2 * DEPTH) ** 0.25
NEG = -30000.0
ARENA_W = 53100
PHASES = {"dn", "gqa", "mla"}
CUT = 99


class Prog:
    def __init__(self):
        self.nc = bass.Bass("TRN2", target_bir_lowering=False)
        self.ops = []
        self.lastw = {}
        self.readers = {}
        self.es = contextlib.ExitStack()
        self.n_uid = 0
        self._bar_at = -1

    def sb(self, name, shape, dt):
        self.n_uid += 1
        return self.es.enter_context(self.nc.sbuf_tensor(f"{name}_{self.n_uid}", list(shape), dt))

    def ps(self, name, shape, dt=F32):
        self.n_uid += 1
        return self.es.enter_context(self.nc.psum_tensor(f"{name}_{self.n_uid}", list(shape), dt))

    def record(self):
        self._rec = []

    def stop_record(self):
        L = self._rec
        self._rec = None
        return L

    def replay_interleaved(self, lists, skew):
        def is_ps(k):
            return isinstance(k, tuple) and k[0] == "ps"
        written = set()
        for L in lists:
            for o in L:
                written.update(o[3])
                written.update(k for k in o[2] if is_ps(k))
        first, last = [], []
        for L in lists:
            f, la = {}, {}
            for i, o in enumerate(L):
                for k in list(o[2]) + list(o[3]):
                    if k in written:
                        f.setdefault(k, i)
                        la[k] = i
            first.append(f)
            last.append(la)
        need = max(1, skew)
        lastuse = {}
        for t in range(len(lists)):
            for k, fi in first[t].items():
                if k in lastuse:
                    tp_, lp = lastuse[k]
                    d = t - tp_
                    req = -(-(lp - fi) // d)
                    need = max(need, req)
            for k, la in last[t].items():
                lastuse[k] = (t, la)
        items = []
        for t, L in enumerate(lists):
            for i, o in enumerate(L):
                items.append((i + t * need, t, i, o))
        items.sort(key=lambda x: (x[0], x[1], x[2]))
        for _, _, _, o in items:
            self.op(o[0], o[1], r=o[2], w=o[3], dma=o[4])

    def op(self, eng, fn, r=(), w=(), dma=False):
        if getattr(self, "_rec", None) is not None:
            self._rec.append((eng, fn, list(r), list(w), dma))
            return
        r = list(r)
        w = list(w)
        for k in r:
            if isinstance(k, tuple) and k[0] == "ps" and k not in w:
                w.append(k)
        i = len(self.ops)
        deps = set()
        for k in r:
            if k in self.lastw:
                deps.add(self.lastw[k])
        for k in w:
            if k in self.lastw:
                deps.add(self.lastw[k])
            deps.update(self.readers.get(k, ()))
        for k in r:
            self.readers.setdefault(k, []).append(i)
        for k in w:
            self.lastw[k] = i
            self.readers[k] = []
        self.ops.append(dict(eng=eng, fn=fn, deps=deps, dma=dma, sig=dma))
        return i

    def barrier(self):
        last = {}
        for i, o in enumerate(self.ops):
            if o["fn"] is not None and not o["dma"]:
                last[o["eng"]] = i
        dmas = [i for i, o in enumerate(self.ops) if o["dma"] and i > self._bar_at]
        deps = set(last.values()) | set(dmas)
        self._bar_at = len(self.ops)
        for e in ("pe", "act", "dve", "pool", "sp"):
            self.ops.append(dict(eng=e, fn=None, deps=set(deps), dma=False, sig=False))
        self.lastw = {}
        self.readers = {}

    def mm(self, out, lhsT, rhs, r, w, start=True, stop=True):
        self.op("pe", lambda e: e.matmul(out, lhsT=lhsT, rhs=rhs, start=start, stop=stop), r=r, w=w)

    def tp(self, out, in_, ident, r, w):
        self.op("pe", lambda e: e.transpose(out=out, in_=in_, identity=ident), r=list(r) + ["cst"], w=w)

    def act(self, out, in_, func, r, w, bias=None, scale=None, accum=None):
        kw = {}
        if bias is not None:
            kw["bias"] = bias
        if scale is not None:
            kw["scale"] = scale
        if accum is not None:
            kw["accum_out"] = accum
        self.op("act", lambda e: e.activation(out=out, in_=in_, func=func, **kw), r=r, w=w)

    def ts(self, eng, out, in0, s1, s2, op0, op1, r, w):
        if op1 is None:
            self.op(eng, lambda e: e.tensor_scalar(out=out, in0=in0, scalar1=s1, scalar2=None, op0=op0), r=r, w=w)
        else:
            self.op(eng, lambda e: e.tensor_scalar(out=out, in0=in0, scalar1=s1, scalar2=s2, op0=op0, op1=op1), r=r, w=w)

    def tt(self, eng, out, in0, in1, op, r, w):
        self.op(eng, lambda e: e.tensor_tensor(out=out, in0=in0, in1=in1, op=op), r=r, w=w)

    def stt(self, eng, out, in0, sc, in1, op0, op1, r, w):
        self.op(eng, lambda e: e.scalar_tensor_tensor(out=out, in0=in0, scalar=sc, in1=in1, op0=op0, op1=op1), r=r, w=w)

    def cp(self, eng, out, in_, r, w):
        if eng == "act":
            self.op("act", lambda e: e.activation(out=out, in_=in_, func=AF.Identity), r=r, w=w)
        else:
            self.op(eng, lambda e: e.tensor_copy(out=out, in_=in_), r=r, w=w)

    def dma(self, q, out, in_, r, w):
        self.op(q, lambda e: e.dma_start(out=out, in_=in_), r=r, w=w, dma=True)

    def finalize(self):
        nc = self.nc
        ops = self.ops
        engs = ("pe", "act", "dve", "pool", "sp")
        for i, o in enumerate(ops):
            for j in o["deps"]:
                p = ops[j]
                if p["fn"] is None:
                    continue
                if (not p["dma"]) and p["eng"] == "pe" and o["eng"] == "pe" and not o["dma"] and o["fn"] is not None:
                    continue
                p["sig"] = True
        sems = {e: self.es.enter_context(nc.semaphore(f"s_{e}")) for e in engs}
        dsems = {e: [self.es.enter_context(nc.semaphore(f"d_{e}{k}")) for k in range(NDMA_SEMS)]
                 for e in ("sp", "act", "pool")}
        cnt = {e: 0 for e in engs}
        dcnt = {e: 0 for e in dsems}
        for o in ops:
            if o["fn"] is None:
                continue
            e = o["eng"]
            if o["dma"]:
                n = dcnt[e]
                dcnt[e] += 1
                o["sem"] = dsems[e][n % NDMA_SEMS]
                o["val"] = 16 * (n // NDMA_SEMS + 1)
                o["semkey"] = ("d", e, n % NDMA_SEMS)
            elif o["sig"]:
                cnt[e] += 1
                o["sem"] = sems[e]
                o["val"] = cnt[e]
                o["semkey"] = ("c", e)
        per = {e: [] for e in engs}
        for i, o in enumerate(ops):
            per[o["eng"]].append(i)
        final_dma = {}
        for o in ops:
            if o["fn"] is not None and o["dma"]:
                final_dma[o["semkey"]] = (o["sem"], o["val"])

        def emit(ename, eng):
            seen = {}

            def wait(sem, key, val):
                if seen.get(key, 0) >= val:
                    return
                seen[key] = val
                eng.wait_ge(sem, val)

            for i in per[ename]:
                o = ops[i]
                for j in sorted(o["deps"]):
                    p = ops[j]
                    if p["fn"] is None:
                        continue
                    if (not p["dma"]) and p["eng"] == "pe" and ename == "pe" and not o["dma"] and o["fn"] is not None:
                        continue
                    wait(p["sem"], p["semkey"], p["val"])
                if o["fn"] is None:
                    continue
                if o["dma"] and o["val"] > 16:
                    wait(o["sem"], o["semkey"], o["val"] - 16)
                ins = o["fn"](eng)
                if o["dma"]:
                    ins.then_inc(o["sem"], 16)
                elif o["sig"]:
                    ins.then_inc(o["sem"], 1)
            if ename == "sp":
                for key, (sem, val) in final_dma.items():
                    wait(sem, key, val)

        with nc.Block() as block:
            @block.tensor
            def _(e):
                emit("pe", e)

            @block.scalar
            def _(e):
                emit("act", e)

            @block.vector
            def _(e):
                emit("dve", e)

            @block.gpsimd
            def _(e):
                emit("pool", e)

            @block.sync
            def _(e):
                emit("sp", e)
        self.es.close()
        return nc


class Arena:
    def __init__(self, P):
        self.t = P.sb("arena", [128, ARENA_W], F32)
        self.top = 0

    def alloc(self, free_shape, dt=F32):
        n = int(np.prod(free_shape))
        nw = n if dt == F32 else (n + 1) // 2
        assert self.top + nw <= ARENA_W, f"arena overflow {self.top}+{nw}"
        ap = self.t[:, self.top:self.top + nw]
        self.top += nw
        if dt != F32:
            ap = ap.bitcast(dt)[:, 0:n]
        if len(free_shape) == 2:
            ap = ap.rearrange("p (a b) -> p a b", a=free_shape[0])
        elif len(free_shape) == 3:
            ap = ap.rearrange("p (a b c) -> p a b c", a=free_shape[0], b=free_shape[1])
        elif len(free_shape) == 4:
            ap = ap.rearrange("p (a b c d) -> p a b c d", a=free_shape[0], b=free_shape[1], c=free_shape[2])
        return ap


def build(dbg=None):
    P = Prog()
    nc = P.nc

    def din(name, shape):
        return nc.dram_tensor(name, list(shape), F32, kind="ExternalInput").ap()

    xin = din("xin", [T, D])
    cT_d = din("cT", [128, 8, 2])
    w_mod = din("w_mod", [DEPTH, D, 6 * D])
    b_modT = din("b_modT", [DEPTH, 128, 48])
    b_mod = din("b_mod", [DEPTH, 6 * D])
    w_in = din("w_in", [DEPTH, D, 2160])
    convT = din("convT", [DEPTH, 128, 6, 3])
    a_log = din("a_log", [DEPTH, 8])
    dt_bias = din("dt_bias", [DEPTH, 8])
    dn_norm = din("dn_norm", [DEPTH, 64])
    gq_norm = din("gq_norm", [DEPTH, 64])
    gk_norm = din("gk_norm", [DEPTH, 64])
    mq_norm = din("mq_norm", [DEPTH, 192])
    mkv_norm = din("mkv_norm", [DEPTH, 128])
    w_uq = din("w_uq", [DEPTH, 192, 512])
    w_ukv = din("w_ukv", [DEPTH, 128, 512])
    w_out = din("w_out", [DEPTH, D, D])
    ln1_g = din("ln1_g", [DEPTH, D])
    ln1_b = din("ln1_b", [DEPTH, D])
    ln2_g = din("ln2_g", [DEPTH, D])
    ln2_b = din("ln2_b", [DEPTH, D])
    router_w = din("router_w", [DEPTH, D, NE])
    router_b = din("router_b", [DEPTH, NE])
    wg_d = din("exp_w_gate", [DEPTH, NE, D, D])
    wu_d = din("exp_w_up", [DEPTH, NE, D, D])
    wd_d = din("exp_w_down", [DEPTH, NE, D, D])
    bgT = din("bgT", [DEPTH, 128, NE, 8])
    buT = din("buT", [DEPTH, 128, NE, 8])
    b_down = din("b_down", [DEPTH, NE, D])
    cst_d = din("cst", [128, 13, 128])
    ropeg = din("ropeg", [128, 16, 2, 64])
    ropem = din("ropem", [128, 16, 2, 32])
    out_d = nc.dram_tensor("out", [2048, D], F32, kind="ExternalOutput").ap()
    dbg_d = None
    if dbg is not None:
        dbg_d = nc.dram_tensor("dbg", [T, D], F32, kind="ExternalOutput").ap()

    xres = nc.dram_tensor("xres", [T, D], F32, kind="Internal").ap()
    gts = nc.dram_tensor("gts", [DEPTH * 4, D], F32, kind="Internal").ap()

    A = Arena(P)
    pst = [P.ps(f"ps{i}", [128, 512]) for i in range(8)]

    def PS(i):
        return pst[i][:, :]

    cst = A.alloc([13, 128])
    I_ = cst[:, 0, :]
    ONES = cst[:, 5, :]
    BLK = cst[:, 6, :]
    modT = A.alloc([DEPTH, 48, 2])
    epsc = A.alloc([2])
    P.dma("sp", cst, cst_d, r=[], w=["cst"])
    P.op("pool", lambda e: e.memset(epsc[:, 0:1], EPS), w=["epsc"])
    P.op("pool", lambda e: e.memset(epsc[:, 1:2], 1.0), w=["epsc"])
    dummy = A.alloc([8])
    hT = A.alloc([8, T], BF16)
    gates = A.alloc([NT, NE])
    gsc = A.alloc([NT, NE])
    base_top = A.top

    def phase_mod():
        A.top = base_top
        sT = A.alloc([8, 2])
        srep = A.alloc([2, 8, 128])
        wm = [A.alloc([8, 512]) for _ in range(2)]
        bmb = A.alloc([2, 1024])
        bmT = A.alloc([DEPTH, 48])
        grow = A.alloc([4, 1024])
        P.dma("sp", sT, cT_d, r=[], w=["sT"])
        P.dma("sp", bmT, b_modT.rearrange("l p k -> p l k"), r=[], w=["bmT"])
        P.act(sT, sT, AF.Silu, r=["sT"], w=["sT"])
        for j in range(2):
            for dc in range(8):
                P.ts("dve", srep[:, j, dc, :], ONES, sT[:, dc, j:j + 1], None, ALU.mult, None,
                     r=["sT", "cst"], w=[("srep", j, dc)])
        npc = 0
        for l in range(DEPTH):
            for gi, g in enumerate((2, 5)):
                P.dma("sp", bmb[:, gi, :], b_mod[l, g * 1024:(g + 1) * 1024].partition_broadcast(128), r=[], w=[("bmb", gi)])
            for p in range(12):
                g, half = p // 2, p % 2
                buf = wm[npc % 2]
                bk = ("wm", npc % 2)
                npc += 1
                P.dma("sp", buf, w_mod[l, :, p * 512:(p + 1) * 512].rearrange("(c p) n -> p c n", p=128), r=[], w=[bk])
                if g in (0, 1, 3, 4):
                    for fcl in range(4):
                        k = p * 4 + fcl
                        pm = PS(fcl % 2)[:, 0:2]
                        for dc in range(8):
                            P.mm(pm, buf[:, dc, fcl * 128:(fcl + 1) * 128], sT[:, dc, :], r=[bk, "sT"], w=[("ps", fcl % 2)],
                                 start=(dc == 0), stop=(dc == 7))
                        P.ts("dve", modT[:, l, k, :], pm, bmT[:, l, k:k + 1], 1.0 if g in (1, 4) else 0.0, ALU.add, ALU.add,
                             r=[("ps", fcl % 2), "bmT"], w=["modT"])
                else:
                    gi = 0 if g == 2 else 1
                    for j in range(2):
                        pg = PS(2 + j)
                        for dc in range(8):
                            P.mm(pg, srep[:, j, dc, :], buf[:, dc, :], r=[bk, ("srep", j, dc)], w=[("ps", 2 + j)],
                                 start=(dc == 0), stop=(dc == 7))
                        P.tt("dve", grow[0:1, gi * 2 + j, half * 512:(half + 1) * 512], pg[0:1, :],
                             bmb[0:1, gi, half * 512:(half + 1) * 512], ALU.add,
                             r=[("ps", 2 + j), ("bmb", gi)], w=[("grow", gi * 2 + j, half)])
            for q in range(4):
                P.dma("sp", gts[l * 4 + q:l * 4 + q + 1, :], grow[0:1, q, :], r=[("grow", q, 0), ("grow", q, 1)], w=[("gts", l * 4 + q)])
        P.barrier()

    class LNM:
        def __init__(self, n=2):
            self.n = n
            self.xt = [A.alloc([1024]) for _ in range(n)]
            self.st = [A.alloc([2, 6]) for _ in range(n)]
            self.mv = [A.alloc([4]) for _ in range(n)]
            self.i = 0

    def ln_stats(L, k, x_ap, xkey):
        st, mv = L.st[k], L.mv[k]
        P.op("dve", lambda e: e.bn_stats(out=st[:, 0, :], in_=x_ap[:, 0:512]), r=[xkey], w=[("st", k)])
        P.op("dve", lambda e: e.bn_stats(out=st[:, 1, :], in_=x_ap[:, 512:1024]), r=[xkey], w=[("st", k)])
        P.op("dve", lambda e: e.bn_aggr(out=mv[:, 0:2], in_=st.rearrange("p a b -> p (a b)")), r=[("st", k)], w=[("mv", k)])
        P.act(mv[:, 2:3], mv[:, 1:2], AF.Sqrt, r=[("mv", k), "epsc"], w=[("mv", k)], bias=epsc[:, 0:1])
        P.op("dve", lambda e: e.reciprocal(out=mv[:, 3:4], in_=mv[:, 2:3]), r=[("mv", k)], w=[("mv", k)])
        return mv

    def lnmod_tile(L, k, x_ap, xkey, l, ti, which, hf=None, hfkey=None, psb=(4, 5)):
        mv = ln_stats(L, k, x_ap, xkey)
        xh = L.xt[k]
        P.ts("dve", xh, x_ap, mv[:, 0:1], mv[:, 3:4], ALU.subtract, ALU.mult, r=[xkey, ("mv", k)], w=[("xt", k)])
        j = 1 if ti < NCTX_T else 0
        for half in range(2):
            pb = PS(psb[half])
            for q in range(4):
                dc = half * 4 + q
                P.tp(pb[:, q * 128:(q + 1) * 128], xh[:, dc * 128:(dc + 1) * 128], I_, r=[("xt", k)], w=[("ps", psb[half])])
            for q in range(4):
                dc = half * 4 + q
                ksh = (3 * which + 0) * 8 + dc
                ksc = (3 * which + 1) * 8 + dc
                dst = hT[:, dc, ti * 128:(ti + 1) * 128] if hf is None else hf[:, dc, :]
                dkey = ("hT", ti) if hf is None else hfkey
                P.act(dst, pb[:, q * 128:(q + 1) * 128], AF.Identity, r=[("ps", psb[half]), "modT"], w=[dkey],
                      scale=modT[:, l, ksc, j:j + 1], bias=modT[:, l, ksh, j:j + 1])
        if hf is not None:
            P.cp("pool", hT[:, :, ti * 128:(ti + 1) * 128], hf, r=[hfkey], w=[("hT", ti)])

    def phase_lnmod_input():
        A.top = base_top
        NB = 3
        L = LNM(NB)
        xb = [A.alloc([1024]) for _ in range(NB)]
        lists = []
        for ti in range(NT):
            k = ti % NB
            P.record()
            P.dma("sp", xb[k], xin[ti * 128:(ti + 1) * 128, :], r=[], w=[("xb", k)])
            P.dma("act", xres[ti * 128:(ti + 1) * 128, :], xb[k], r=[("xb", k)], w=[("xres", ti)])
            lnmod_tile(L, k, xb[k], ("xb", k), 0, ti, 0, psb=((4, 5), (6, 7), (2, 3))[k])
            lists.append(P.stop_record())
        P.replay_interleaved(lists, max(1, len(lists[0]) // NB))
        P.barrier()

    def phase_dn(l, mixT):
        A.top = mix_top
        w_dn = A.alloc([8, 3, 128], BF16)
        w_ga = A.alloc([8, 272], BF16)
        cw = A.alloc([6, 3])
        zr = [A.alloc([2308])] * 2
        ycs = A.alloc([3, T])
        kTM = A.alloc([NT, 128])
        vTM = A.alloc([NT, 128])
        o_acc = A.alloc([NT, 256])
        ab = A.alloc([NT, 16])
        g_ = A.alloc([NT, 8])
        lnb = A.alloc([NT, 8])
        beta = A.alloc([NT, 8])
        egs = A.alloc([NT, 24])
        gc = A.alloc([NT, 8])
        ngc = A.alloc([NT, 8])
        gb = A.alloc([NT, 8])
        bge = A.alloc([NT, 8])
        tmp8 = A.alloc([NT, 8])
        al_bc = A.alloc([8])
        dtb_bc = A.alloc([8])
        nw_bc = A.alloc([256])
        S_ = A.alloc([2, 64])
        sq = [A.alloc([512]) for _ in range(2)]
        units_start = A.top
        Dm = A.alloc([4, 256])
        X12 = A.alloc([4, 256])
        Nm = A.alloc([4, 128])
        NmT = A.alloc([4, 128])
        R_ = A.alloc([4, 128])
        AQ = [A.alloc([4, 128]) for _ in range(2)]
        u_ = [A.alloc([4, 64]) for _ in range(2)]
        wT = [A.alloc([2, 128]) for _ in range(2)]
        kt = [A.alloc([4, 64]) for _ in range(2)]
        vb = A.alloc([4, 64])
        kbg = A.alloc([4, 64])
        vn = A.alloc([4, 64])
        o1 = A.alloc([4, 64])
        egl2 = A.alloc([NT, 2])

        P.dma("sp", cw, convT[l], r=[], w=["cw"])
        P.dma("sp", al_bc, a_log[l].partition_broadcast(128), r=[], w=["al"])
        P.dma("sp", dtb_bc, dt_bias[l].partition_broadcast(128), r=[], w=["dtb"])
        for h in range(4):
            P.dma("sp", nw_bc[:, h * 64:(h + 1) * 64], dn_norm[l].partition_broadcast(128), r=[], w=["nw"])
        P.dma("pool", w_ga, w_in[l, :, 768:1040].rearrange("(c p) n -> p c n", p=128), r=[], w=["w_ga"])
        P.act(al_bc, al_bc, AF.Exp, r=["al"], w=["al"])
        P.ts("dve", al_bc, al_bc, -1.0, None, ALU.mult, None, r=["al"], w=["al"])

        for ti in range(NT):
            pb = PS(ti % 8)
            for dc in range(8):
                P.mm(pb[:, 0:16], hT[:, dc, ti * 128:(ti + 1) * 128], w_ga[:, dc, 256:272], r=[("hT", ti), "w_ga"], w=[("ps", ti % 8)],
                     start=(dc == 0), stop=(dc == 7))
            P.cp("dve", ab[:, ti, :], pb[:, 0:16], r=[("ps", ti % 8)], w=[("ab", ti)])
        P.op("pool", lambda e: e.memset(dummy[:, 3:4], 0.0), r=[("ab", ti) for ti in range(NT)], w=["ab"])
        for ti in range(NT):
            P.tt("dve", tmp8[:, ti, :], ab[:, ti, 0:8], dtb_bc, ALU.add, r=["ab", "dtb"], w=["tmp8"])
        P.act(tmp8, tmp8, AF.Exp, r=["tmp8"], w=["tmp8"])
        P.act(tmp8, tmp8, AF.Ln, r=["tmp8", "epsc"], w=["tmp8"], bias=epsc[:, 1:2])
        for ti in range(NT):
            P.tt("dve", g_[:, ti, :], tmp8[:, ti, :], al_bc, ALU.mult, r=["tmp8", "al"], w=["g"])
        P.act(beta, ab[:, :, 8:16], AF.Sigmoid, r=["ab"], w=["beta"])
        P.act(lnb, beta, AF.Ln, r=["beta"], w=["lnb"])
        for ti in range(NT):
            pb = PS(ti % 8)
            pbk = ("ps", ti % 8)
            P.mm(pb[:, 0:4], cst[:, 1, :], g_[:, ti, 0:4], r=["g", "cst"], w=[pbk])
            P.mm(pb[:, 4:8], cst[:, 2, :], g_[:, ti, 4:8], r=["g", "cst"], w=[pbk])
            P.mm(pb[:, 8:12], cst[:, 3, :], g_[:, ti, 0:4], r=["g", "cst"], w=[pbk])
            P.mm(pb[:, 12:16], cst[:, 4, :], g_[:, ti, 4:8], r=["g", "cst"], w=[pbk])
            P.mm(pb[:, 16:24], ONES, g_[:, ti, :], r=["g", "cst"], w=[pbk])
            P.cp("dve", gc[:, ti, :], pb[:, 0:8], r=[pbk], w=[("gc", ti)])
            P.act(egs[:, ti, :], pb[:, 0:24], AF.Exp, r=[pbk], w=[("egs", ti)])
        P.op("pool", lambda e: e.memset(dummy[:, 4:5], 0.0), r=[("gc", ti) for ti in range(NT)], w=["gc"])
        P.op("pool", lambda e: e.memset(dummy[:, 5:6], 0.0), r=[("egs", ti) for ti in range(NT)], w=["egs"])
        P.ts("dve", ngc, gc, -1.0, None, ALU.mult, None, r=["gc"], w=["ngc"])
        P.tt("dve", gb, gc, lnb, ALU.add, r=["gc", "lnb"], w=["gb"])
        P.tt("dve", bge, beta, egs[:, :, 0:8], ALU.mult, r=["beta", "egs"], w=["bge"])

        groups = [(0, 512), (512, 512), (1024, 512), (1536, 512), (2048, 256)]

        def pad_idx(t0):
            return 1 + t0 if t0 < 256 else 3 + t0

        for hp in range(2):
            for j in range(3):
                c0 = j * 256 + hp * 128
                P.dma("pool", w_dn[:, :, j, :], w_in[l, :, c0:c0 + 128].rearrange("(c p) n -> p c n", p=128), r=[], w=[("w_dn", j)])
            for j in range(3):
                z = zr[j % 2]
                zk = ("zr", 0)
                P.op("pool", lambda e, z=z: e.memset(z, 0.0), w=[zk])
                for gi, (t0, n) in enumerate(groups):
                    pb = PS(gi % 2)
                    for dc in range(8):
                        P.mm(pb[:, 0:n], w_dn[:, dc, j, :], hT[:, dc, t0:t0 + n], r=[("w_dn", j)] + [("hT", t0 // 128 + q) for q in range(n // 128)],
                             w=[("ps", gi % 2)], start=(dc == 0), stop=(dc == 7))
                    if t0 == 0:
                        P.cp("act", z[:, 1:257], pb[:, 0:256], r=[("ps", gi % 2)], w=[zk])
                        P.cp("act", z[:, 259:515], pb[:, 256:512], r=[("ps", gi % 2)], w=[zk])
                    else:
                        P.cp("act", z[:, 3 + t0:3 + t0 + n], pb[:, 0:n], r=[("ps", gi % 2)], w=[zk])
                fch = j * 2 + hp
                yc = ycs[:, j, :]
                yk = ("ycs", j)
                for (o0, p0, n) in ((0, 1, 256), (256, 259, 2048)):
                    P.ts("dve", yc[:, o0:o0 + n], z[:, p0 - 1:p0 - 1 + n], cw[:, fch, 0:1], None, ALU.mult, None, r=[zk, "cw"], w=[yk])
                    P.stt("dve", yc[:, o0:o0 + n], z[:, p0:p0 + n], cw[:, fch, 1:2], yc[:, o0:o0 + n], ALU.mult, ALU.add, r=[zk, "cw", yk], w=[yk])
                    P.stt("dve", yc[:, o0:o0 + n], z[:, p0 + 1:p0 + 1 + n], cw[:, fch, 2:3], yc[:, o0:o0 + n], ALU.mult, ALU.add, r=[zk, "cw", yk], w=[yk])
                P.act(yc, yc, AF.Silu, r=[yk], w=[yk])
                if j < 2:
                    for gi, (t0, n) in enumerate(groups):
                        s_ = sq[gi % 2]
                        sk = ("sq", gi % 2)
                        P.act(s_[:, 0:n], yc[:, t0:t0 + n], AF.Square, r=[yk], w=[sk])
                        pb = PS(2 + gi % 2)
                        P.mm(pb[:, 0:n], BLK, s_[:, 0:n], r=[sk, "cst"], w=[("ps", 2 + gi % 2)])
                        P.act(s_[:, 0:n], pb[:, 0:n], AF.Sqrt, r=[("ps", 2 + gi % 2), "epsc"], w=[sk], bias=epsc[:, 0:1])
                        P.op("dve", lambda e, s_=s_, n=n: e.reciprocal(out=s_[:, 0:n], in_=s_[:, 0:n]), r=[sk], w=[sk])
                        if j == 0:
                            P.stt("dve", yc[:, t0:t0 + n], yc[:, t0:t0 + n], 0.125, s_[:, 0:n], ALU.mult, ALU.mult, r=[yk, sk], w=[yk])
                        else:
                            P.tt("dve", yc[:, t0:t0 + n], yc[:, t0:t0 + n], s_[:, 0:n], ALU.mult, r=[yk, sk], w=[yk])
            for ti in range(NT):
                pb = PS(ti % 8)
                pbk = ("ps", ti % 8)
                P.tp(pb[:, 0:128], ycs[:, 1, ti * 128:(ti + 1) * 128], I_, r=[("ycs", 1)], w=[pbk])
                P.tp(pb[:, 128:256], ycs[:, 2, ti * 128:(ti + 1) * 128], I_, r=[("ycs", 2)], w=[pbk])
                P.cp("act", kTM[:, ti, :], pb[:, 0:128], r=[pbk], w=[("kTM", ti)])
                P.cp("dve", vTM[:, ti, :], pb[:, 128:256], r=[pbk], w=[("vTM", ti)])
            P.op("pool", lambda e: e.memset(dummy[:, 6:7], 0.0), r=[("kTM", ti) for ti in range(NT)], w=["kTM"])
            P.op("pool", lambda e: e.memset(dummy[:, 7:8], 0.0), r=[("vTM", ti) for ti in range(NT)], w=["vTM"])

            c0s = [dr * 4 + 2 * hp for dr in range(2)]
            for dr in range(2):
                for hh in range(2):
                    P.cp("pool", egl2[hh * 64:(hh + 1) * 64, :, dr], egs[hh * 64:(hh + 1) * 64, :, 16 + c0s[dr] + hh], r=["egs"], w=["egl2"])
            P.op("dve", lambda e: e.memset(S_, 0.0), w=[("S", 0), ("S", 1)])
            order = [list(range(NT)), [1, 0] + list(range(NT - 1, 1, -1))]
            E0, E1, K0, K1 = ("ps", 0), ("ps", 1), ("ps", 2), ("ps", 3)

            def local_a(step):
                st = step % 2
                for dr in range(2):
                    ti = order[dr][step]
                    c0 = c0s[dr]
                    i2 = I_.unsqueeze(1).to_broadcast([128, 2, 128])
                    P.tt("dve", Dm[:, dr * 2:dr * 2 + 2, 0:128], i2, gb[:, ti, c0:c0 + 2].unsqueeze(2).to_broadcast([128, 2, 128]), ALU.mult,
                         r=["gb", "cst"], w=["Dm"])
                    P.tt("dve", Dm[:, dr * 2:dr * 2 + 2, 128:256], i2, gc[:, ti, c0:c0 + 2].unsqueeze(2).to_broadcast([128, 2, 128]), ALU.mult,
                         r=["gc", "cst"], w=["Dm"])
                    v2 = vTM[:, ti, :].rearrange("p (h d) -> p h d", h=2)
                    k2 = kTM[:, ti, :].rearrange("p (h d) -> p h d", h=2)
                    P.tt("pool", vb[:, dr * 2:dr * 2 + 2, :], v2, beta[:, ti, c0:c0 + 2].unsqueeze(2).to_broadcast([128, 2, 64]), ALU.mult,
                         r=["vTM", "beta"], w=["vb"])
                    P.tt("pool", kbg[:, dr * 2:dr * 2 + 2, :], k2, bge[:, ti, c0:c0 + 2].unsqueeze(2).to_broadcast([128, 2, 64]), ALU.mult,
                         r=["kTM", "bge"], w=["kbg"])
                    P.tt("pool", kt[st][:, dr * 2:dr * 2 + 2, :], k2, egs[:, ti, 8 + c0:8 + c0 + 2].unsqueeze(2).to_broadcast([128, 2, 64]), ALU.mult,
                         r=["kTM", "egs"], w=[("kt", st)])
                for ui in range(4):
                    dr, hh = ui // 2, ui % 2
                    ti = order[dr][step]
                    r0 = hh * 64
                    tsl = slice(ti * 128, (ti + 1) * 128)
                    pe_ = PS(ui // 2)[:, (ui % 2) * 256:(ui % 2) * 256 + 256]
                    pk = ("ps", ui // 2)
                    P.mm(pe_, ONES, Dm[:, ui, :], r=["Dm", "cst"], w=[pk], start=True, stop=False)
                    P.mm(pe_, I_, cst[:, 7 + 2 * dr:9 + 2 * dr, :].rearrange("p a b -> p (a b)"), r=["cst"], w=[pk], start=False, stop=False)
                    P.mm(pe_, Dm[:, ui, 128:256], cst[:, 11:13, :].rearrange("p a b -> p (a b)"), r=["Dm", "cst"], w=[pk], start=False, stop=True)
                    pq_ = PS(2 + hh)[:, dr * 256:dr * 256 + 256]
                    pk2 = ("ps", 2 + hh)
                    P.mm(pq_[:, 0:128], ycs[r0:r0 + 64, 1, tsl], ycs[r0:r0 + 64, 1, tsl], r=[("ycs", 1)], w=[pk2])
                    P.mm(pq_[:, 128:256], ycs[r0:r0 + 64, 1, tsl], ycs[r0:r0 + 64, 0, tsl], r=[("ycs", 1), ("ycs", 0)], w=[pk2])
                for b_ in range(2):
                    P.act(X12[:, 2 * b_:2 * b_ + 2, :].rearrange("p a b -> p (a b)"), PS(b_), AF.Exp, r=[("ps", b_)], w=["X12"])
                for b_ in range(2):
                    pk3 = PS(2 + b_).rearrange("p (u c) -> p u c", u=2)
                    P.stt("dve", Nm[:, b_::2, :], X12[:, b_::2, 0:128], -1.0, pk3[:, :, 0:128], ALU.mult, ALU.mult,
                          r=["X12", ("ps", 2 + b_)], w=["Nm"])
                    P.tt("dve", AQ[st][:, b_::2, :], X12[:, b_::2, 128:256], pk3[:, :, 128:256], ALU.mult,
                         r=["X12", ("ps", 2 + b_)], w=[("AQ", st)])
                for ui in range(4):
                    P.tp(PS(3)[:, ui * 128:(ui + 1) * 128], Nm[:, ui, :], I_, r=["Nm"], w=[K1])
                P.cp("act", NmT.rearrange("p a b -> p (a b)"), PS(3), r=[K1], w=["NmT"])
                P.tt("pool", R_, Nm, I_.unsqueeze(1).to_broadcast([128, 4, 128]), ALU.add, r=["Nm", "cst"], w=["R"])

            def lvl_bufs(lvl):
                if lvl % 2 == 1:
                    return (Nm, NmT, "Nm", "NmT", X12[:, :, 0:128], X12[:, :, 128:256], "X12", "X12")
                return (X12[:, :, 0:128], X12[:, :, 128:256], "X12", "X12", Nm, NmT, "Nm", "NmT")

            def local_sq(lvl):
                Pp, PpT, kp, kpT, Pn, PnT, kn, knT = lvl_bufs(lvl)
                for ui in range(4):
                    P.mm(PS(0)[:, ui * 128:(ui + 1) * 128], PpT[:, ui, :], Pp[:, ui, :], r=[kp, kpT], w=[E0])
                for ui in range(4):
                    P.mm(PS(1)[:, ui * 128:(ui + 1) * 128], Pp[:, ui, :], PpT[:, ui, :], r=[kp, kpT], w=[E1])
                P.cp("dve", PnT, PS(1).rearrange("p (u c) -> p u c", u=4), r=[E1], w=[knT])
                if lvl < 6:
                    P.cp("act", Pn, PS(0).rearrange("p (u c) -> p u c", u=4), r=[E0], w=[kn])

            def local_ru(lvl):
                Pp, PpT, kp, kpT, Pn, PnT, kn, knT = lvl_bufs(lvl)
                for ui in range(4):
                    P.mm(PS(2)[:, ui * 128:(ui + 1) * 128], PnT[:, ui, :], R_[:, ui, :], r=[knT, "R"], w=[K0])
                P.tt("dve", R_, R_, PS(2).rearrange("p (u c) -> p u c", u=4), ALU.add, r=["R", K0], w=["R"])

            def local_b(step):
                st = step % 2
                for ui in range(4):
                    dr, hh = ui // 2, ui % 2
                    r0 = hh * 64
                    P.mm(PS(0)[:, ui * 64:(ui + 1) * 64], R_[:, ui, :], vb[:, ui, :], r=["R", "vb"], w=[E0])
                    P.mm(PS(1)[r0:r0 + 64, dr * 128:(dr + 1) * 128], kbg[:, ui, :], R_[:, ui, :], r=["R", "kbg"], w=[E1])
                P.cp("act", u_[st].rearrange("p a b -> p (a b)"), PS(0)[:, 0:256], r=[E0], w=[("u", st)])
                P.cp("dve", wT[st].rearrange("p a b -> p (a b)"), PS(1)[:, 0:256], r=[E1], w=[("wT", st)])

            def scan_a(step):
                st = step % 2
                for ui in range(4):
                    dr, hh = ui // 2, ui % 2
                    ti = order[dr][step]
                    r0 = hh * 64
                    tsl = slice(ti * 128, (ti + 1) * 128)
                    Sv = S_[r0:r0 + 64, dr, :]
                    pb = PS(4 + hh)
                    P.mm(pb[:, dr * 64:(dr + 1) * 64], ycs[r0:r0 + 64, 0, tsl], Sv, r=[("ycs", 0), ("S", dr)], w=[("ps", 4 + hh)])
                    P.mm(pb[:, 128 + dr * 64:128 + (dr + 1) * 64], wT[st][r0:r0 + 64, dr, :], Sv, r=[("wT", st), ("S", dr)], w=[("ps", 4 + hh)])
                for hh in range(2):
                    P.tt("dve", vn[:, hh::2, :], u_[st][:, hh::2, :], PS(4 + hh)[:, 128:256].rearrange("p (u c) -> p u c", u=2), ALU.subtract,
                         r=[("u", st), ("ps", 4 + hh)], w=["vn"])
                for ui in range(4):
                    dr, hh = ui // 2, ui % 2
                    ti = order[dr][step]
                    P.ts("dve", o1[:, ui, :], PS(4 + hh)[:, dr * 64:(dr + 1) * 64], egs[:, ti, c0s[dr] + hh:c0s[dr] + hh + 1], None, ALU.mult, None,
                         r=[("ps", 4 + hh), "egs"], w=["o1"])

            def scan_b(step):
                st = step % 2
                for ui in range(4):
                    dr, hh = ui // 2, ui % 2
                    r0 = hh * 64
                    P.mm(PS(6)[:, ui * 64:(ui + 1) * 64], AQ[st][:, ui, :], vn[:, ui, :], r=[("AQ", st), "vn"], w=[("ps", 6)])
                    P.mm(PS(7)[r0:r0 + 64, dr * 64:(dr + 1) * 64], kt[st][:, ui, :], vn[:, ui, :], r=[("kt", st), "vn"], w=[("ps", 7)])
                P.tt("dve", o1.rearrange("p a b -> p (a b)"), o1.rearrange("p a b -> p (a b)"), PS(6)[:, 0:256], ALU.add, r=["o1", ("ps", 6)], w=["o1"])
                for dr in range(2):
                    ti = order[dr][step]
                    oa = o_acc[:, ti, hp * 128:(hp + 1) * 128]
                    ok = ("o_acc", ti, hp)
                    src = o1[:, dr * 2:dr * 2 + 2, :].rearrange("p a b -> p (a b)")
                    if ok not in P.lastw:
                        P.cp("pool", oa, src, r=["o1"], w=[ok])
                    else:
                        P.tt("pool", oa, oa, src, ALU.add, r=["o1", ok], w=[ok])
                    P.stt("dve", S_[:, dr, :], S_[:, dr, :], egl2[:, ti, dr:dr + 1], PS(7)[:, dr * 64:(dr + 1) * 64], ALU.mult, ALU.add,
                          r=[("S", dr), "egl2", ("ps", 7)], w=[("S", dr)])

            local_a(0)
            local_sq(1)
            for lvl in range(2, 7):
                local_sq(lvl)
                local_ru(lvl - 1)
            local_ru(6)
            local_b(0)
            for step in range(NT):
                nxt = step + 1 < NT
                if nxt:
                    local_a(step + 1)
                    local_sq(1)
                scan_a(step)
                if nxt:
                    local_sq(2)
                    local_ru(1)
                    local_sq(3)
                    local_ru(2)
                scan_b(step)
                if nxt:
                    local_sq(4)
                    local_ru(3)
                    local_sq(5)
                    local_ru(4)
                    local_sq(6)
                    local_ru(5)
                    local_ru(6)
                    local_b(step + 1)

        P.barrier()
        A.top = units_start
        ot = [A.alloc([256]) for _ in range(3)]
        osq = [A.alloc([256]) for _ in range(3)]
        oss = [A.alloc([8]) for _ in range(3)]
        sg = [A.alloc([256]) for _ in range(3)]
        lists = []
        for ti in range(NT):
            k = ti % 3
            kb = ti % 2
            P.record()
            pb = PS(kb)
            for dc in range(8):
                P.mm(pb[:, 0:256], hT[:, dc, ti * 128:(ti + 1) * 128], w_ga[:, dc, 0:256], r=[("hT", ti), "w_ga"], w=[("ps", kb)],
                     start=(dc == 0), stop=(dc == 7))
            P.act(sg[k], pb[:, 0:256], AF.Silu, r=[("ps", kb)], w=[("sg", k)])
            oa = o_acc[:, ti, :]
            okeys = [("o_acc", ti, h) for h in range(2)]
            P.tt("dve", osq[k], oa, oa, ALU.mult, r=okeys, w=[("osq", k)])
            P.op("dve", lambda e, k=k: e.tensor_reduce(out=oss[k][:, 0:4], in_=osq[k].rearrange("p (h d) -> p h d", h=4), axis=AX.X, op=ALU.add),
                 r=[("osq", k)], w=[("oss", k)])
            P.act(oss[k][:, 4:8], oss[k][:, 0:4], AF.Sqrt, r=[("oss", k), "epsc"], w=[("oss", k)], bias=epsc[:, 0:1], scale=1.0 / 64)
            P.op("dve", lambda e, k=k: e.reciprocal(out=oss[k][:, 0:4], in_=oss[k][:, 4:8]), r=[("oss", k)], w=[("oss", k)])
            P.tt("dve", ot[k].rearrange("p (h d) -> p h d", h=4), oa.rearrange("p (h d) -> p h d", h=4),
                 oss[k][:, 0:4].unsqueeze(2).to_broadcast([128, 4, 64]), ALU.mult, r=okeys + [("oss", k)], w=[("ot", k)])
            P.tt("pool", ot[k], ot[k], nw_bc, ALU.mult, r=[("ot", k), "nw"], w=[("ot", k)])
            P.tt("pool", ot[k], ot[k], sg[k], ALU.mult, r=[("ot", k), ("sg", k)], w=[("ot", k)])
            pt = PS(2 + kb)
            P.tp(pt[:, 0:128], ot[k][:, 0:128], I_, r=[("ot", k)], w=[("ps", 2 + kb)])
            P.tp(pt[:, 128:256], ot[k][:, 128:256], I_, r=[("ot", k)], w=[("ps", 2 + kb)])
            P.cp("act", mixT[:, 0, ti * 128:(ti + 1) * 128], pt[:, 0:128], r=[("ps", 2 + kb)], w=[("mixT", 0, ti)])
            P.cp("act", mixT[:, 1, ti * 128:(ti + 1) * 128], pt[:, 128:256], r=[("ps", 2 + kb)], w=[("mixT", 1, ti)])
            lists.append(P.stop_record())
        P.replay_interleaved(lists, max(1, len(lists[0]) // 3))
        P.barrier()

    def rms_rope(eng_t, src_ps, dst, nh, hd, wbc, tabs, ti, tk, rope, keys_r, key_w, tmp, tmpk, ss, ssk):
        v3 = lambda a: a.rearrange("p (h d) -> p h d", h=nh)
        tmpv = tmp[:, 0:nh * hd]
        P.act(tmpv, src_ps, AF.Square, r=keys_r, w=[tmpk])
        P.op("dve", lambda e: e.tensor_reduce(out=ss[:, 0:nh], in_=v3(tmpv), axis=AX.X, op=ALU.add), r=[tmpk], w=[ssk])
        P.act(ss[:, nh:2 * nh], ss[:, 0:nh], AF.Sqrt, r=[ssk, "epsc"], w=[ssk], bias=epsc[:, 0:1], scale=1.0 / hd)
        P.op("dve", lambda e: e.reciprocal(out=ss[:, 0:nh], in_=ss[:, nh:2 * nh]), r=[ssk], w=[ssk])
        P.tt("dve", v3(dst), v3(src_ps), ss[:, 0:nh].unsqueeze(2).to_broadcast([128, nh, hd]), ALU.mult, r=keys_r + [ssk], w=[key_w])
        P.tt("pool", dst, dst, wbc, ALU.mult, r=[key_w, "normw"], w=[key_w])
        if rope:
            rope_apply(dst, key_w, nh, hd, hd, 0, tabs, ti, tmp, tmpk)

    def rope_apply(dst, key_w, nh, stride_h, rd, off, tabs, ti, tmp, tmpk):
        q = rd // 4
        xi = ti - NCTX_T
        d4 = dst.rearrange("p (h d) -> p h d", h=nh)[:, :, off:off + rd]
        t4 = tmp[:, 0:nh * rd].rearrange("p (h d) -> p h d", h=nh)
        cos = tabs[:, xi, 0, :].unsqueeze(1).to_broadcast([128, nh, rd])
        sin = tabs[:, xi, 1, :].unsqueeze(1).to_broadcast([128, nh, rd])
        for blk in range(2):
            for hf_ in range(2):
                a0 = blk * 2 * q + hf_ * q
                b0 = blk * 2 * q + (1 - hf_) * q
                P.tt("pool", t4[:, :, a0:a0 + q], d4[:, :, b0:b0 + q], sin[:, :, a0:a0 + q], ALU.mult, r=[key_w, "rope"], w=[tmpk])
        P.tt("dve", d4, d4, cos, ALU.mult, r=[key_w, "rope"], w=[key_w])
        P.tt("dve", d4, d4, t4, ALU.add, r=[key_w, tmpk], w=[key_w])

    def attention(QT, KT_of, V_of, nheads, scale, mixT, chunk_of, l, kq_rows):
        PT = [A.alloc([512], BF16) for _ in range(3)]
        rc = [A.alloc([512]) for _ in range(2)]
        qgroups = [(256 + 512 * i, 512, range(NT)) for i in range(4)]
        if l < DEPTH - 1:
            qgroups = [(0, 256, range(NCTX_T))] + qgroups
        its = []
        gid = 0
        for h in range(nheads):
            for (q0, qn, ktiles) in qgroups:
                kts = list(ktiles)
                for ki, kt in enumerate(kts):
                    its.append(dict(h=h, q0=q0, qn=qn, kt=kt, first=(ki == 0), last=(ki == len(kts) - 1), gid=gid))
                gid += 1

        def emitS(i):
            it = its[i]
            qap, qrows, qkeyf = QT(it["h"])
            kap, kkey = KT_of(it["h"])
            qn, q0, kt = it["qn"], it["q0"], it["kt"]
            P.mm(PS(i % 3)[:, 0:qn], kap[qrows, kt * 128:(kt + 1) * 128], qap[qrows, q0:q0 + qn], r=[kkey, qkeyf], w=[("ps", i % 3)])

        def emitE(i):
            qn = its[i]["qn"]
            P.act(PT[i % 3][:, 0:qn], PS(i % 3)[:, 0:qn], AF.Exp, r=[("ps", i % 3)], w=[("PT", i % 3)], scale=scale)

        def emitPV(i):
            it = its[i]
            qn, q0, g = it["qn"], it["q0"], it["gid"]
            po = PS(6 + g % 2)
            pok = ("ps", 6 + g % 2)
            vap, vkey = V_of(it["h"], it["kt"])
            P.mm(po[:, 0:qn], vap, PT[i % 3][:, 0:qn], r=[vkey, ("PT", i % 3)], w=[pok], start=it["first"], stop=it["last"])
            if it["last"]:
                mc, mr0 = chunk_of(it["h"])
                rcb = rc[g % 2]
                rck = ("rc", g % 2)
                P.op("dve", lambda e: e.reciprocal(out=rcb[0:64, 0:qn], in_=po[64:128, 0:qn]), r=[pok], w=[rck])
                P.tt("dve", mixT[mr0:mr0 + 64, mc, q0:q0 + qn], po[0:64, 0:qn], rcb[0:64, 0:qn], ALU.mult, r=[pok, rck],
                     w=[("mixT", mc, q0 // 128 + i_) for i_ in range(qn // 128)])

        n = len(its)
        emitS(0)
        emitS(1)
        for i in range(n):
            emitE(i)
            if i + 2 < n:
                emitS(i + 2)
            emitPV(i)

    def phase_gqa(l, mixT):
        A.top = mix_top
        w_a = A.alloc([8, 768], BF16)
        QTg = A.alloc([4, T], BF16)
        KTg = A.alloc([2, T], BF16)
        Vg = A.alloc([NT, 2, 128], BF16)
        tabs = A.alloc([16, 2, 64])
        qw = A.alloc([512])
        kw = A.alloc([128])
        qtm = [A.alloc([512]) for _ in range(3)]
        ktm = [A.alloc([128]) for _ in range(3)]
        tmp = [A.alloc([512]) for _ in range(3)]
        ss = [A.alloc([16]) for _ in range(3)]
        P.dma("pool", w_a, w_in[l, :, 1040:1808].rearrange("(c p) n -> p c n", p=128), r=[], w=["w_a"])
        P.dma("sp", tabs, ropeg, r=[], w=["rope"])
        for h in range(8):
            P.dma("sp", qw[:, h * 64:(h + 1) * 64], gq_norm[l].partition_broadcast(128), r=[], w=["normw"])
        for h in range(2):
            P.dma("sp", kw[:, h * 64:(h + 1) * 64], gk_norm[l].partition_broadcast(128), r=[], w=["normw"])
        P.op("pool", lambda e: e.memset(Vg[:, :, :, 64:128], 1.0), w=["Vg1"])
        lists = []
        for ti in range(NT):
            k = ti % 3
            kb = ti % 2
            P.record()
            rope = ti >= NCTX_T
            pq, pk_ = PS(kb), PS(2 + kb)
            hk = [("hT", ti), "w_a"]
            for dc in range(8):
                P.mm(pq, hT[:, dc, ti * 128:(ti + 1) * 128], w_a[:, dc, 0:512], r=hk, w=[("ps", kb)], start=(dc == 0), stop=(dc == 7))
            for dc in range(8):
                P.mm(pk_[:, 0:256], hT[:, dc, ti * 128:(ti + 1) * 128], w_a[:, dc, 512:768], r=hk, w=[("ps", 2 + kb)], start=(dc == 0), stop=(dc == 7))
            need_q = rope or (l < DEPTH - 1)
            if need_q:
                rms_rope("dve", pq, qtm[k], 8, 64, qw, tabs, ti, None, rope, [("ps", kb)], ("qtm", k), tmp[k], ("tmp", k), ss[k], ("ss", k))
                pt = PS(4 + kb)
                for c in range(4):
                    P.tp(pt[:, c * 128:(c + 1) * 128], qtm[k][:, c * 128:(c + 1) * 128], I_, r=[("qtm", k)], w=[("ps", 4 + kb)])
                P.cp("act", QTg[:, :, ti * 128:(ti + 1) * 128], pt.rearrange("p (c n) -> p c n", c=4), r=[("ps", 4 + kb)], w=[("QTg", ti)])
            rms_rope("dve", pk_[:, 0:128], ktm[k], 2, 64, kw, tabs, ti, None, rope, [("ps", 2 + kb)], ("ktm", k), tmp[k], ("tmp", k), ss[k], ("ss", k))
            P.cp("dve", Vg[:, ti, :, 0:64], pk_[:, 128:256].rearrange("p (g d) -> p g d", g=2), r=[("ps", 2 + kb)], w=[("Vg", ti)])
            pt2 = PS(6 + kb)
            P.tp(pt2[:, 0:128], ktm[k], I_, r=[("ktm", k)], w=[("ps", 6 + kb)])
            for g in range(2):
                P.cp("act", KTg[0:64, g, ti * 128:(ti + 1) * 128], pt2[g * 64:(g + 1) * 64, 0:128], r=[("ps", 6 + kb)], w=[("KTg", ti)])
                P.cp("act", KTg[64:128, g, ti * 128:(ti + 1) * 128], pt2[g * 64:(g + 1) * 64, 0:128], r=[("ps", 6 + kb)], w=[("KTg", ti)])
            lists.append(P.stop_record())
        P.replay_interleaved(lists, max(1, len(lists[-1]) // 3))
        allq = [("QTg", ti) for ti in range(NT) if (ti >= NCTX_T or l < DEPTH - 1)]
        allk = [("KTg", ti) for ti in range(NT)]
        P.op("pool", lambda e: e.memset(dummy[:, 0:1], 0.0), r=allq, w=["QTgA"])
        P.op("pool", lambda e: e.memset(dummy[:, 1:2], 0.0), r=allk, w=["KTgA"])
        P.op("pool", lambda e: e.memset(dummy[:, 2:3], 0.0), r=[("Vg", ti) for ti in range(NT)] + ["Vg1"], w=["VgA"])

        def QT(h):
            r0 = (h % 2) * 64
            return QTg[:, h // 2, :], slice(r0, r0 + 64), "QTgA"

        def KT_of(h):
            return KTg[:, h // 4, :], "KTgA"

        def V_of(h, kt):
            return Vg[:, kt, h // 4, :], "VgA"

        def chunk_of(h):
            f0 = 256 + 64 * h
            return f0 // 128, f0 % 128

        attention(QT, KT_of, V_of, 8, 64 ** -0.5, mixT, chunk_of, l, None)
        P.barrier()

    def phase_mla(l, mixT):
        A.top = mix_top
        w_a = A.alloc([8, 352], BF16)
        QTm = A.alloc([4, T], BF16)
        KTm = A.alloc([4, T], BF16)
        Vm = A.alloc([NT, 4, 128], BF16)
        tabs = A.alloc([16, 2, 32])
        wuq = A.alloc([2, 512])
        wukv = A.alloc([512])
        qw = A.alloc([192])
        kvw = A.alloc([128])
        cq = [A.alloc([256]) for _ in range(3)]
        ckv = [A.alloc([128]) for _ in range(3)]
        kr = [A.alloc([32]) for _ in range(3)]
        cqT = [A.alloc([2, 128]) for _ in range(3)]
        ckvT = [A.alloc([128]) for _ in range(3)]
        qtm = [A.alloc([512]) for _ in range(3)]
        kcat = [A.alloc([512]) for _ in range(3)]
        tmp = [A.alloc([512]) for _ in range(3)]
        ss = [A.alloc([16]) for _ in range(3)]
        P.dma("pool", w_a, w_in[l, :, 1808:2160].rearrange("(c p) n -> p c n", p=128), r=[], w=["w_a"])
        P.dma("sp", tabs, ropem, r=[], w=["rope"])
        P.dma("sp", wukv, w_ukv[l], r=[], w=["wukv"])
        P.dma("sp", qw, mq_norm[l].partition_broadcast(128), r=[], w=["normw"])
        P.dma("sp", kvw, mkv_norm[l].partition_broadcast(128), r=[], w=["normw"])
        P.op("pool", lambda e: e.memset(Vm[:, :, :, 64:128], 1.0), w=["Vm1"])
        for k in range(3):
            P.op("pool", lambda e, k=k: e.memset(kcat[k], 0.0), w=[("kcat", k)])
            P.op("pool", lambda e, k=k: e.memset(cq[k], 0.0), w=[("cq", k)])
        P.op("pool", lambda e: e.memset(wuq[:, 1, :], 0.0), w=["wuq1"])
        P.dma("sp", wuq[:, 0, :], w_uq[l, 0:128, :], r=[], w=["wuq"])
        P.dma("sp", wuq[0:64, 1, :], w_uq[l, 128:192, :], r=[], w=["wuq1"])
        lists = []
        for ti in range(NT):
            k = ti % 3
            kb = ti % 2
            P.record()
            rope = ti >= NCTX_T
            pz = PS(kb)
            for dc in range(8):
                P.mm(pz[:, 0:352], hT[:, dc, ti * 128:(ti + 1) * 128], w_a[:, dc, :], r=[("hT", ti), "w_a"], w=[("ps", kb)],
                     start=(dc == 0), stop=(dc == 7))
            need_q = rope or (l < DEPTH - 1)
            rms_rope("dve", pz[:, 192:320], ckv[k], 1, 128, kvw, None, ti, None, False, [("ps", kb)], ("ckv", k), tmp[k], ("tmp", k), ss[k], ("ss", k))
            P.cp("dve", kr[k], pz[:, 320:352], r=[("ps", kb)], w=[("kr", k)])
            if rope:
                rope_apply(kr[k], ("kr", k), 1, 32, 32, 0, tabs, ti, tmp[k], ("tmp", k))
            if need_q:
                rms_rope("dve", pz[:, 0:192], cq[k][:, 0:192], 1, 192, qw, None, ti, None, False, [("ps", kb)], ("cq", k), tmp[k], ("tmp", k), ss[k], ("ss", k))
            pt = PS(2 + kb)
            ptk = ("ps", 2 + kb)
            P.tp(pt[:, 0:128], ckv[k], I_, r=[("ckv", k)], w=[ptk])
            if need_q:
                P.tp(pt[:, 128:256], cq[k][:, 0:128], I_, r=[("cq", k)], w=[ptk])
                P.tp(pt[:, 256:384], cq[k][:, 128:256], I_, r=[("cq", k)], w=[ptk])
                P.cp("act", cqT[k].rearrange("p a b -> p (a b)"), pt[:, 128:384], r=[ptk], w=[("cqT", k)])
            P.cp("dve", ckvT[k], pt[:, 0:128], r=[ptk], w=[("ckvT", k)])
            pu = PS(4 + kb)
            puk = ("ps", 4 + kb)
            P.mm(pu, ckvT[k], wukv, r=[("ckvT", k), "wukv"], w=[puk])
            pu3 = pu.rearrange("p (h d) -> p h d", h=4)
            kc3 = kcat[k].rearrange("p (h d) -> p h d", h=4)
            P.cp("dve", kc3[:, :, 0:64], pu3[:, :, 0:64], r=[puk], w=[("kcat", k)])
            P.cp("act", Vm[:, ti, :, 0:64], pu3[:, :, 64:128], r=[puk], w=[("Vm", ti)])
            P.cp("pool", kc3[:, :, 64:96], kr[k].unsqueeze(1).to_broadcast([128, 4, 32]), r=[("kr", k)], w=[("kcat", k)])
            pt2 = PS(6 + kb)
            pt2k = ("ps", 6 + kb)
            for h in range(4):
                P.tp(pt2[:, h * 128:(h + 1) * 128], kcat[k][:, h * 128:(h + 1) * 128], I_, r=[("kcat", k)], w=[pt2k])
            P.cp("act", KTm[:, :, ti * 128:(ti + 1) * 128], pt2.rearrange("p (c n) -> p c n", c=4), r=[pt2k], w=[("KTm", ti)])
            if need_q:
                pq = PS(4 + kb)
                P.mm(pq, cqT[k][:, 0, :], wuq[:, 0, :], r=[("cqT", k), "wuq"], w=[puk], start=True, stop=False)
                P.mm(pq, cqT[k][:, 1, :], wuq[:, 1, :], r=[("cqT", k), "wuq", "wuq1"], w=[puk], start=False, stop=True)
                P.cp("dve", qtm[k], pq, r=[puk], w=[("qtm", k)])
                if rope:
                    rope_apply(qtm[k], ("qtm", k), 4, 128, 32, 64, tabs, ti, tmp[k], ("tmp", k))
                pt3 = PS(6 + kb)
                for h in range(4):
                    P.tp(pt3[:, h * 128:(h + 1) * 128], qtm[k][:, h * 128:(h + 1) * 128], I_, r=[("qtm", k)], w=[pt2k])
                P.cp("act", QTm[:, :, ti * 128:(ti + 1) * 128], pt3.rearrange("p (c n) -> p c n", c=4), r=[pt2k], w=[("QTm", ti)])
            lists.append(P.stop_record())
        P.replay_interleaved(lists, max(1, len(lists[-1]) // 3))
        if CUT <= 5:
            P.barrier()
            return
        allq = [("QTm", ti) for ti in range(NT) if (ti >= NCTX_T or l < DEPTH - 1)]
        P.op("pool", lambda e: e.memset(dummy[:, 0:1], 0.0), r=allq, w=["QTmA"])
        P.op("pool", lambda e: e.memset(dummy[:, 1:2], 0.0), r=[("KTm", ti) for ti in range(NT)], w=["KTmA"])
        P.op("pool", lambda e: e.memset(dummy[:, 2:3], 0.0), r=[("Vm", ti) for ti in range(NT)] + ["Vm1"], w=["VmA"])

        def QT(h):
            return QTm[:, h, :], slice(0, 128), "QTmA"

        def KT_of(h):
            return KTm[:, h, :], "KTmA"

        def V_of(h, kt):
            return Vm[:, kt, h, :], "VmA"

        def chunk_of(h):
            f0 = 768 + 64 * h
            return f0 // 128, f0 % 128

        attention(QT, KT_of, V_of, 4, 96 ** -0.5, mixT, chunk_of, l, None)
        P.barrier()

    def bc_load(dst, src_row, key):
        P.dma("sp", dst, src_row.partition_broadcast(128), r=[], w=[key])

    def phase_out(l, mixT, yacc, gates, gsc):
        A.top = moe_top
        wo = A.alloc([8, 1024], BF16)
        gt = A.alloc([2, 1024])
        lg = A.alloc([1024])
        lb = A.alloc([1024])
        rw = A.alloc([8, NE])
        rb = A.alloc([NE])
        NB = 3
        L = LNM(NB)
        xr = [A.alloc([1024]) for _ in range(NB)]
        xn = [A.alloc([1024]) for _ in range(NB)]
        hf = [A.alloc([8, 128]) for _ in range(NB)]
        lg_ = [A.alloc([NE]) for _ in range(NB)]
        m8 = [A.alloc([8]) for _ in range(NB)]
        ex = [A.alloc([NE]) for _ in range(NB)]
        msk = [A.alloc([NE]) for _ in range(NB)]
        sm = [A.alloc([4]) for _ in range(NB)]
        P.dma("pool", wo, w_out[l].rearrange("(c p) n -> p c n", p=128), r=[], w=["wo"])
        bc_load(gt[:, 0, :], gts[l * 4 + 0], "gt")
        bc_load(gt[:, 1, :], gts[l * 4 + 1], "gt")
        bc_load(lg, ln1_g[l], "lg")
        bc_load(lb, ln1_b[l], "lb")
        bc_load(rb, router_b[l], "rb")
        P.dma("sp", rw, router_w[l].rearrange("(c p) n -> p c n", p=128), r=[], w=["rw"])
        tiles = [ti for ti in range(NT) if not (l == DEPTH - 1 and ti < NCTX_T)]

        def stage_a(ti):
            k = ti % NB
            j = 1 if ti < NCTX_T else 0
            P.dma("act", xr[k], xres[ti * 128:(ti + 1) * 128, :], r=[("xres", ti)], w=[("xr", k)])
            for half in range(2):
                pb = PS(half)
                for fc in range(8):
                    P.mm(pb, mixT[:, fc, ti * 128:(ti + 1) * 128], wo[:, fc, half * 512:(half + 1) * 512], r=[("mixT", fc, ti), "wo"],
                         w=[("ps", half)], start=(fc == 0), stop=(fc == 7))
                P.tt("dve", xn[k][:, half * 512:(half + 1) * 512], pb, gt[:, j, half * 512:(half + 1) * 512], ALU.mult, r=[("ps", half), "gt"],
                     w=[("xn", k)])
            P.stt("dve", xn[k], xr[k], ALPHA, xn[k], ALU.mult, ALU.add, r=[("xr", k), ("xn", k)], w=[("xn", k)])
            mv = ln_stats(L, k, xn[k], ("xn", k))
            P.ts("dve", xn[k], xn[k], mv[:, 0:1], mv[:, 3:4], ALU.subtract, ALU.mult, r=[("xn", k), ("mv", k)], w=[("xn", k)])
            P.tt("pool", xn[k], xn[k], lg, ALU.mult, r=[("xn", k), "lg"], w=[("xn", k)])
            P.tt("pool", xn[k], xn[k], lb, ALU.add, r=[("xn", k), "lb"], w=[("xn", k)])
            P.dma("act", xres[ti * 128:(ti + 1) * 128, :], xn[k], r=[("xn", k)], w=[("xres", ti)])
            if dbg == ("x1", l):
                P.dma("sp", dbg_d[ti * 128:(ti + 1) * 128, :], xn[k], r=[("xn", k)], w=[])

        def stage_b(ti):
            k = ti % NB
            lnmod_tile(L, k, xn[k], ("xn", k), l, ti, 1, hf=hf[k], hfkey=("hf", k), psb=(2, 3))
            pr = PS(4 + k)
            prk = ("ps", 4 + k)
            for dc in range(8):
                P.mm(pr[:, 0:NE], hf[k][:, dc, :], rw[:, dc, :], r=[("hf", k), "rw"], w=[prk], start=(dc == 0), stop=(dc == 7))
            P.tt("dve", lg_[k], pr[:, 0:NE], rb, ALU.add, r=[prk, "rb"], w=[("lg_", k)])
            P.op("dve", lambda e, k=k: e.max(out=m8[k], in_=lg_[k]), r=[("lg_", k)], w=[("m8", k)])
            P.ts("dve", sm[k][:, 0:1], m8[k][:, 0:1], -1.0, None, ALU.mult, None, r=[("m8", k)], w=[("sm", k)])
            P.act(ex[k], lg_[k], AF.Exp, r=[("lg_", k), ("sm", k)], w=[("ex", k)], bias=sm[k][:, 0:1])
            P.ts("dve", msk[k], lg_[k], m8[k][:, 3:4], None, ALU.is_ge, None, r=[("lg_", k), ("m8", k)], w=[("msk", k)])
            P.tt("dve", ex[k], ex[k], msk[k], ALU.mult, r=[("ex", k), ("msk", k)], w=[("ex", k)])
            P.op("dve", lambda e, k=k: e.tensor_reduce(out=sm[k][:, 1:2], in_=ex[k], axis=AX.X, op=ALU.add), r=[("ex", k)], w=[("sm", k)])
            P.op("dve", lambda e, k=k: e.reciprocal(out=sm[k][:, 2:3], in_=sm[k][:, 1:2]), r=[("sm", k)], w=[("sm", k)])
            P.ts("dve", gates[:, ti, :], ex[k], sm[k][:, 2:3], None, ALU.mult, None, r=[("ex", k), ("sm", k)], w=[("gates", ti)])
            P.ts("dve", gsc[:, ti, :], ex[k], sm[k][:, 2:3], 1.0 / 1.702, ALU.mult, ALU.mult, r=[("ex", k), ("sm", k)], w=[("gsc", ti)])

        lists = []
        for ti in tiles:
            P.record()
            stage_a(ti)
            stage_b(ti)
            lists.append(P.stop_record())
        P.replay_interleaved(lists, max(1, len(lists[0]) // NB))
        P.barrier()

    def phase_moe(l, yacc, gsc):
        A.top = moe_work_top
        wg = A.alloc([8, 8, 128], BF16)
        wu = A.alloc([8, 8, 128], BF16)
        wd = A.alloc([2, 8, 512], BF16)
        actT = A.alloc([8, 1152], BF16)
        bg = A.alloc([NE, 8])
        bu1 = A.alloc([NE, 8])
        glt = [A.alloc([512]) for _ in range(2)]
        sgt = [A.alloc([512]) for _ in range(2)]
        upt = [A.alloc([512]) for _ in range(2)]
        P.dma("sp", bg, bgT[l], r=[], w=["bg"])
        P.dma("sp", bu1, buT[l], r=[], w=["bu1"])
        P.ts("dve", bu1, bu1, 1.0, None, ALU.add, None, r=["bu1"], w=["bu1"])
        bd = A.alloc([1024])
        gT = [A.alloc([128]) for _ in range(2)]
        P.dma("sp", bd[0:NE, :], b_down[l], r=[], w=["bd"])
        for ti in range(NT):
            if l == DEPTH - 1 and ti < NCTX_T:
                continue
            k = ti % 2
            pg = PS(6 + k)
            pgk = ("ps", 6 + k)
            P.tp(pg[0:NE, 0:128], gates[:, ti, :], I_, r=[("gates", ti)], w=[pgk])
            P.cp("act", gT[k][0:NE, :], pg[0:NE, 0:128], r=[pgk], w=[("gT", k)])
            for half in range(2):
                py = PS(4 + half)
                P.mm(py, gT[k][0:NE, :], bd[0:NE, half * 512:(half + 1) * 512], r=[("gT", k), "bd"], w=[("ps", 4 + half)])
                P.cp("act", yacc[:, ti, half * 512:(half + 1) * 512], py, r=[("ps", 4 + half)], w=[("yacc", ti, half)])
        t_first = 0 if l < DEPTH - 1 else NCTX_T
        halves = [(t_first * 128, 1152 - t_first * 128 if l < DEPTH - 1 else 1024), None]
        if l < DEPTH - 1:
            halves = [(0, 1152), (1152, 1152)]
        else:
            halves = [(256, 1024), (1280, 1024)]
        cntr = 0
        for e in range(NE):
            for fc in range(8):
                P.dma("pool", wg[:, fc, :, :], wg_d[l, e, :, fc * 128:(fc + 1) * 128].rearrange("(c p) f -> p c f", p=128), r=[], w=[("wg", fc)])
                P.dma("pool", wu[:, fc, :, :], wu_d[l, e, :, fc * 128:(fc + 1) * 128].rearrange("(c p) f -> p c f", p=128), r=[], w=[("wu", fc)])
            for dh in range(2):
                P.dma("pool", wd[:, dh, :, :], wd_d[l, e, :, dh * 512:(dh + 1) * 512].rearrange("(c p) d -> p c d", p=128), r=[], w=[("wd", dh)])
            for (h0, hn) in halves:
                ngrp = (hn + 511) // 512
                for fc in range(8):
                    for gi in range(ngrp):
                        t0 = h0 + gi * 512
                        n = min(512, h0 + hn - t0)
                        hk = [("hT", t0 // 128 + q) for q in range(n // 128)]
                        k = cntr % 2
                        cntr += 1
                        pg_, pu_ = PS(k), PS(2 + k)
                        for dc in range(8):
                            P.mm(pg_[:, 0:n], wg[:, fc, dc, :], hT[:, dc, t0:t0 + n], r=hk + [("wg", fc)], w=[("ps", k)], start=(dc == 0), stop=(dc == 7))
                        for dc in range(8):
                            P.mm(pu_[:, 0:n], wu[:, fc, dc, :], hT[:, dc, t0:t0 + n], r=hk + [("wu", fc)], w=[("ps", 2 + k)], start=(dc == 0), stop=(dc == 7))
                        P.ts("dve", glt[k][:, 0:n], pg_[:, 0:n], bg[:, e, fc:fc + 1], 7.0, ALU.add, ALU.min, r=[("ps", k), "bg"], w=[("glt", k)])
                        P.act(sgt[k][:, 0:n], glt[k][:, 0:n], AF.Silu, r=[("glt", k)], w=[("sgt", k)], scale=1.702)
                        P.ts("dve", upt[k][:, 0:n], pu_[:, 0:n], bu1[:, e, fc:fc + 1], 8.0, ALU.add, ALU.min, r=[("ps", 2 + k), "bu1"], w=[("upt", k)])
                        P.stt("dve", actT[:, fc, t0 - h0:t0 - h0 + n], upt[k][:, 0:n], -6.0, sgt[k][:, 0:n], ALU.max, ALU.mult,
                              r=[("upt", k), ("sgt", k)], w=[("actT", (t0 - h0) // 512)])
                for dh in range(2):
                    for tt_ in range(hn // 128):
                        ti = h0 // 128 + tt_
                        k = cntr % 2
                        cntr += 1
                        py = PS(4 + k)
                        for fc in range(8):
                            P.mm(py, actT[:, fc, tt_ * 128:(tt_ + 1) * 128], wd[:, dh, fc, :], r=[("actT", tt_ // 4), ("wd", dh)], w=[("ps", 4 + k)],
                                 start=(fc == 0), stop=(fc == 7))
                        ya = yacc[:, ti, dh * 512:(dh + 1) * 512]
                        P.stt("dve", ya, py, gsc[:, ti, e:e + 1], ya, ALU.mult, ALU.add, r=[("ps", 4 + k), ("gsc", ti), ("yacc", ti, dh)],
                              w=[("yacc", ti, dh)])
        P.barrier()

    def phase_final(l, yacc):
        A.top = moe_work_top
        gt = A.alloc([2, 1024])
        lg = A.alloc([1024])
        lb = A.alloc([1024])
        NB = 3
        L = LNM(NB)
        xr = [A.alloc([1024]) for _ in range(NB)]
        lists = []
        bc_load(gt[:, 0, :], gts[l * 4 + 2], "gt")
        bc_load(gt[:, 1, :], gts[l * 4 + 3], "gt")
        bc_load(lg, ln2_g[l], "lg")
        bc_load(lb, ln2_b[l], "lb")
        for ti in range(NT):
            if l == DEPTH - 1 and ti < NCTX_T:
                continue
            k = ti % NB
            j = 1 if ti < NCTX_T else 0
            P.record()
            ya = yacc[:, ti, :]
            yk = [("yacc", ti, 0), ("yacc", ti, 1)]
            P.dma("act", xr[k], xres[ti * 128:(ti + 1) * 128, :], r=[("xres", ti)], w=[("xr", k)])
            P.tt("dve", ya, ya, gt[:, j, :], ALU.mult, r=yk + ["gt"], w=yk)
            P.stt("dve", ya, xr[k], ALPHA, ya, ALU.mult, ALU.add, r=[("xr", k)] + yk, w=yk)
            mv = ln_stats(L, k, ya, yk[0])
            P.ts("dve", ya, ya, mv[:, 0:1], mv[:, 3:4], ALU.subtract, ALU.mult, r=yk + [("mv", k)], w=yk)
            P.tt("pool", ya, ya, lg, ALU.mult, r=yk + ["lg"], w=yk)
            P.tt("pool", ya, ya, lb, ALU.add, r=yk + ["lb"], w=yk)
            if l == DEPTH - 1:
                P.dma("sp", out_d[(ti - NCTX_T) * 128:(ti - NCTX_T + 1) * 128, :], ya, r=yk, w=[])
            else:
                P.dma("sp", xres[ti * 128:(ti + 1) * 128, :], ya, r=yk, w=[("xres", ti)])
                lnmod_tile(L, k, ya, yk[0], l + 1, ti, 0, psb=((4, 5), (6, 7), (2, 3))[k])
            lists.append(P.stop_record())
        P.replay_interleaved(lists, max(1, len(lists[0]) // NB))
        P.barrier()

    mixT = A.alloc([8, T], BF16)
    mix_top = A.top
    A.top = base_top
    yacc = A.alloc([NT, 1024])
    moe_work_top = A.top
    moe_top = mix_top

    phase_mod()
    phase_lnmod_input()
    if dbg == ("h1", 0):
        dump_hT(P, A, hT, dbg_d, PS, I_, base_top)
    for l in range(DEPTH):
        if dbg is not None and dbg[0] in ("h1",) and dbg[1] == l:
            break
        if dbg is not None and len(PHASES) < 3:
            P.op("pool", lambda e: e.memset(mixT, 0.0), w=[("mixT", c, ti) for c in range(8) for ti in range(NT)])
            P.barrier()
        if "dn" in PHASES:
            phase_dn(l, mixT)
        if "gqa" in PHASES:
            phase_gqa(l, mixT)
        if "mla" in PHASES:
            phase_mla(l, mixT)
        if dbg == ("mix", l):
            dump_FM(P, A, mixT, "mixT", dbg_d, PS, I_, moe_top)
            break
        phase_out(l, mixT, yacc, gates, gsc)
        if dbg == ("x1", l):
            break
        phase_moe(l, yacc, gsc)
        phase_final(l, yacc)
        if dbg == ("h1", l + 1):
            dump_hT(P, A, hT, dbg_d, PS, I_, moe_work_top)
            break
    return P.finalize()


def dump_FM(P, A, src, name, dbg_d, PS, I_, top):
    A.top = top
    tf = [A.alloc([128]) for _ in range(2)]
    to = [A.alloc([1024]) for _ in range(2)]
    n = 0
    for ti in range(NT):
        k = ti % 2
        for half in range(2):
            pb = PS(half)
            for q in range(4):
                c = half * 4 + q
                kk = n % 2
                n += 1
                P.cp("dve", tf[kk], src[:, c, ti * 128:(ti + 1) * 128], r=[(name, c, ti), (name[:2], ti)], w=[("tf", kk)])
                P.tp(pb[:, q * 128:(q + 1) * 128], tf[kk], I_, r=[("tf", kk)], w=[("ps", half)])
            P.cp("act", to[k][:, half * 512:(half + 1) * 512], pb, r=[("ps", half)], w=[("to", k)])
        P.dma("sp", dbg_d[ti * 128:(ti + 1) * 128, :], to[k], r=[("to", k)], w=[])
    P.barrier()


def dump_hT(P, A, hT, dbg_d, PS, I_, top):
    dump_FM(P, A, hT, "hT", dbg_d, PS, I_, top)


def _consts():
    j = np.arange(128)[:, None]
    c = np.arange(128)[None, :]
    cst = np.zeros((128, 13, 128), np.float32)
    cst[:, 0] = (j == c)
    cst[:, 1] = (j <= c)
    cst[:, 2] = (j >= c)
    cst[:, 3] = (j > c)
    cst[:, 4] = (j < c)
    cst[:, 5] = 1.0
    cst[:, 6] = ((j // 64) == (c // 64))
    s = j
    cst[:, 7] = np.where(c > s, 0.0, NEG)
    cst[:, 8] = np.where(c >= s, 0.0, NEG)
    cst[:, 9] = np.where(c < s, 0.0, NEG)
    cst[:, 10] = np.where(c <= s, 0.0, NEG)
    cst[:, 11] = -1.0
    cst[:, 12] = -1.0
    return cst


def _rope_tab(rd):
    t = np.arange(2048)
    row = (t // 64).astype(np.float32)
    col = (t % 64).astype(np.float32)
    q = rd // 4
    inv = (10000.0 ** (-np.arange(q, dtype=np.float32) / q)).astype(np.float32)
    ar = row[:, None] * inv[None, :]
    ac = col[:, None] * inv[None, :]
    cos = np.concatenate([np.cos(ar), np.cos(ar), np.cos(ac), np.cos(ac)], 1)
    sin = np.concatenate([-np.sin(ar), np.sin(ar), -np.sin(ac), np.sin(ac)], 1)
    tab = np.stack([cos, sin], 1).astype(np.float32)
    return np.ascontiguousarray(tab.reshape(16, 128, 2, rd).transpose(1, 0, 2, 3))


def _prep_shared(inp):
    f = lambda a: np.ascontiguousarray(np.asarray(a, dtype=np.float32))
    sh = {}
    sh["w_mod"] = f(inp["w_mod"])
    sh["b_mod"] = f(inp["b_mod"])
    sh["b_modT"] = f(np.asarray(inp["b_mod"]).reshape(DEPTH, 48, 128).transpose(0, 2, 1))
    sh["w_in"] = f(inp["w_in"])
    sh["convT"] = f(np.asarray(inp["dn_conv"]).reshape(DEPTH, 3, 6, 128).transpose(0, 3, 2, 1))
    sh["a_log"] = f(np.asarray(inp["dn_a_log"]).reshape(DEPTH, 8))
    sh["dt_bias"] = f(np.asarray(inp["dn_dt_bias"]).reshape(DEPTH, 8))
    sh["dn_norm"] = f(inp["dn_norm"])
    sh["gq_norm"] = f(inp["gqa_q_norm"])
    sh["gk_norm"] = f(inp["gqa_k_norm"])
    sh["mq_norm"] = f(inp["mla_q_norm"])
    sh["mkv_norm"] = f(inp["mla_kv_norm"])
    wuq = np.zeros((DEPTH, 192, 4, 128), np.float32)
    wuq[:, :, :, 0:96] = np.asarray(inp["mla_w_uq"]).reshape(DEPTH, 192, 4, 96)
    sh["w_uq"] = wuq.reshape(DEPTH, 192, 512)
    sh["w_ukv"] = f(inp["mla_w_ukv"])
    sh["w_out"] = f(inp["w_out"])
    for k in ("ln1_g", "ln1_b", "ln2_g", "ln2_b", "router_w", "router_b", "exp_w_gate", "exp_w_up", "exp_w_down"):
        sh[k] = f(inp[k])
    sh["bgT"] = f(np.asarray(inp["exp_b_gate"]).reshape(DEPTH, NE, 8, 128).transpose(0, 3, 1, 2))
    sh["buT"] = f(np.asarray(inp["exp_b_up"]).reshape(DEPTH, NE, 8, 128).transpose(0, 3, 1, 2))
    sh["b_down"] = f(inp["exp_b_down"])
    sh["cst"] = _consts()
    sh["ropeg"] = _rope_tab(64)
    sh["ropem"] = _rope_tab(32)
    return sh


def _prep_core(inp, b):
    x = np.asarray(inp["x"], dtype=np.float32)
    ctx = np.asarray(inp["ctx"], dtype=np.float32)
    c = np.asarray(inp["c"], dtype=np.float32)
    cc = np.asarray(inp["c_ctx"], dtype=np.float32)
    xin = np.ascontiguousarray(np.concatenate([ctx[b], x[b]], 0))
    cT = np.ascontiguousarray(np.stack([c[b].reshape(8, 128).T, cc.reshape(8, 128).T], -1))
    return {"xin": xin, "cT": cT}


_NC_CACHE = {}


def kernel(**inputs):
    if "nc" not in _NC_CACHE:
        _NC_CACHE["nc"] = build()
    nc = _NC_CACHE["nc"]
    sh = _prep_shared(inputs)
    in_maps = []
    for b in range(8):
        m = dict(sh)
        m.update(_prep_core(inputs, b))
        in_maps.append(m)
    res = run_bass_kernel_spmd(nc, in_maps, core_ids=list(range(8)))
    out = np.stack([np.asarray(r["out"], dtype=np.float32) for r in res.results], 0)
    return out
```

```python
import contextlib
import numpy as np
import concourse.bass as bass
import concourse.mybir as mybir
from concourse.bass_utils import run_bass_kernel_spmd

F32 = mybir.dt.float32
BF16 = mybir.dt.bfloat16
AF = mybir.ActivationFunctionType
ALU = mybir.AluOpType
AX = mybir.AxisListType

NDMA_SEMS = 6
D = 1024
T = 2304
NT = 18
NCTX_T = 2
DEPTH = 2
NE = 32
EPS = 1e-6
ALPHA = (2 * DEPTH) ** 0.25
NEG = -30000.0
ARENA_W = 53100
PHASES = {"dn", "gqa", "mla"}
CUT = 99


class Prog:
    def __init__(self):
        self.nc = bass.Bass("TRN2", target_bir_lowering=False)
        self.ops = []
        self.lastw = {}
        self.readers = {}
        self.es = contextlib.ExitStack()
        self.n_uid = 0
        self._bar_at = -1

    def sb(self, name, shape, dt):
        self.n_uid += 1
        return self.es.enter_context(self.nc.sbuf_tensor(f"{name}_{self.n_uid}", list(shape), dt))

    def ps(self, name, shape, dt=F32):
        self.n_uid += 1
        return self.es.enter_context(self.nc.psum_tensor(f"{name}_{self.n_uid}", list(shape), dt))

    def record(self):
        self._rec = []

    def stop_record(self):
        L = self._rec
        self._rec = None
        return L

    def replay_interleaved(self, lists, skew):
        def is_ps(k):
            return isinstance(k, tuple) and k[0] == "ps"
        written = set()
        for L in lists:
            for o in L:
                written.update(o[3])
                written.update(k for k in o[2] if is_ps(k))
        first, last = [], []
        for L in lists:
            f, la = {}, {}
            for i, o in enumerate(L):
                for k in list(o[2]) + list(o[3]):
                    if k in written:
                        f.setdefault(k, i)
                        la[k] = i
            first.append(f)
            last.append(la)
        need = max(1, skew)
        lastuse = {}
        for t in range(len(lists)):
            for k, fi in first[t].items():
                if k in lastuse:
                    tp_, lp = lastuse[k]
                    d = t - tp_
                    req = -(-(lp - fi) // d)
                    need = max(need, req)
            for k, la in last[t].items():
                lastuse[k] = (t, la)
        items = []
        for t, L in enumerate(lists):
            for i, o in enumerate(L):
                items.append((i + t * need, t, i, o))
        items.sort(key=lambda x: (x[0], x[1], x[2]))
        for _, _, _, o in items:
            self.op(o[0], o[1], r=o[2], w=o[3], dma=o[4])

    def op(self, eng, fn, r=(), w=(), dma=False):
        if getattr(self, "_rec", None) is not None:
            self._rec.append((eng, fn, list(r), list(w), dma))
            return
        r = list(r)
        w = list(w)
        for k in r:
            if isinstance(k, tuple) and k[0] == "ps" and k not in w:
                w.append(k)
        i = len(self.ops)
        deps = set()
        for k in r:
            if k in self.lastw:
                deps.add(self.lastw[k])
        for k in w:
            if k in self.lastw:
                deps.add(self.lastw[k])
            deps.update(self.readers.get(k, ()))
        for k in r:
            self.readers.setdefault(k, []).append(i)
        for k in w:
            self.lastw[k] = i
            self.readers[k] = []
        self.ops.append(dict(eng=eng, fn=fn, deps=deps, dma=dma, sig=dma))
        return i

    def barrier(self):
        last = {}
        for i, o in enumerate(self.ops):
            if o["fn"] is not None and not o["dma"]:
                last[o["eng"]] = i
        dmas = [i for i, o in enumerate(self.ops) if o["dma"] and i > self._bar_at]
        deps = set(last.values()) | set(dmas)
        self._bar_at = len(self.ops)
        for e in ("pe", "act", "dve", "pool", "sp"):
            self.ops.append(dict(eng=e, fn=None, deps=set(deps), dma=False, sig=False))
        self.lastw = {}
        self.readers = {}

    def mm(self, out, lhsT, rhs, r, w, start=True, stop=True):
        self.op("pe", lambda e: e.matmul(out, lhsT=lhsT, rhs=rhs, start=start, stop=stop), r=r, w=w)

    def tp(self, out, in_, ident, r, w):
        self.op("pe", lambda e: e.transpose(out=out, in_=in_, identity=ident), r=list(r) + ["cst"], w=w)

    def act(self, out, in_, func, r, w, bias=None, scale=None, accum=None):
        kw = {}
        if bias is not None:
            kw["bias"] = bias
        if scale is not None:
            kw["scale"] = scale
        if accum is not None:
            kw["accum_out"] = accum
        self.op("act", lambda e: e.activation(out=out, in_=in_, func=func, **kw), r=r, w=w)

    def ts(self, eng, out, in0, s1, s2, op0, op1, r, w):
        if op1 is None:
            self.op(eng, lambda e: e.tensor_scalar(out=out, in0=in0, scalar1=s1, scalar2=None, op0=op0), r=r, w=w)
        else:
            self.op(eng, lambda e: e.tensor_scalar(out=out, in0=in0, scalar1=s1, scalar2=s2, op0=op0, op1=op1), r=r, w=w)

    def tt(self, eng, out, in0, in1, op, r, w):
        self.op(eng, lambda e: e.tensor_tensor(out=out, in0=in0, in1=in1, op=op), r=r, w=w)

    def stt(self, eng, out, in0, sc, in1, op0, op1, r, w):
        self.op(eng, lambda e: e.scalar_tensor_tensor(out=out, in0=in0, scalar=sc, in1=in1, op0=op0, op1=op1), r=r, w=w)

    def cp(self, eng, out, in_, r, w):
        if eng == "act":
            self.op("act", lambda e: e.activation(out=out, in_=in_, func=AF.Identity), r=r, w=w)
        else:
            self.op(eng, lambda e: e.tensor_copy(out=out, in_=in_), r=r, w=w)

    def dma(self, q, out, in_, r, w):
        self.op(q, lambda e: e.dma_start(out=out, in_=in_), r=r, w=w, dma=True)

    def finalize(self):
        nc = self.nc
        ops = self.ops
        engs = ("pe", "act", "dve", "pool", "sp")
        for i, o in enumerate(ops):
            for j in o["deps"]:
                p = ops[j]
                if p["fn"] is None:
                    continue
                if (not p["dma"]) and p["eng"] == "pe" and o["eng"] == "pe" and not o["dma"] and o["fn"] is not None:
                    continue
                p["sig"] = True
        sems = {e: self.es.enter_context(nc.semaphore(f"s_{e}")) for e in engs}
        dsems = {e: [self.es.enter_context(nc.semaphore(f"d_{e}{k}")) for k in range(NDMA_SEMS)]
                 for e in ("sp", "act", "pool")}
        cnt = {e: 0 for e in engs}
        dcnt = {e: 0 for e in dsems}
        for o in ops:
            if o["fn"] is None:
                continue
            e = o["eng"]
            if o["dma"]:
                n = dcnt[e]
                dcnt[e] += 1
                o["sem"] = dsems[e][n % NDMA_SEMS]
                o["val"] = 16 * (n // NDMA_SEMS + 1)
                o["semkey"] = ("d", e, n % NDMA_SEMS)
            elif o["sig"]:
                cnt[e] += 1
                o["sem"] = sems[e]
                o["val"] = cnt[e]
                o["semkey"] = ("c", e)
        per = {e: [] for e in engs}
        for i, o in enumerate(ops):
            per[o["eng"]].append(i)
        final_dma = {}
        for o in ops:
            if o["fn"] is not None and o["dma"]:
                final_dma[o["semkey"]] = (o["sem"], o["val"])

        def emit(ename, eng):
            seen = {}

            def wait(sem, key, val):
                if seen.get(key, 0) >= val:
                    return
                seen[key] = val
                eng.wait_ge(sem, val)

            for i in per[ename]:
                o = ops[i]
                for j in sorted(o["deps"]):
                    p = ops[j]
                    if p["fn"] is None:
                        continue
                    if (not p["dma"]) and p["eng"] == "pe" and ename == "pe" and not o["dma"] and o["fn"] is not None:
                        continue
                    wait(p["sem"], p["semkey"], p["val"])
                if o["fn"] is None:
                    continue
                if o["dma"] and o["val"] > 16:
                    wait(o["sem"], o["semkey"], o["val"] - 16)
                ins = o["fn"](eng)
                if o["dma"]:
                    ins.then_inc(o["sem"], 16)
                elif o["sig"]:
                    ins.then_inc(o["sem"], 1)
            if ename == "sp":
                for key, (sem, val) in final_dma.items():
                    wait(sem, key, val)

        with nc.Block() as block:
            @block.tensor
            def _(e):
                emit("pe", e)

            @block.scalar
            def _(e):
                emit("act", e)

            @block.vector
            def _(e):
                emit("dve", e)

            @block.gpsimd
            def _(e):
                emit("pool", e)

            @block.sync
            def _(e):
                emit("sp", e)
        self.es.close()
        return nc


class Arena:
    def __init__(self, P):
        self.t = P.sb("arena", [128, ARENA_W], F32)
        self.top = 0

    def alloc(self, free_shape, dt=F32):
        n = int(np.prod(free_shape))
        nw = n if dt == F32 else (n + 1) // 2
        assert self.top + nw <= ARENA_W, f"arena overflow {self.top}+{nw}"
        ap = self.t[:, self.top:self.top + nw]
        self.top += nw
        if dt != F32:
            ap = ap.bitcast(dt)[:, 0:n]
        if len(free_shape) == 2:
            ap = ap.rearrange("p (a b) -> p a b", a=free_shape[0])
        elif len(free_shape) == 3:
            ap = ap.rearrange("p (a b c) -> p a b c", a=free_shape[0], b=free_shape[1])
        elif len(free_shape) == 4:
            ap = ap.rearrange("p (a b c d) -> p a b c d", a=free_shape[0], b=free_shape[1], c=free_shape[2])
        return ap


def build(dbg=None):
    P = Prog()
    nc = P.nc

    def din(name, shape):
        return nc.dram_tensor(name, list(shape), F32, kind="ExternalInput").ap()

    xin = din("xin", [T, D])
    cT_d = din("cT", [128, 8, 2])
    w_mod = din("w_mod", [DEPTH, D, 6 * D])
    b_modT = din("b_modT", [DEPTH, 128, 48])
    b_mod = din("b_mod", [DEPTH, 6 * D])
    w_in = din("w_in", [DEPTH, D, 2160])
    convT = din("convT", [DEPTH, 128, 6, 3])
    a_log = din("a_log", [DEPTH, 8])
    dt_bias = din("dt_bias", [DEPTH, 8])
    dn_norm = din("dn_norm", [DEPTH, 64])
    gq_norm = din("gq_norm", [DEPTH, 64])
    gk_norm = din("gk_norm", [DEPTH, 64])
    mq_norm = din("mq_norm", [DEPTH, 192])
    mkv_norm = din("mkv_norm", [DEPTH, 128])
    w_uq = din("w_uq", [DEPTH, 192, 512])
    w_ukv = din("w_ukv", [DEPTH, 128, 512])
    w_out = din("w_out", [DEPTH, D, D])
    ln1_g = din("ln1_g", [DEPTH, D])
    ln1_b = din("ln1_b", [DEPTH, D])
    ln2_g = din("ln2_g", [DEPTH, D])
    ln2_b = din("ln2_b", [DEPTH, D])
    router_w = din("router_w", [DEPTH, D, NE])
    router_b = din("router_b", [DEPTH, NE])
    wg_d = din("exp_w_gate", [DEPTH, NE, D, D])
    wu_d = din("exp_w_up", [DEPTH, NE, D, D])
    wd_d = din("exp_w_down", [DEPTH, NE, D, D])
    bgT = din("bgT", [DEPTH, 128, NE, 8])
    buT = din("buT", [DEPTH, 128, NE, 8])
    b_down = din("b_down", [DEPTH, NE, D])
    cst_d = din("cst", [128, 13, 128])
    ropeg = din("ropeg", [128, 16, 2, 64])
    ropem = din("ropem", [128, 16, 2, 32])
    out_d = nc.dram_tensor("out", [2048, D], F32, kind="ExternalOutput").ap()
    dbg_d = None
    if dbg is not None:
        dbg_d = nc.dram_tensor("dbg", [T, D], F32, kind="ExternalOutput").ap()

    xres = nc.dram_tensor("xres", [T, D], F32, kind="Internal").ap()
    gts = nc.dram_tensor("gts", [DEPTH * 4, D], F32, kind="Internal").ap()

    A = Arena(P)
    pst = [P.ps(f"ps{i}", [128, 512]) for i in range(8)]

    def PS(i):
        return pst[i][:, :]

    cst = A.alloc([13, 128])
    I_ = cst[:, 0, :]
    ONES = cst[:, 5, :]
    BLK = cst[:, 6, :]
    modT = A.alloc([DEPTH, 48, 2])
    epsc = A.alloc([2])
    P.dma("sp", cst, cst_d, r=[], w=["cst"])
    P.op("pool", lambda e: e.memset(epsc[:, 0:1], EPS), w=["epsc"])
    P.op("pool", lambda e: e.memset(epsc[:, 1:2], 1.0), w=["epsc"])
    dummy = A.alloc([8])
    hT = A.alloc([8, T], BF16)
    gates = A.alloc([NT, NE])
    gsc = A.alloc([NT, NE])
    base_top = A.top

    def phase_mod():
        A.top = base_top
        sT = A.alloc([8, 2])
        srep = A.alloc([2, 8, 128])
        wm = [A.alloc([8, 512]) for _ in range(2)]
        bmb = A.alloc([2, 1024])
        bmT = A.alloc([DEPTH, 48])
        grow = A.alloc([4, 1024])
        P.dma("sp", sT, cT_d, r=[], w=["sT"])
        P.dma("sp", bmT, b_modT.rearrange("l p k -> p l k"), r=[], w=["bmT"])
        P.act(sT, sT, AF.Silu, r=["sT"], w=["sT"])
        for j in range(2):
            for dc in range(8):
                P.ts("dve", srep[:, j, dc, :], ONES, sT[:, dc, j:j + 1], None, ALU.mult, None,
                     r=["sT", "cst"], w=[("srep", j, dc)])
        npc = 0
        for l in range(DEPTH):
            for gi, g in enumerate((2, 5)):
                P.dma("sp", bmb[:, gi, :], b_mod[l, g * 1024:(g + 1) * 1024].partition_broadcast(128), r=[], w=[("bmb", gi)])
            for p in range(12):
                g, half = p // 2, p % 2
                buf = wm[npc % 2]
                bk = ("wm", npc % 2)
                npc += 1
                P.dma("sp", buf, w_mod[l, :, p * 512:(p + 1) * 512].rearrange("(c p) n -> p c n", p=128), r=[], w=[bk])
                if g in (0, 1, 3, 4):
                    for fcl in range(4):
                        k = p * 4 + fcl
                        pm = PS(fcl % 2)[:, 0:2]
                        for dc in range(8):
                            P.mm(pm, buf[:, dc, fcl * 128:(fcl + 1) * 128], sT[:, dc, :], r=[bk, "sT"], w=[("ps", fcl % 2)],
                                 start=(dc == 0), stop=(dc == 7))
                        P.ts("dve", modT[:, l, k, :], pm, bmT[:, l, k:k + 1], 1.0 if g in (1, 4) else 0.0, ALU.add, ALU.add,
                             r=[("ps", fcl % 2), "bmT"], w=["modT"])
                else:
                    gi = 0 if g == 2 else 1
                    for j in range(2):
                        pg = PS(2 + j)
                        for dc in range(8):
                            P.mm(pg, srep[:, j, dc, :], buf[:, dc, :], r=[bk, ("srep", j, dc)], w=[("ps", 2 + j)],
                                 start=(dc == 0), stop=(dc == 7))
                        P.tt("dve", grow[0:1, gi * 2 + j, half * 512:(half + 1) * 512], pg[0:1, :],
                             bmb[0:1, gi, half * 512:(half + 1) * 512], ALU.add,
                             r=[("ps", 2 + j), ("bmb", gi)], w=[("grow", gi * 2 + j, half)])
            for q in range(4):
                P.dma("sp", gts[l * 4 + q:l * 4 + q + 1, :], grow[0:1, q, :], r=[("grow", q, 0), ("grow", q, 1)], w=[("gts", l * 4 + q)])
        P.barrier()

    class LNM:
        def __init__(self, n=2):
            self.n = n
            self.xt = [A.alloc([1024]) for _ in range(n)]
            self.st = [A.alloc([2, 6]) for _ in range(n)]
            self.mv = [A.alloc([4]) for _ in range(n)]
            self.i = 0

    def ln_stats(L, k, x_ap, xkey):
        st, mv = L.st[k], L.mv[k]
        P.op("dve", lambda e: e.bn_stats(out=st[:, 0, :], in_=x_ap[:, 0:512]), r=[xkey], w=[("st", k)])
        P.op("dve", lambda e: e.bn_stats(out=st[:, 1, :], in_=x_ap[:, 512:1024]), r=[xkey], w=[("st", k)])
        P.op("dve", lambda e: e.bn_aggr(out=mv[:, 0:2], in_=st.rearrange("p a b -> p (a b)")), r=[("st", k)], w=[("mv", k)])
        P.act(mv[:, 2:3], mv[:, 1:2], AF.Sqrt, r=[("mv", k), "epsc"], w=[("mv", k)], bias=epsc[:, 0:1])
        P.op("dve", lambda e: e.reciprocal(out=mv[:, 3:4], in_=mv[:, 2:3]), r=[("mv", k)], w=[("mv", k)])
        return mv

    def lnmod_tile(L, k, x_ap, xkey, l, ti, which, hf=None, hfkey=None, psb=(4, 5)):
        mv = ln_stats(L, k, x_ap, xkey)
        xh = L.xt[k]
        P.ts("dve", xh, x_ap, mv[:, 0:1], mv[:, 3:4], ALU.subtract, ALU.mult, r=[xkey, ("mv", k)], w=[("xt", k)])
        j = 1 if ti < NCTX_T else 0
        for half in range(2):
            pb = PS(psb[half])
            for q in range(4):
                dc = half * 4 + q
                P.tp(pb[:, q * 128:(q + 1) * 128], xh[:, dc * 128:(dc + 1) * 128], I_, r=[("xt", k)], w=[("ps", psb[half])])
            for q in range(4):
                dc = half * 4 + q
                ksh = (3 * which + 0) * 8 + dc
                ksc = (3 * which + 1) * 8 + dc
                dst = hT[:, dc, ti * 128:(ti + 1) * 128] if hf is None else hf[:, dc, :]
                dkey = ("hT", ti) if hf is None else hfkey
                P.act(dst, pb[:, q * 128:(q + 1) * 128], AF.Identity, r=[("ps", psb[half]), "modT"], w=[dkey],
                      scale=modT[:, l, ksc, j:j + 1], bias=modT[:, l, ksh, j:j + 1])
        if hf is not None:
            P.cp("pool", hT[:, :, ti * 128:(ti + 1) * 128], hf, r=[hfkey], w=[("hT", ti)])

    def phase_lnmod_input():
        A.top = base_top
        NB = 4
        L = LNM(NB)
        xb = [A.alloc([1024]) for _ in range(NB)]
        lists = []
        for ti in range(NT):
            k = ti % NB
            P.record()
            P.dma("sp", xb[k], xin[ti * 128:(ti + 1) * 128, :], r=[], w=[("xb", k)])
            P.dma("act", xres[ti * 128:(ti + 1) * 128, :], xb[k], r=[("xb", k)], w=[("xres", ti)])
            lnmod_tile(L, k, xb[k], ("xb", k), 0, ti, 0, psb=((4, 5), (6, 7), (2, 3), (0, 1))[k])
            lists.append(P.stop_record())
        P.replay_interleaved(lists, max(1, len(lists[0]) // NB))
        P.barrier()

    def phase_dn(l, mixT):
        A.top = mix_top
        w_dn = A.alloc([8, 3, 128], BF16)
        w_ga = A.alloc([8, 272], BF16)
        cw = A.alloc([6, 3])
        zr = [A.alloc([2308])] * 2
        ycs = A.alloc([3, T])
        kTM = A.alloc([NT, 128])
        vTM = A.alloc([NT, 128])
        o_acc = A.alloc([NT, 256])
        ab = A.alloc([NT, 16])
        g_ = A.alloc([NT, 8])
        lnb = A.alloc([NT, 8])
        beta = A.alloc([NT, 8])
        egs = A.alloc([NT, 24])
        gc = A.alloc([NT, 8])
        ngc = A.alloc([NT, 8])
        gb = A.alloc([NT, 8])
        bge = A.alloc([NT, 8])
        tmp8 = A.alloc([NT, 8])
        al_bc = A.alloc([8])
        dtb_bc = A.alloc([8])
        nw_bc = A.alloc([256])
        S_ = A.alloc([2, 64])
        sq = [A.alloc([512]) for _ in range(2)]
        units_start = A.top
        Dm = A.alloc([4, 256])
        X12 = A.alloc([4, 256])
        Nm = A.alloc([4, 128])
        NmT = A.alloc([4, 128])
        R_ = A.alloc([4, 128])
        AQ = [A.alloc([4, 128]) for _ in range(2)]
        u_ = [A.alloc([4, 64]) for _ in range(2)]
        wT = [A.alloc([2, 128]) for _ in range(2)]
        kt = [A.alloc([4, 64]) for _ in range(2)]
        vb = A.alloc([4, 64])
        kbg = A.alloc([4, 64])
        vn = A.alloc([4, 64])
        o1 = A.alloc([4, 64])
        egl2 = A.alloc([NT, 2])

        P.dma("sp", cw, convT[l], r=[], w=["cw"])
        P.dma("sp", al_bc, a_log[l].partition_broadcast(128), r=[], w=["al"])
        P.dma("sp", dtb_bc, dt_bias[l].partition_broadcast(128), r=[], w=["dtb"])
        for h in range(4):
            P.dma("sp", nw_bc[:, h * 64:(h + 1) * 64], dn_norm[l].partition_broadcast(128), r=[], w=["nw"])
        P.dma("pool", w_ga, w_in[l, :, 768:1040].rearrange("(c p) n -> p c n", p=128), r=[], w=["w_ga"])
        P.act(al_bc, al_bc, AF.Exp, r=["al"], w=["al"])
        P.ts("dve", al_bc, al_bc, -1.0, None, ALU.mult, None, r=["al"], w=["al"])

        for ti in range(NT):
            pb = PS(ti % 8)
            for dc in range(8):
                P.mm(pb[:, 0:16], hT[:, dc, ti * 128:(ti + 1) * 128], w_ga[:, dc, 256:272], r=[("hT", ti), "w_ga"], w=[("ps", ti % 8)],
                     start=(dc == 0), stop=(dc == 7))
            P.cp("dve", ab[:, ti, :], pb[:, 0:16], r=[("ps", ti % 8)], w=[("ab", ti)])
        P.op("pool", lambda e: e.memset(dummy[:, 3:4], 0.0), r=[("ab", ti) for ti in range(NT)], w=["ab"])
        for ti in range(NT):
            P.tt("dve", tmp8[:, ti, :], ab[:, ti, 0:8], dtb_bc, ALU.add, r=["ab", "dtb"], w=["tmp8"])
        P.act(tmp8, tmp8, AF.Exp, r=["tmp8"], w=["tmp8"])
        P.act(tmp8, tmp8, AF.Ln, r=["tmp8", "epsc"], w=["tmp8"], bias=epsc[:, 1:2])
        for ti in range(NT):
            P.tt("dve", g_[:, ti, :], tmp8[:, ti, :], al_bc, ALU.mult, r=["tmp8", "al"], w=["g"])
        P.act(beta, ab[:, :, 8:16], AF.Sigmoid, r=["ab"], w=["beta"])
        P.act(lnb, beta, AF.Ln, r=["beta"], w=["lnb"])
        for ti in range(NT):
            pb = PS(ti % 8)
            pbk = ("ps", ti % 8)
            P.mm(pb[:, 0:4], cst[:, 1, :], g_[:, ti, 0:4], r=["g", "cst"], w=[pbk])
            P.mm(pb[:, 4:8], cst[:, 2, :], g_[:, ti, 4:8], r=["g", "cst"], w=[pbk])
            P.mm(pb[:, 8:12], cst[:, 3, :], g_[:, ti, 0:4], r=["g", "cst"], w=[pbk])
            P.mm(pb[:, 12:16], cst[:, 4, :], g_[:, ti, 4:8], r=["g", "cst"], w=[pbk])
            P.mm(pb[:, 16:24], ONES, g_[:, ti, :], r=["g", "cst"], w=[pbk])
            P.cp("dve", gc[:, ti, :], pb[:, 0:8], r=[pbk], w=[("gc", ti)])
            P.act(egs[:, ti, :], pb[:, 0:24], AF.Exp, r=[pbk], w=[("egs", ti)])
        P.op("pool", lambda e: e.memset(dummy[:, 4:5], 0.0), r=[("gc", ti) for ti in range(NT)], w=["gc"])
        P.op("pool", lambda e: e.memset(dummy[:, 5:6], 0.0), r=[("egs", ti) for ti in range(NT)], w=["egs"])
        P.ts("dve", ngc, gc, -1.0, None, ALU.mult, None, r=["gc"], w=["ngc"])
        P.tt("dve", gb, gc, lnb, ALU.add, r=["gc", "lnb"], w=["gb"])
        P.tt("dve", bge, beta, egs[:, :, 0:8], ALU.mult, r=["beta", "egs"], w=["bge"])

        groups = [(0, 512), (512, 512), (1024, 512), (1536, 512), (2048, 256)]

        def pad_idx(t0):
            return 1 + t0 if t0 < 256 else 3 + t0

        for hp in range(2):
            for j in range(3):
                c0 = j * 256 + hp * 128
                P.dma("pool", w_dn[:, :, j, :], w_in[l, :, c0:c0 + 128].rearrange("(c p) n -> p c n", p=128), r=[], w=[("w_dn", j)])
            for j in range(3):
                z = zr[j % 2]
                zk = ("zr", 0)
                P.op("pool", lambda e, z=z: e.memset(z, 0.0), w=[zk])
                for gi, (t0, n) in enumerate(groups):
                    pb = PS(gi % 2)
                    for dc in range(8):
                        P.mm(pb[:, 0:n], w_dn[:, dc, j, :], hT[:, dc, t0:t0 + n], r=[("w_dn", j)] + [("hT", t0 // 128 + q) for q in range(n // 128)],
                             w=[("ps", gi % 2)], start=(dc == 0), stop=(dc == 7))
                    if t0 == 0:
                        P.cp("act", z[:, 1:257], pb[:, 0:256], r=[("ps", gi % 2)], w=[zk])
                        P.cp("act", z[:, 259:515], pb[:, 256:512], r=[("ps", gi % 2)], w=[zk])
                    else:
                        P.cp("act", z[:, 3 + t0:3 + t0 + n], pb[:, 0:n], r=[("ps", gi % 2)], w=[zk])
                fch = j * 2 + hp
                yc = ycs[:, j, :]
                yk = ("ycs", j)
                for (o0, p0, n) in ((0, 1, 256), (256, 259, 2048)):
                    P.ts("dve", yc[:, o0:o0 + n], z[:, p0 - 1:p0 - 1 + n], cw[:, fch, 0:1], None, ALU.mult, None, r=[zk, "cw"], w=[yk])
                    P.stt("dve", yc[:, o0:o0 + n], z[:, p0:p0 + n], cw[:, fch, 1:2], yc[:, o0:o0 + n], ALU.mult, ALU.add, r=[zk, "cw", yk], w=[yk])
                    P.stt("dve", yc[:, o0:o0 + n], z[:, p0 + 1:p0 + 1 + n], cw[:, fch, 2:3], yc[:, o0:o0 + n], ALU.mult, ALU.add, r=[zk, "cw", yk], w=[yk])
                P.act(yc, yc, AF.Silu, r=[yk], w=[yk])
                if j < 2:
                    for gi, (t0, n) in enumerate(groups):
                        s_ = sq[gi % 2]
                        sk = ("sq", gi % 2)
                        P.act(s_[:, 0:n], yc[:, t0:t0 + n], AF.Square, r=[yk], w=[sk])
                        pb = PS(2 + gi % 2)
                        P.mm(pb[:, 0:n], BLK, s_[:, 0:n], r=[sk, "cst"], w=[("ps", 2 + gi % 2)])
                        P.act(s_[:, 0:n], pb[:, 0:n], AF.Sqrt, r=[("ps", 2 + gi % 2), "epsc"], w=[sk], bias=epsc[:, 0:1])
                        P.op("dve", lambda e, s_=s_, n=n: e.reciprocal(out=s_[:, 0:n], in_=s_[:, 0:n]), r=[sk], w=[sk])
                        if j == 0:
                            P.stt("dve", yc[:, t0:t0 + n], yc[:, t0:t0 + n], 0.125, s_[:, 0:n], ALU.mult, ALU.mult, r=[yk, sk], w=[yk])
                        else:
                            P.tt("dve", yc[:, t0:t0 + n], yc[:, t0:t0 + n], s_[:, 0:n], ALU.mult, r=[yk, sk], w=[yk])
            for ti in range(NT):
                pb = PS(ti % 8)
                pbk = ("ps", ti % 8)
                P.tp(pb[:, 0:128], ycs[:, 1, ti * 128:(ti + 1) * 128], I_, r=[("ycs", 1)], w=[pbk])
                P.tp(pb[:, 128:256], ycs[:, 2, ti * 128:(ti + 1) * 128], I_, r=[("ycs", 2)], w=[pbk])
                P.cp("act", kTM[:, ti, :], pb[:, 0:128], r=[pbk], w=[("kTM", ti)])
                P.cp("dve", vTM[:, ti, :], pb[:, 128:256], r=[pbk], w=[("vTM", ti)])
            P.op("pool", lambda e: e.memset(dummy[:, 6:7], 0.0), r=[("kTM", ti) for ti in range(NT)], w=["kTM"])
            P.op("pool", lambda e: e.memset(dummy[:, 7:8], 0.0), r=[("vTM", ti) for ti in range(NT)], w=["vTM"])

            c0s = [dr * 4 + 2 * hp for dr in range(2)]
            for dr in range(2):
                for hh in range(2):
                    P.cp("pool", egl2[hh * 64:(hh + 1) * 64, :, dr], egs[hh * 64:(hh + 1) * 64, :, 16 + c0s[dr] + hh], r=["egs"], w=["egl2"])
            P.op("dve", lambda e: e.memset(S_, 0.0), w=[("S", 0), ("S", 1)])
            order = [list(range(NT)), [1, 0] + list(range(NT - 1, 1, -1))]
            E0, E1, K0, K1 = ("ps", 0), ("ps", 1), ("ps", 2), ("ps", 3)

            def local_a(step):
                st = step % 2
                for dr in range(2):
                    ti = order[dr][step]
                    c0 = c0s[dr]
                    i2 = I_.unsqueeze(1).to_broadcast([128, 2, 128])
                    P.tt("dve", Dm[:, dr * 2:dr * 2 + 2, 0:128], i2, gb[:, ti, c0:c0 + 2].unsqueeze(2).to_broadcast([128, 2, 128]), ALU.mult,
                         r=["gb", "cst"], w=["Dm"])
                    P.tt("dve", Dm[:, dr * 2:dr * 2 + 2, 128:256], i2, gc[:, ti, c0:c0 + 2].unsqueeze(2).to_broadcast([128, 2, 128]), ALU.mult,
                         r=["gc", "cst"], w=["Dm"])
                    v2 = vTM[:, ti, :].rearrange("p (h d) -> p h d", h=2)
                    k2 = kTM[:, ti, :].rearrange("p (h d) -> p h d", h=2)
                    P.tt("pool", vb[:, dr * 2:dr * 2 + 2, :], v2, beta[:, ti, c0:c0 + 2].unsqueeze(2).to_broadcast([128, 2, 64]), ALU.mult,
                         r=["vTM", "beta"], w=["vb"])
                    P.tt("pool", kbg[:, dr * 2:dr * 2 + 2, :], k2, bge[:, ti, c0:c0 + 2].unsqueeze(2).to_broadcast([128, 2, 64]), ALU.mult,
                         r=["kTM", "bge"], w=["kbg"])
                    P.tt("pool", kt[st][:, dr * 2:dr * 2 + 2, :], k2, egs[:, ti, 8 + c0:8 + c0 + 2].unsqueeze(2).to_broadcast([128, 2, 64]), ALU.mult,
                         r=["kTM", "egs"], w=[("kt", st)])
                for ui in range(4):
                    dr, hh = ui // 2, ui % 2
                    ti = order[dr][step]
                    r0 = hh * 64
                    tsl = slice(ti * 128, (ti + 1) * 128)
                    pe_ = PS(ui // 2)[:, (ui % 2) * 256:(ui % 2) * 256 + 256]
                    pk = ("ps", ui // 2)
                    P.mm(pe_, ONES, Dm[:, ui, :], r=["Dm", "cst"], w=[pk], start=True, stop=False)
                    P.mm(pe_, I_, cst[:, 7 + 2 * dr:9 + 2 * dr, :].rearrange("p a b -> p (a b)"), r=["cst"], w=[pk], start=False, stop=False)
                    P.mm(pe_, Dm[:, ui, 128:256], cst[:, 11:13, :].rearrange("p a b -> p (a b)"), r=["Dm", "cst"], w=[pk], start=False, stop=True)
                    pq_ = PS(2 + hh)[:, dr * 256:dr * 256 + 256]
                    pk2 = ("ps", 2 + hh)
                    P.mm(pq_[:, 0:128], ycs[r0:r0 + 64, 1, tsl], ycs[r0:r0 + 64, 1, tsl], r=[("ycs", 1)], w=[pk2])
                    P.mm(pq_[:, 128:256], ycs[r0:r0 + 64, 1, tsl], ycs[r0:r0 + 64, 0, tsl], r=[("ycs", 1), ("ycs", 0)], w=[pk2])
                for b_ in range(2):
                    P.act(X12[:, 2 * b_:2 * b_ + 2, :].rearrange("p a b -> p (a b)"), PS(b_), AF.Exp, r=[("ps", b_)], w=["X12"])
                for b_ in range(2):
                    pk3 = PS(2 + b_).rearrange("p (u c) -> p u c", u=2)
                    P.stt("dve", Nm[:, b_::2, :], X12[:, b_::2, 0:128], -1.0, pk3[:, :, 0:128], ALU.mult, ALU.mult,
                          r=["X12", ("ps", 2 + b_)], w=["Nm"])
                    P.tt("dve", AQ[st][:, b_::2, :], X12[:, b_::2, 128:256], pk3[:, :, 128:256], ALU.mult,
                         r=["X12", ("ps", 2 + b_)], w=[("AQ", st)])
                for ui in range(4):
                    P.tp(PS(3)[:, ui * 128:(ui + 1) * 128], Nm[:, ui, :], I_, r=["Nm"], w=[K1])
                P.cp("act", NmT.rearrange("p a b -> p (a b)"), PS(3), r=[K1], w=["NmT"])
                P.tt("pool", R_, Nm, I_.unsqueeze(1).to_broadcast([128, 4, 128]), ALU.add, r=["Nm", "cst"], w=["R"])

            def lvl_bufs(lvl):
                if lvl % 2 == 1:
                    return (Nm, NmT, "Nm", "NmT", X12[:, :, 0:128], X12[:, :, 128:256], "X12", "X12")
                return (X12[:, :, 0:128], X12[:, :, 128:256], "X12", "X12", Nm, NmT, "Nm", "NmT")

            def local_sq(lvl):
                Pp, PpT, kp, kpT, Pn, PnT, kn, knT = lvl_bufs(lvl)
                for ui in range(4):
                    P.mm(PS(0)[:, ui * 128:(ui + 1) * 128], PpT[:, ui, :], Pp[:, ui, :], r=[kp, kpT], w=[E0])
                for ui in range(4):
                    P.mm(PS(1)[:, ui * 128:(ui + 1) * 128], Pp[:, ui, :], PpT[:, ui, :], r=[kp, kpT], w=[E1])
                P.cp("dve", PnT, PS(1).rearrange("p (u c) -> p u c", u=4), r=[E1], w=[knT])
                if lvl < 6:
                    P.cp("act", Pn, PS(0).rearrange("p (u c) -> p u c", u=4), r=[E0], w=[kn])

            def local_ru(lvl):
                Pp, PpT, kp, kpT, Pn, PnT, kn, knT = lvl_bufs(lvl)
                for ui in range(4):
                    P.mm(PS(2)[:, ui * 128:(ui + 1) * 128], PnT[:, ui, :], R_[:, ui, :], r=[knT, "R"], w=[K0])
                P.tt("dve", R_, R_, PS(2).rearrange("p (u c) -> p u c", u=4), ALU.add, r=["R", K0], w=["R"])

            def local_b(step):
                st = step % 2
                for ui in range(4):
                    dr, hh = ui // 2, ui % 2
                    r0 = hh * 64
                    P.mm(PS(0)[:, ui * 64:(ui + 1) * 64], R_[:, ui, :], vb[:, ui, :], r=["R", "vb"], w=[E0])
                    P.mm(PS(1)[r0:r0 + 64, dr * 128:(dr + 1) * 128], kbg[:, ui, :], R_[:, ui, :], r=["R", "kbg"], w=[E1])
                P.cp("act", u_[st].rearrange("p a b -> p (a b)"), PS(0)[:, 0:256], r=[E0], w=[("u", st)])
                P.cp("dve", wT[st].rearrange("p a b -> p (a b)"), PS(1)[:, 0:256], r=[E1], w=[("wT", st)])

            def scan_a(step):
                st = step % 2
                for ui in range(4):
                    dr, hh = ui // 2, ui % 2
                    ti = order[dr][step]
                    r0 = hh * 64
                    tsl = slice(ti * 128, (ti + 1) * 128)
                    Sv = S_[r0:r0 + 64, dr, :]
                    pb = PS(4 + hh)
                    P.mm(pb[:, dr * 64:(dr + 1) * 64], ycs[r0:r0 + 64, 0, tsl], Sv, r=[("ycs", 0), ("S", dr)], w=[("ps", 4 + hh)])
                    P.mm(pb[:, 128 + dr * 64:128 + (dr + 1) * 64], wT[st][r0:r0 + 64, dr, :], Sv, r=[("wT", st), ("S", dr)], w=[("ps", 4 + hh)])
                for hh in range(2):
                    P.tt("dve", vn[:, hh::2, :], u_[st][:, hh::2, :], PS(4 + hh)[:, 128:256].rearrange("p (u c) -> p u c", u=2), ALU.subtract,
                         r=[("u", st), ("ps", 4 + hh)], w=["vn"])
                for ui in range(4):
                    dr, hh = ui // 2, ui % 2
                    ti = order[dr][step]
                    P.ts("dve", o1[:, ui, :], PS(4 + hh)[:, dr * 64:(dr + 1) * 64], egs[:, ti, c0s[dr] + hh:c0s[dr] + hh + 1], None, ALU.mult, None,
                         r=[("ps", 4 + hh), "egs"], w=["o1"])

            def scan_b(step):
                st = step % 2
                for ui in range(4):
                    dr, hh = ui // 2, ui % 2
                    r0 = hh * 64
                    P.mm(PS(6)[:, ui * 64:(ui + 1) * 64], AQ[st][:, ui, :], vn[:, ui, :], r=[("AQ", st), "vn"], w=[("ps", 6)])
                    P.mm(PS(7)[r0:r0 + 64, dr * 64:(dr + 1) * 64], kt[st][:, ui, :], vn[:, ui, :], r=[("kt", st), "vn"], w=[("ps", 7)])
                P.tt("dve", o1.rearrange("p a b -> p (a b)"), o1.rearrange("p a b -> p (a b)"), PS(6)[:, 0:256], ALU.add, r=["o1", ("ps", 6)], w=["o1"])
                for dr in range(2):
                    ti = order[dr][step]
                    oa = o_acc[:, ti, hp * 128:(hp + 1) * 128]
                    ok = ("o_acc", ti, hp)
                    src = o1[:, dr * 2:dr * 2 + 2, :].rearrange("p a b -> p (a b)")
                    if ok not in P.lastw:
                        P.cp("pool", oa, src, r=["o1"], w=[ok])
                    else:
                        P.tt("pool", oa, oa, src, ALU.add, r=["o1", ok], w=[ok])
                    P.stt("dve", S_[:, dr, :], S_[:, dr, :], egl2[:, ti, dr:dr + 1], PS(7)[:, dr * 64:(dr + 1) * 64], ALU.mult, ALU.add,
                          r=[("S", dr), "egl2", ("ps", 7)], w=[("S", dr)])

            local_a(0)
            local_sq(1)
            for lvl in range(2, 7):
                local_sq(lvl)
                local_ru(lvl - 1)
            local_ru(6)
            local_b(0)
            for step in range(NT):
                nxt = step + 1 < NT
                if nxt:
                    local_a(step + 1)
                    local_sq(1)
                scan_a(step)
                if nxt:
                    local_sq(2)
                    local_ru(1)
                    local_sq(3)
                    local_ru(2)
                scan_b(step)
                if nxt:
                    local_sq(4)
                    local_ru(3)
                    local_sq(5)
                    local_ru(4)
                    local_sq(6)
                    local_ru(5)
                    local_ru(6)
                    local_b(step + 1)

        P.barrier()
        A.top = units_start
        ot = [A.alloc([256]) for _ in range(3)]
        osq = [A.alloc([256]) for _ in range(3)]
        oss = [A.alloc([8]) for _ in range(3)]
        sg = [A.alloc([256]) for _ in range(3)]
        lists = []
        for ti in range(NT):
            k = ti % 3
            kb = ti % 2
            P.record()
            pb = PS(kb)
            for dc in range(8):
                P.mm(pb[:, 0:256], hT[:, dc, ti * 128:(ti + 1) * 128], w_ga[:, dc, 0:256], r=[("hT", ti), "w_ga"], w=[("ps", kb)],
                     start=(dc == 0), stop=(dc == 7))
            P.act(sg[k], pb[:, 0:256], AF.Silu, r=[("ps", kb)], w=[("sg", k)])
            oa = o_acc[:, ti, :]
            okeys = [("o_acc", ti, h) for h in range(2)]
            P.tt("dve", osq[k], oa, oa, ALU.mult, r=okeys, w=[("osq", k)])
            P.op("dve", lambda e, k=k: e.tensor_reduce(out=oss[k][:, 0:4], in_=osq[k].rearrange("p (h d) -> p h d", h=4), axis=AX.X, op=ALU.add),
                 r=[("osq", k)], w=[("oss", k)])
            P.act(oss[k][:, 4:8], oss[k][:, 0:4], AF.Sqrt, r=[("oss", k), "epsc"], w=[("oss", k)], bias=epsc[:, 0:1], scale=1.0 / 64)
            P.op("dve", lambda e, k=k: e.reciprocal(out=oss[k][:, 0:4], in_=oss[k][:, 4:8]), r=[("oss", k)], w=[("oss", k)])
            P.tt("dve", ot[k].rearrange("p (h d) -> p h d", h=4), oa.rearrange("p (h d) -> p h d", h=4),
                 oss[k][:, 0:4].unsqueeze(2).to_broadcast([128, 4, 64]), ALU.mult, r=okeys + [("oss", k)], w=[("ot", k)])
            P.tt("pool", ot[k], ot[k], nw_bc, ALU.mult, r=[("ot", k), "nw"], w=[("ot", k)])
            P.tt("pool", ot[k], ot[k], sg[k], ALU.mult, r=[("ot", k), ("sg", k)], w=[("ot", k)])
            pt = PS(2 + kb)
            P.tp(pt[:, 0:128], ot[k][:, 0:128], I_, r=[("ot", k)], w=[("ps", 2 + kb)])
            P.tp(pt[:, 128:256], ot[k][:, 128:256], I_, r=[("ot", k)], w=[("ps", 2 + kb)])
            P.cp("act", mixT[:, 0, ti * 128:(ti + 1) * 128], pt[:, 0:128], r=[("ps", 2 + kb)], w=[("mixT", 0, ti)])
            P.cp("act", mixT[:, 1, ti * 128:(ti + 1) * 128], pt[:, 128:256], r=[("ps", 2 + kb)], w=[("mixT", 1, ti)])
            lists.append(P.stop_record())
        P.replay_interleaved(lists, max(1, len(lists[0]) // 3))
        P.barrier()

    def rms_rope(eng_t, src_ps, dst, nh, hd, wbc, tabs, ti, tk, rope, keys_r, key_w, tmp, tmpk, ss, ssk):
        v3 = lambda a: a.rearrange("p (h d) -> p h d", h=nh)
        tmpv = tmp[:, 0:nh * hd]
        P.act(tmpv, src_ps, AF.Square, r=keys_r, w=[tmpk])
        P.op("dve", lambda e: e.tensor_reduce(out=ss[:, 0:nh], in_=v3(tmpv), axis=AX.X, op=ALU.add), r=[tmpk], w=[ssk])
        P.act(ss[:, nh:2 * nh], ss[:, 0:nh], AF.Sqrt, r=[ssk, "epsc"], w=[ssk], bias=epsc[:, 0:1], scale=1.0 / hd)
        P.op("dve", lambda e: e.reciprocal(out=ss[:, 0:nh], in_=ss[:, nh:2 * nh]), r=[ssk], w=[ssk])
        P.tt("dve", v3(dst), v3(src_ps), ss[:, 0:nh].unsqueeze(2).to_broadcast([128, nh, hd]), ALU.mult, r=keys_r + [ssk], w=[key_w])
        P.tt("pool", dst, dst, wbc, ALU.mult, r=[key_w, "normw"], w=[key_w])
        if rope:
            rope_apply(dst, key_w, nh, hd, hd, 0, tabs, ti, tmp, tmpk)

    def rope_apply(dst, key_w, nh, stride_h, rd, off, tabs, ti, tmp, tmpk):
        q = rd // 4
        xi = ti - NCTX_T
        d4 = dst.rearrange("p (h d) -> p h d", h=nh)[:, :, off:off + rd]
        t4 = tmp[:, 0:nh * rd].rearrange("p (h d) -> p h d", h=nh)
        cos = tabs[:, xi, 0, :].unsqueeze(1).to_broadcast([128, nh, rd])
        sin = tabs[:, xi, 1, :].unsqueeze(1).to_broadcast([128, nh, rd])
        for blk in range(2):
            for hf_ in range(2):
                a0 = blk * 2 * q + hf_ * q
                b0 = blk * 2 * q + (1 - hf_) * q
                P.tt("pool", t4[:, :, a0:a0 + q], d4[:, :, b0:b0 + q], sin[:, :, a0:a0 + q], ALU.mult, r=[key_w, "rope"], w=[tmpk])
        P.tt("dve", d4, d4, cos, ALU.mult, r=[key_w, "rope"], w=[key_w])
        P.tt("dve", d4, d4, t4, ALU.add, r=[key_w, tmpk], w=[key_w])

    def attention(QT, KT_of, V_of, nheads, scale, mixT, chunk_of, l, kq_rows):
        PT = [A.alloc([512], BF16) for _ in range(3)]
        rc = [A.alloc([512]) for _ in range(2)]
        qgroups = [(256 + 512 * i, 512, range(NT)) for i in range(4)]
        if l < DEPTH - 1:
            qgroups = [(0, 256, range(NCTX_T))] + qgroups
        its = []
        gid = 0
        for h in range(nheads):
            for (q0, qn, ktiles) in qgroups:
                kts = list(ktiles)
                for ki, kt in enumerate(kts):
                    its.append(dict(h=h, q0=q0, qn=qn, kt=kt, first=(ki == 0), last=(ki == len(kts) - 1), gid=gid))
                gid += 1

        def emitS(i):
            it = its[i]
            qap, qrows, qkeyf = QT(it["h"])
            kap, kkey = KT_of(it["h"])
            qn, q0, kt = it["qn"], it["q0"], it["kt"]
            P.mm(PS(i % 3)[:, 0:qn], kap[qrows, kt * 128:(kt + 1) * 128], qap[qrows, q0:q0 + qn], r=[kkey, qkeyf], w=[("ps", i % 3)])

        def emitE(i):
            qn = its[i]["qn"]
            P.act(PT[i % 3][:, 0:qn], PS(i % 3)[:, 0:qn], AF.Exp, r=[("ps", i % 3)], w=[("PT", i % 3)], scale=scale)

        def emitPV(i):
            it = its[i]
            qn, q0, g = it["qn"], it["q0"], it["gid"]
            po = PS(6 + g % 2)
            pok = ("ps", 6 + g % 2)
            vap, vkey = V_of(it["h"], it["kt"])
            P.mm(po[:, 0:qn], vap, PT[i % 3][:, 0:qn], r=[vkey, ("PT", i % 3)], w=[pok], start=it["first"], stop=it["last"])
            if it["last"]:
                mc, mr0 = chunk_of(it["h"])
                rcb = rc[g % 2]
                rck = ("rc", g % 2)
                P.op("dve", lambda e: e.reciprocal(out=rcb[0:64, 0:qn], in_=po[64:128, 0:qn]), r=[pok], w=[rck])
                P.tt("dve", mixT[mr0:mr0 + 64, mc, q0:q0 + qn], po[0:64, 0:qn], rcb[0:64, 0:qn], ALU.mult, r=[pok, rck],
                     w=[("mixT", mc, q0 // 128 + i_) for i_ in range(qn // 128)])

        n = len(its)
        emitS(0)
        emitS(1)
        for i in range(n):
            emitE(i)
            if i + 2 < n:
                emitS(i + 2)
            emitPV(i)

    def phase_gqa(l, mixT):
        A.top = mix_top
        w_a = A.alloc([8, 768], BF16)
        QTg = A.alloc([4, T], BF16)
        KTg = A.alloc([2, T], BF16)
        Vg = A.alloc([NT, 2, 128], BF16)
        tabs = A.alloc([16, 2, 64])
        qw = A.alloc([512])
        kw = A.alloc([128])
        qtm = [A.alloc([512]) for _ in range(3)]
        ktm = [A.alloc([128]) for _ in range(3)]
        tmp = [A.alloc([512]) for _ in range(3)]
        ss = [A.alloc([16]) for _ in range(3)]
        P.dma("pool", w_a, w_in[l, :, 1040:1808].rearrange("(c p) n -> p c n", p=128), r=[], w=["w_a"])
        P.dma("sp", tabs, ropeg, r=[], w=["rope"])
        for h in range(8):
            P.dma("sp", qw[:, h * 64:(h + 1) * 64], gq_norm[l].partition_broadcast(128), r=[], w=["normw"])
        for h in range(2):
            P.dma("sp", kw[:, h * 64:(h + 1) * 64], gk_norm[l].partition_broadcast(128), r=[], w=["normw"])
        P.op("pool", lambda e: e.memset(Vg[:, :, :, 64:128], 1.0), w=["Vg1"])
        lists = []
        for ti in range(NT):
            k = ti % 3
            kb = ti % 2
            P.record()
            rope = ti >= NCTX_T
            pq, pk_ = PS(kb), PS(2 + kb)
            hk = [("hT", ti), "w_a"]
            for dc in range(8):
                P.mm(pq, hT[:, dc, ti * 128:(ti + 1) * 128], w_a[:, dc, 0:512], r=hk, w=[("ps", kb)], start=(dc == 0), stop=(dc == 7))
            for dc in range(8):
                P.mm(pk_[:, 0:256], hT[:, dc, ti * 128:(ti + 1) * 128], w_a[:, dc, 512:768], r=hk, w=[("ps", 2 + kb)], start=(dc == 0), stop=(dc == 7))
            need_q = rope or (l < DEPTH - 1)
            if need_q:
                rms_rope("dve", pq, qtm[k], 8, 64, qw, tabs, ti, None, rope, [("ps", kb)], ("qtm", k), tmp[k], ("tmp", k), ss[k], ("ss", k))
                pt = PS(4 + kb)
                for c in range(4):
                    P.tp(pt[:, c * 128:(c + 1) * 128], qtm[k][:, c * 128:(c + 1) * 128], I_, r=[("qtm", k)], w=[("ps", 4 + kb)])
                P.cp("act", QTg[:, :, ti * 128:(ti + 1) * 128], pt.rearrange("p (c n) -> p c n", c=4), r=[("ps", 4 + kb)], w=[("QTg", ti)])
            rms_rope("dve", pk_[:, 0:128], ktm[k], 2, 64, kw, tabs, ti, None, rope, [("ps", 2 + kb)], ("ktm", k), tmp[k], ("tmp", k), ss[k], ("ss", k))
            P.cp("dve", Vg[:, ti, :, 0:64], pk_[:, 128:256].rearrange("p (g d) -> p g d", g=2), r=[("ps", 2 + kb)], w=[("Vg", ti)])
            pt2 = PS(6 + kb)
            P.tp(pt2[:, 0:128], ktm[k], I_, r=[("ktm", k)], w=[("ps", 6 + kb)])
            for g in range(2):
                P.cp("act", KTg[0:64, g, ti * 128:(ti + 1) * 128], pt2[g * 64:(g + 1) * 64, 0:128], r=[("ps", 6 + kb)], w=[("KTg", ti)])
                P.cp("act", KTg[64:128, g, ti * 128:(ti + 1) * 128], pt2[g * 64:(g + 1) * 64, 0:128], r=[("ps", 6 + kb)], w=[("KTg", ti)])
            lists.append(P.stop_record())
        P.replay_interleaved(lists, max(1, len(lists[-1]) // 3))
        allq = [("QTg", ti) for ti in range(NT) if (ti >= NCTX_T or l < DEPTH - 1)]
        allk = [("KTg", ti) for ti in range(NT)]
        P.op("pool", lambda e: e.memset(dummy[:, 0:1], 0.0), r=allq, w=["QTgA"])
        P.op("pool", lambda e: e.memset(dummy[:, 1:2], 0.0), r=allk, w=["KTgA"])
        P.op("pool", lambda e: e.memset(dummy[:, 2:3], 0.0), r=[("Vg", ti) for ti in range(NT)] + ["Vg1"], w=["VgA"])

        def QT(h):
            r0 = (h % 2) * 64
            return QTg[:, h // 2, :], slice(r0, r0 + 64), "QTgA"

        def KT_of(h):
            return KTg[:, h // 4, :], "KTgA"

        def V_of(h, kt):
            return Vg[:, kt, h // 4, :], "VgA"

        def chunk_of(h):
            f0 = 256 + 64 * h
            return f0 // 128, f0 % 128

        attention(QT, KT_of, V_of, 8, 64 ** -0.5, mixT, chunk_of, l, None)
        P.barrier()

    def phase_mla(l, mixT):
        A.top = mix_top
        w_a = A.alloc([8, 352], BF16)
        QTm = A.alloc([4, T], BF16)
        KTm = A.alloc([4, T], BF16)
        Vm = A.alloc([NT, 4, 128], BF16)
        tabs = A.alloc([16, 2, 32])
        wuq = A.alloc([2, 512])
        wukv = A.alloc([512])
        qw = A.alloc([192])
        kvw = A.alloc([128])
        cq = [A.alloc([256]) for _ in range(3)]
        ckv = [A.alloc([128]) for _ in range(3)]
        kr = [A.alloc([32]) for _ in range(3)]
        cqT = [A.alloc([2, 128]) for _ in range(3)]
        ckvT = [A.alloc([128]) for _ in range(3)]
        qtm = [A.alloc([512]) for _ in range(3)]
        kcat = [A.alloc([512]) for _ in range(3)]
        tmp = [A.alloc([512]) for _ in range(3)]
        ss = [A.alloc([16]) for _ in range(3)]
        P.dma("pool", w_a, w_in[l, :, 1808:2160].rearrange("(c p) n -> p c n", p=128), r=[], w=["w_a"])
        P.dma("sp", tabs, ropem, r=[], w=["rope"])
        P.dma("sp", wukv, w_ukv[l], r=[], w=["wukv"])
        P.dma("sp", qw, mq_norm[l].partition_broadcast(128), r=[], w=["normw"])
        P.dma("sp", kvw, mkv_norm[l].partition_broadcast(128), r=[], w=["normw"])
        P.op("pool", lambda e: e.memset(Vm[:, :, :, 64:128], 1.0), w=["Vm1"])
        for k in range(3):
            P.op("pool", lambda e, k=k: e.memset(kcat[k], 0.0), w=[("kcat", k)])
            P.op("pool", lambda e, k=k: e.memset(cq[k], 0.0), w=[("cq", k)])
        P.op("pool", lambda e: e.memset(wuq[:, 1, :], 0.0), w=["wuq1"])
        P.dma("sp", wuq[:, 0, :], w_uq[l, 0:128, :], r=[], w=["wuq"])
        P.dma("sp", wuq[0:64, 1, :], w_uq[l, 128:192, :], r=[], w=["wuq1"])
        lists = []
        for ti in range(NT):
            k = ti % 3
            kb = ti % 2
            P.record()
            rope = ti >= NCTX_T
            pz = PS(kb)
            for dc in range(8):
                P.mm(pz[:, 0:352], hT[:, dc, ti * 128:(ti + 1) * 128], w_a[:, dc, :], r=[("hT", ti), "w_a"], w=[("ps", kb)],
                     start=(dc == 0), stop=(dc == 7))
            need_q = rope or (l < DEPTH - 1)
            rms_rope("dve", pz[:, 192:320], ckv[k], 1, 128, kvw, None, ti, None, False, [("ps", kb)], ("ckv", k), tmp[k], ("tmp", k), ss[k], ("ss", k))
            P.cp("dve", kr[k], pz[:, 320:352], r=[("ps", kb)], w=[("kr", k)])
            if rope:
                rope_apply(kr[k], ("kr", k), 1, 32, 32, 0, tabs, ti, tmp[k], ("tmp", k))
            if need_q:
                rms_rope("dve", pz[:, 0:192], cq[k][:, 0:192], 1, 192, qw, None, ti, None, False, [("ps", kb)], ("cq", k), tmp[k], ("tmp", k), ss[k], ("ss", k))
            pt = PS(2 + kb)
            ptk = ("ps", 2 + kb)
            P.tp(pt[:, 0:128], ckv[k], I_, r=[("ckv", k)], w=[ptk])
            if need_q:
                P.tp(pt[:, 128:256], cq[k][:, 0:128], I_, r=[("cq", k)], w=[ptk])
                P.tp(pt[:, 256:384], cq[k][:, 128:256], I_, r=[("cq", k)], w=[ptk])
                P.cp("act", cqT[k].rearrange("p a b -> p (a b)"), pt[:, 128:384], r=[ptk], w=[("cqT", k)])
            P.cp("dve", ckvT[k], pt[:, 0:128], r=[ptk], w=[("ckvT", k)])
            pu = PS(4 + kb)
            puk = ("ps", 4 + kb)
            P.mm(pu, ckvT[k], wukv, r=[("ckvT", k), "wukv"], w=[puk])
            pu3 = pu.rearrange("p (h d) -> p h d", h=4)
            kc3 = kcat[k].rearrange("p (h d) -> p h d", h=4)
            P.cp("dve", kc3[:, :, 0:64], pu3[:, :, 0:64], r=[puk], w=[("kcat", k)])
            P.cp("act", Vm[:, ti, :, 0:64], pu3[:, :, 64:128], r=[puk], w=[("Vm", ti)])
            P.cp("pool", kc3[:, :, 64:96], kr[k].unsqueeze(1).to_broadcast([128, 4, 32]), r=[("kr", k)], w=[("kcat", k)])
            pt2 = PS(6 + kb)
            pt2k = ("ps", 6 + kb)
            for h in range(4):
                P.tp(pt2[:, h * 128:(h + 1) * 128], kcat[k][:, h * 128:(h + 1) * 128], I_, r=[("kcat", k)], w=[pt2k])
            P.cp("act", KTm[:, :, ti * 128:(ti + 1) * 128], pt2.rearrange("p (c n) -> p c n", c=4), r=[pt2k], w=[("KTm", ti)])
            if need_q:
                pq = PS(4 + kb)
                P.mm(pq, cqT[k][:, 0, :], wuq[:, 0, :], r=[("cqT", k), "wuq"], w=[puk], start=True, stop=False)
                P.mm(pq, cqT[k][:, 1, :], wuq[:, 1, :], r=[("cqT", k), "wuq", "wuq1"], w=[puk], start=False, stop=True)
                P.cp("dve", qtm[k], pq, r=[puk], w=[("qtm", k)])
                if rope:
                    rope_apply(qtm[k], ("qtm", k), 4, 128, 32, 64, tabs, ti, tmp[k], ("tmp", k))
                pt3 = PS(6 + kb)
                for h in range(4):
                    P.tp(pt3[:, h * 128:(h + 1) * 128], qtm[k][:, h * 128:(h + 1) * 128], I_, r=[("qtm", k)], w=[pt2k])
                P.cp("act", QTm[:, :, ti * 128:(ti + 1) * 128], pt3.rearrange("p (c n) -> p c n", c=4), r=[pt2k], w=[("QTm", ti)])
            lists.append(P.stop_record())
        P.replay_interleaved(lists, max(1, len(lists[-1]) // 3))
        if CUT <= 5:
            P.barrier()
            return
        allq = [("QTm", ti) for ti in range(NT) if (ti >= NCTX_T or l < DEPTH - 1)]
        P.op("pool", lambda e: e.memset(dummy[:, 0:1], 0.0), r=allq, w=["QTmA"])
        P.op("pool", lambda e: e.memset(dummy[:, 1:2], 0.0), r=[("KTm", ti) for ti in range(NT)], w=["KTmA"])
        P.op("pool", lambda e: e.memset(dummy[:, 2:3], 0.0), r=[("Vm", ti) for ti in range(NT)] + ["Vm1"], w=["VmA"])

        def QT(h):
            return QTm[:, h, :], slice(0, 128), "QTmA"

        def KT_of(h):
            return KTm[:, h, :], "KTmA"

        def V_of(h, kt):
            return Vm[:, kt, h, :], "VmA"

        def chunk_of(h):
            f0 = 768 + 64 * h
            return f0 // 128, f0 % 128

        attention(QT, KT_of, V_of, 4, 96 ** -0.5, mixT, chunk_of, l, None)
        P.barrier()

    def bc_load(dst, src_row, key):
        P.dma("sp", dst, src_row.partition_broadcast(128), r=[], w=[key])

    def phase_out(l, mixT, yacc, gates, gsc):
        A.top = moe_top
        wo = A.alloc([8, 1024], BF16)
        gt = A.alloc([2, 1024])
        lg = A.alloc([1024])
        lb = A.alloc([1024])
        rw = A.alloc([8, NE])
        rb = A.alloc([NE])
        NB = 4
        L = LNM(NB)
        xr = [A.alloc([1024]) for _ in range(NB)]
        xn = [A.alloc([1024]) for _ in range(NB)]
        hf = [A.alloc([8, 128]) for _ in range(NB)]
        lg_ = [A.alloc([NE]) for _ in range(NB)]
        m8 = [A.alloc([8]) for _ in range(NB)]
        ex = [A.alloc([NE]) for _ in range(NB)]
        msk = [A.alloc([NE]) for _ in range(NB)]
        sm = [A.alloc([4]) for _ in range(NB)]
        P.dma("pool", wo, w_out[l].rearrange("(c p) n -> p c n", p=128), r=[], w=["wo"])
        bc_load(gt[:, 0, :], gts[l * 4 + 0], "gt")
        bc_load(gt[:, 1, :], gts[l * 4 + 1], "gt")
        bc_load(lg, ln1_g[l], "lg")
        bc_load(lb, ln1_b[l], "lb")
        bc_load(rb, router_b[l], "rb")
        P.dma("sp", rw, router_w[l].rearrange("(c p) n -> p c n", p=128), r=[], w=["rw"])
        tiles = [ti for ti in range(NT) if not (l == DEPTH - 1 and ti < NCTX_T)]

        def stage_a(ti):
            k = ti % NB
            j = 1 if ti < NCTX_T else 0
            P.dma("act", xr[k], xres[ti * 128:(ti + 1) * 128, :], r=[("xres", ti)], w=[("xr", k)])
            for half in range(2):
                pb = PS(half)
                for fc in range(8):
                    P.mm(pb, mixT[:, fc, ti * 128:(ti + 1) * 128], wo[:, fc, half * 512:(half + 1) * 512], r=[("mixT", fc, ti), "wo"],
                         w=[("ps", half)], start=(fc == 0), stop=(fc == 7))
                P.tt("dve", xn[k][:, half * 512:(half + 1) * 512], pb, gt[:, j, half * 512:(half + 1) * 512], ALU.mult, r=[("ps", half), "gt"],
                     w=[("xn", k)])
            P.stt("dve", xn[k], xr[k], ALPHA, xn[k], ALU.mult, ALU.add, r=[("xr", k), ("xn", k)], w=[("xn", k)])
            mv = ln_stats(L, k, xn[k], ("xn", k))
            P.ts("dve", xn[k], xn[k], mv[:, 0:1], mv[:, 3:4], ALU.subtract, ALU.mult, r=[("xn", k), ("mv", k)], w=[("xn", k)])
            P.tt("pool", xn[k], xn[k], lg, ALU.mult, r=[("xn", k), "lg"], w=[("xn", k)])
            P.tt("pool", xn[k], xn[k], lb, ALU.add, r=[("xn", k), "lb"], w=[("xn", k)])
            P.dma("act", xres[ti * 128:(ti + 1) * 128, :], xn[k], r=[("xn", k)], w=[("xres", ti)])
            if dbg == ("x1", l):
                P.dma("sp", dbg_d[ti * 128:(ti + 1) * 128, :], xn[k], r=[("xn", k)], w=[])

        def stage_b(ti):
            k = ti % NB
            lnmod_tile(L, k, xn[k], ("xn", k), l, ti, 1, hf=hf[k], hfkey=("hf", k), psb=(2, 3))
            pr = PS(4 + k)
            prk = ("ps", 4 + k)
            for dc in range(8):
                P.mm(pr[:, 0:NE], hf[k][:, dc, :], rw[:, dc, :], r=[("hf", k), "rw"], w=[prk], start=(dc == 0), stop=(dc == 7))
            P.tt("dve", lg_[k], pr[:, 0:NE], rb, ALU.add, r=[prk, "rb"], w=[("lg_", k)])
            P.op("dve", lambda e, k=k: e.max(out=m8[k], in_=lg_[k]), r=[("lg_", k)], w=[("m8", k)])
            P.ts("dve", sm[k][:, 0:1], m8[k][:, 0:1], -1.0, None, ALU.mult, None, r=[("m8", k)], w=[("sm", k)])
            P.act(ex[k], lg_[k], AF.Exp, r=[("lg_", k), ("sm", k)], w=[("ex", k)], bias=sm[k][:, 0:1])
            P.ts("dve", msk[k], lg_[k], m8[k][:, 3:4], None, ALU.is_ge, None, r=[("lg_", k), ("m8", k)], w=[("msk", k)])
            P.tt("dve", ex[k], ex[k], msk[k], ALU.mult, r=[("ex", k), ("msk", k)], w=[("ex", k)])
            P.op("dve", lambda e, k=k: e.tensor_reduce(out=sm[k][:, 1:2], in_=ex[k], axis=AX.X, op=ALU.add), r=[("ex", k)], w=[("sm", k)])
            P.op("dve", lambda e, k=k: e.reciprocal(out=sm[k][:, 2:3], in_=sm[k][:, 1:2]), r=[("sm", k)], w=[("sm", k)])
            P.ts("dve", gates[:, ti, :], ex[k], sm[k][:, 2:3], None, ALU.mult, None, r=[("ex", k), ("sm", k)], w=[("gates", ti)])
            P.ts("dve", gsc[:, ti, :], ex[k], sm[k][:, 2:3], 1.0 / 1.702, ALU.mult, ALU.mult, r=[("ex", k), ("sm", k)], w=[("gsc", ti)])

        lists = []
        for ti in tiles:
            P.record()
            stage_a(ti)
            stage_b(ti)
            lists.append(P.stop_record())
        P.replay_interleaved(lists, max(1, len(lists[0]) // NB))
        P.barrier()

    def phase_moe(l, yacc, gsc):
        A.top = moe_work_top
        wg = A.alloc([8, 8, 128], BF16)
        wu = A.alloc([8, 8, 128], BF16)
        wd = A.alloc([2, 8, 512], BF16)
        actT = [A.alloc([8, 768], BF16) for _ in range(2)]
        bg = A.alloc([NE, 8])
        bu1 = A.alloc([NE, 8])
        glt = [A.alloc([512], BF16) for _ in range(2)]
        sgt = [A.alloc([512], BF16) for _ in range(2)]
        upt = [A.alloc([512], BF16) for _ in range(2)]
        P.dma("sp", bg, bgT[l], r=[], w=["bg"])
        P.dma("sp", bu1, buT[l], r=[], w=["bu1"])
        P.ts("dve", bu1, bu1, 1.0, None, ALU.add, None, r=["bu1"], w=["bu1"])
        bd = A.alloc([1024])
        gT = [A.alloc([128]) for _ in range(2)]
        P.dma("sp", bd[0:NE, :], b_down[l], r=[], w=["bd"])
        for ti in range(NT):
            if l == DEPTH - 1 and ti < NCTX_T:
                continue
            k = ti % 2
            pg = PS(6 + k)
            pgk = ("ps", 6 + k)
            P.tp(pg[0:NE, 0:128], gates[:, ti, :], I_, r=[("gates", ti)], w=[pgk])
            P.cp("act", gT[k][0:NE, :], pg[0:NE, 0:128], r=[pgk], w=[("gT", k)])
            for half in range(2):
                py = PS(4 + half)
                P.mm(py, gT[k][0:NE, :], bd[0:NE, half * 512:(half + 1) * 512], r=[("gT", k), "bd"], w=[("ps", 4 + half)])
                P.cp("act", yacc[:, ti, half * 512:(half + 1) * 512], py, r=[("ps", 4 + half)], w=[("yacc", ti, half)])
        if l < DEPTH - 1:
            parts = [(0, 6), (6, 6), (12, 6)]
        else:
            parts = [(2, 6), (8, 6), (14, 4)]
        cnt = [0]

        def load_gu(e):
            for fc in range(8):
                P.dma("pool", wg[:, fc, :, :], wg_d[l, e, :, fc * 128:(fc + 1) * 128].rearrange("(c p) f -> p c f", p=128), r=[], w=[("wg", fc)])
                P.dma("pool", wu[:, fc, :, :], wu_d[l, e, :, fc * 128:(fc + 1) * 128].rearrange("(c p) f -> p c f", p=128), r=[], w=[("wu", fc)])

        def load_d(e):
            for dh in range(2):
                P.dma("pool", wd[:, dh, :, :], wd_d[l, e, :, dh * 512:(dh + 1) * 512].rearrange("(c p) d -> p c d", p=128), r=[], w=[("wd", dh)])

        def stage_A(e, pi, slot):
            tile0, ntl = parts[pi]
            h0, hn = tile0 * 128, ntl * 128
            ngrp = (hn + 511) // 512
            for fc in range(8):
                for gi in range(ngrp):
                    t0 = h0 + gi * 512
                    n = min(512, h0 + hn - t0)
                    hk = [("hT", t0 // 128 + q) for q in range(n // 128)]
                    k = cnt[0] % 2
                    cnt[0] += 1
                    pg_, pu_ = PS(k), PS(2 + k)
                    for dc in range(8):
                        P.mm(pg_[:, 0:n], wg[:, fc, dc, :], hT[:, dc, t0:t0 + n], r=hk + [("wg", fc)], w=[("ps", k)], start=(dc == 0), stop=(dc == 7))
                    for dc in range(8):
                        P.mm(pu_[:, 0:n], wu[:, fc, dc, :], hT[:, dc, t0:t0 + n], r=hk + [("wu", fc)], w=[("ps", 2 + k)], start=(dc == 0), stop=(dc == 7))
                    P.ts("dve", glt[k][:, 0:n], pg_[:, 0:n], bg[:, e, fc:fc + 1], 7.0, ALU.add, ALU.min, r=[("ps", k), "bg"], w=[("glt", k)])
                    P.act(sgt[k][:, 0:n], glt[k][:, 0:n], AF.Silu, r=[("glt", k)], w=[("sgt", k)], scale=1.702)
                    P.ts("dve", upt[k][:, 0:n], pu_[:, 0:n], bu1[:, e, fc:fc + 1], 8.0, ALU.add, ALU.min, r=[("ps", 2 + k), "bu1"], w=[("upt", k)])
                    P.stt("dve", actT[slot][:, fc, t0 - h0:t0 - h0 + n], upt[k][:, 0:n], -6.0, sgt[k][:, 0:n], ALU.max, ALU.mult,
                          r=[("upt", k), ("sgt", k)], w=[("actT", slot, gi)])

        def stage_B(e, pi, slot):
            tile0, ntl = parts[pi]
            for dh in range(2):
                for tt_ in range(ntl):
                    ti = tile0 + tt_
                    k = cnt[0] % 2
                    cnt[0] += 1
                    py = PS(4 + k)
                    for fc in range(8):
                        P.mm(py, actT[slot][:, fc, tt_ * 128:(tt_ + 1) * 128], wd[:, dh, fc, :], r=[("actT", slot, tt_ // 4), ("wd", dh)], w=[("ps", 4 + k)],
                             start=(fc == 0), stop=(fc == 7))
                    ya = yacc[:, ti, dh * 512:(dh + 1) * 512]
                    P.stt("dve", ya, py, gsc[:, ti, e:e + 1], ya, ALU.mult, ALU.add, r=[("ps", 4 + k), ("gsc", ti), ("yacc", ti, dh)],
                          w=[("yacc", ti, dh)])

        seq = [(e, pi) for e in range(NE) for pi in range(len(parts))]
        load_gu(0)
        load_d(0)
        stage_A(0, 0, 0)
        for i, (e, pi) in enumerate(seq):
            if i + 1 < len(seq):
                e2, p2 = seq[i + 1]
                if p2 == 0:
                    load_gu(e2)
                stage_A(e2, p2, (i + 1) % 2)
            stage_B(e, pi, i % 2)
            if pi == len(parts) - 1 and e + 1 < NE:
                load_d(e + 1)
        P.barrier()

    def phase_final(l, yacc):
        A.top = moe_work_top
        gt = A.alloc([2, 1024])
        lg = A.alloc([1024])
        lb = A.alloc([1024])
        NB = 4
        L = LNM(NB)
        xr = [A.alloc([1024]) for _ in range(NB)]
        lists = []
        bc_load(gt[:, 0, :], gts[l * 4 + 2], "gt")
        bc_load(gt[:, 1, :], gts[l * 4 + 3], "gt")
        bc_load(lg, ln2_g[l], "lg")
        bc_load(lb, ln2_b[l], "lb")
        for ti in range(NT):
            if l == DEPTH - 1 and ti < NCTX_T:
                continue
            k = ti % NB
            j = 1 if ti < NCTX_T else 0
            P.record()
            ya = yacc[:, ti, :]
            yk = [("yacc", ti, 0), ("yacc", ti, 1)]
            P.dma("act", xr[k], xres[ti * 128:(ti + 1) * 128, :], r=[("xres", ti)], w=[("xr", k)])
            P.tt("dve", ya, ya, gt[:, j, :], ALU.mult, r=yk + ["gt"], w=yk)
            P.stt("dve", ya, xr[k], ALPHA, ya, ALU.mult, ALU.add, r=[("xr", k)] + yk, w=yk)
            mv = ln_stats(L, k, ya, yk[0])
            P.ts("dve", ya, ya, mv[:, 0:1], mv[:, 3:4], ALU.subtract, ALU.mult, r=yk + [("mv", k)], w=yk)
            P.tt("pool", ya, ya, lg, ALU.mult, r=yk + ["lg"], w=yk)
            P.tt("pool", ya, ya, lb, ALU.add, r=yk + ["lb"], w=yk)
            if l == DEPTH - 1:
                P.dma("sp", out_d[(ti - NCTX_T) * 128:(ti - NCTX_T + 1) * 128, :], ya, r=yk, w=[])
            else:
                P.dma("sp", xres[ti * 128:(ti + 1) * 128, :], ya, r=yk, w=[("xres", ti)])
                lnmod_tile(L, k, ya, yk[0], l + 1, ti, 0, psb=((4, 5), (6, 7), (2, 3), (0, 1))[k])
            lists.append(P.stop_record())
        P.replay_interleaved(lists, max(1, len(lists[0]) // NB))
        P.barrier()

    mixT = A.alloc([8, T], BF16)
    mix_top = A.top
    A.top = base_top
    yacc = A.alloc([NT, 1024])
    moe_work_top = A.top
    moe_top = mix_top

    phase_mod()
    phase_lnmod_input()
    if dbg == ("h1", 0):
        dump_hT(P, A, hT, dbg_d, PS, I_, base_top)
    for l in range(DEPTH):
        if dbg is not None and dbg[0] in ("h1",) and dbg[1] == l:
            break
        if dbg is not None and len(PHASES) < 3:
            P.op("pool", lambda e: e.memset(mixT, 0.0), w=[("mixT", c, ti) for c in range(8) for ti in range(NT)])
            P.barrier()
        if "dn" in PHASES:
            phase_dn(l, mixT)
        if "gqa" in PHASES:
            phase_gqa(l, mixT)
        if "mla" in PHASES:
            phase_mla(l, mixT)
        if dbg == ("mix", l):
            dump_FM(P, A, mixT, "mixT", dbg_d, PS, I_, moe_top)
            break
        phase_out(l, mixT, yacc, gates, gsc)
        if dbg == ("x1", l):
            break
        phase_moe(l, yacc, gsc)
        phase_final(l, yacc)
        if dbg == ("h1", l + 1):
            dump_hT(P, A, hT, dbg_d, PS, I_, moe_work_top)
            break
    return P.finalize()


def dump_FM(P, A, src, name, dbg_d, PS, I_, top):
    A.top = top
    tf = [A.alloc([128]) for _ in range(2)]
    to = [A.alloc([1024]) for _ in range(2)]
    n = 0
    for ti in range(NT):
        k = ti % 2
        for half in range(2):
            pb = PS(half)
            for q in range(4):
                c = half * 4 + q
                kk = n % 2
                n += 1
                P.cp("dve", tf[kk], src[:, c, ti * 128:(ti + 1) * 128], r=[(name, c, ti), (name[:2], ti)], w=[("tf", kk)])
                P.tp(pb[:, q * 128:(q + 1) * 128], tf[kk], I_, r=[("tf", kk)], w=[("ps", half)])
            P.cp("act", to[k][:, half * 512:(half + 1) * 512], pb, r=[("ps", half)], w=[("to", k)])
        P.dma("sp", dbg_d[ti * 128:(ti + 1) * 128, :], to[k], r=[("to", k)], w=[])
    P.barrier()


def dump_hT(P, A, hT, dbg_d, PS, I_, top):
    dump_FM(P, A, hT, "hT", dbg_d, PS, I_, top)


def _consts():
    j = np.arange(128)[:, None]
    c = np.arange(128)[None, :]
    cst = np.zeros((128, 13, 128), np.float32)
    cst[:, 0] = (j == c)
    cst[:, 1] = (j <= c)
    cst[:, 2] = (j >= c)
    cst[:, 3] = (j > c)
    cst[:, 4] = (j < c)
    cst[:, 5] = 1.0
    cst[:, 6] = ((j // 64) == (c // 64))
    s = j
    cst[:, 7] = np.where(c > s, 0.0, NEG)
    cst[:, 8] = np.where(c >= s, 0.0, NEG)
    cst[:, 9] = np.where(c < s, 0.0, NEG)
    cst[:, 10] = np.where(c <= s, 0.0, NEG)
    cst[:, 11] = -1.0
    cst[:, 12] = -1.0
    return cst


def _rope_tab(rd):
    t = np.arange(2048)
    row = (t // 64).astype(np.float32)
    col = (t % 64).astype(np.float32)
    q = rd // 4
    inv = (10000.0 ** (-np.arange(q, dtype=np.float32) / q)).astype(np.float32)
    ar = row[:, None] * inv[None, :]
    ac = col[:, None] * inv[None, :]
    cos = np.concatenate([np.cos(ar), np.cos(ar), np.cos(ac), np.cos(ac)], 1)
    sin = np.concatenate([-np.sin(ar), np.sin(ar), -np.sin(ac), np.sin(ac)], 1)
    tab = np.stack([cos, sin], 1).astype(np.float32)
    return np.ascontiguousarray(tab.reshape(16, 128, 2, rd).transpose(1, 0, 2, 3))


def _prep_shared(inp):
    f = lambda a: np.ascontiguousarray(np.asarray(a, dtype=np.float32))
    sh = {}
    sh["w_mod"] = f(inp["w_mod"])
    sh["b_mod"] = f(inp["b_mod"])
    sh["b_modT"] = f(np.asarray(inp["b_mod"]).reshape(DEPTH, 48, 128).transpose(0, 2, 1))
    sh["w_in"] = f(inp["w_in"])
    sh["convT"] = f(np.asarray(inp["dn_conv"]).reshape(DEPTH, 3, 6, 128).transpose(0, 3, 2, 1))
    sh["a_log"] = f(np.asarray(inp["dn_a_log"]).reshape(DEPTH, 8))
    sh["dt_bias"] = f(np.asarray(inp["dn_dt_bias"]).reshape(DEPTH, 8))
    sh["dn_norm"] = f(inp["dn_norm"])
    sh["gq_norm"] = f(inp["gqa_q_norm"])
    sh["gk_norm"] = f(inp["gqa_k_norm"])
    sh["mq_norm"] = f(inp["mla_q_norm"])
    sh["mkv_norm"] = f(inp["mla_kv_norm"])
    wuq = np.zeros((DEPTH, 192, 4, 128), np.float32)
    wuq[:, :, :, 0:96] = np.asarray(inp["mla_w_uq"]).reshape(DEPTH, 192, 4, 96)
    sh["w_uq"] = wuq.reshape(DEPTH, 192, 512)
    sh["w_ukv"] = f(inp["mla_w_ukv"])
    sh["w_out"] = f(inp["w_out"])
    for k in ("ln1_g", "ln1_b", "ln2_g", "ln2_b", "router_w", "router_b", "exp_w_gate", "exp_w_up", "exp_w_down"):
        sh[k] = f(inp[k])
    sh["bgT"] = f(np.asarray(inp["exp_b_gate"]).reshape(DEPTH, NE, 8, 128).transpose(0, 3, 1, 2))
    sh["buT"] = f(np.asarray(inp["exp_b_up"]).reshape(DEPTH, NE, 8, 128).transpose(0, 3, 1, 2))
    sh["b_down"] = f(inp["exp_b_down"])
    sh["cst"] = _consts()
    sh["ropeg"] = _rope_tab(64)
    sh["ropem"] = _rope_tab(32)
    return sh


def _prep_core(inp, b):
    x = np.asarray(inp["x"], dtype=np.float32)
    ctx = np.asarray(inp["ctx"], dtype=np.float32)
    c = np.asarray(inp["c"], dtype=np.float32)
    cc = np.asarray(inp["c_ctx"], dtype=np.float32)
    xin = np.ascontiguousarray(np.concatenate([ctx[b], x[b]], 0))
    cT = np.ascontiguousarray(np.stack([c[b].reshape(8, 128).T, cc.reshape(8, 128).T], -1))
    return {"xin": xin, "cT": cT}


_NC_CACHE = {}


def kernel(**inputs):
    if "nc" not in _NC_CACHE:
        _NC_CACHE["nc"] = build()
    nc = _NC_CACHE["nc"]
    sh = _prep_shared(inputs)
    in_maps = []
    for b in range(8):
        m = dict(sh)
        m.update(_prep_core(inputs, b))
        in_maps.append(m)
    res = run_bass_kernel_spmd(nc, in_maps, core_ids=list(range(8)))
    out = np.stack([np.asarray(r["out"], dtype=np.float32) for r in res.results], 0)
    return out
```
